# Optimizing a Trainium2 kernel written in Bass

```python
import math
import jax, jax.numpy as jnp
from jax import lax
import numpy as np

D_MODEL = 1024
BATCH = 16
SEQ = 2048
DEPTH = 2

EPS = 1e-6
HEAD_DIM = 64
Q_BLOCK = 128
LRU_WIDTH = 512
LRU_BLOCKS = 8
LRU_BLOCK = LRU_WIDTH // LRU_BLOCKS
CONV_WIDTH = 4
LRU_C = 8.0
NSA_HEADS = 8
NSA_KV_GROUPS = 2
NSA_HPG = NSA_HEADS // NSA_KV_GROUPS
CMP_LEN = 32
CMP_STRIDE = 16
CMP_HIDDEN = 128
SLC_LEN = 64
SLC_TOPN = 8
WIN = 512
FORCE_SCORE = 1e4
DSA_HEADS = 16
DSA_KV_HEADS = 2
DSA_HPG = DSA_HEADS // DSA_KV_HEADS
IDX_HEADS = 8
IDX_DIM = 64
IDX_TOPK_MAX = 256
D_FF = -(-8 * D_MODEL // (3 * 256)) * 256
NSA_Q = NSA_HEADS * HEAD_DIM
NSA_KV = NSA_KV_GROUPS * HEAD_DIM
IN0_SIZES = (LRU_WIDTH, LRU_WIDTH, NSA_Q, NSA_KV, NSA_KV, NSA_KV, NSA_KV, NSA_KV, NSA_KV, NSA_HEADS * 3)
IN0 = sum(IN0_SIZES)
MIX0 = LRU_WIDTH + NSA_Q
IN1_SIZES = (DSA_HEADS * HEAD_DIM, DSA_KV_HEADS * HEAD_DIM, DSA_KV_HEADS * HEAD_DIM, IDX_HEADS * IDX_DIM, IDX_DIM, IDX_HEADS)
IN1 = sum(IN1_SIZES)
MIX1 = DSA_HEADS * HEAD_DIM

kernel_name = "hybrid_rglru_nsa_dsa_adaln"


def _split(t, sizes):
    return jnp.split(t, np.cumsum(sizes)[:-1].tolist(), axis=-1)


def rmsnorm(x, g):
    xf = x.astype(jnp.float32)
    y = xf * lax.rsqrt(jnp.mean(xf * xf, axis=-1, keepdims=True) + EPS)
    return (y * g.astype(jnp.float32)).astype(x.dtype)


def modulate(x, g, shift, scale):
    return rmsnorm(x, g) * (1.0 + scale[:, None, :]) + shift[:, None, :]


def masked_softmax(s, mask):
    s = jnp.where(mask, s.astype(jnp.float32), -jnp.inf)
    m = jnp.max(s, axis=-1, keepdims=True)
    m = jnp.where(jnp.isfinite(m), m, 0.0)
    p = jnp.exp(s - m)
    return p / jnp.maximum(jnp.sum(p, axis=-1, keepdims=True), 1e-30)


def swiglu(h, wg, wu, wd):
    return (jax.nn.silu(h @ wg) * (h @ wu)) @ wd


def adaln(c, w_mod, b_mod):
    mod = jax.nn.silu(c) @ w_mod + b_mod
    return jnp.split(mod, 6, axis=-1)


def causal_conv(x, w, b):
    S = x.shape[1]
    xp = jnp.pad(x, ((0, 0), (CONV_WIDTH - 1, 0), (0, 0)))
    out = xp[:, 0:S] * w[0]
    for k in range(1, CONV_WIDTH):
        out = out + xp[:, k:k + S] * w[k]
    return out + b


def rg_lru(x, wa, ba, wx, bx, lam):
    B, S, _ = x.shape
    xf = x.astype(jnp.float32)
    xb = xf.reshape(B, S, LRU_BLOCKS, LRU_BLOCK)
    r = jax.nn.sigmoid(jnp.einsum('bshi,hij->bshj', xb, wa).reshape(B, S, LRU_WIDTH) + ba)
    i = jax.nn.sigmoid(jnp.einsum('bshi,hij->bshj', xb, wx).reshape(B, S, LRU_WIDTH) + bx)
    log_a = -LRU_C * r * jax.nn.softplus(-lam.astype(jnp.float32))
    a = jnp.exp(log_a)
    mult = jnp.sqrt(-jnp.expm1(2.0 * log_a))
    mult = jnp.where((jnp.arange(S) == 0)[None, :, None], 1.0, mult)
    u = mult * (i * xf)

    def combine(e1, e2):
        a1, b1 = e1
        a2, b2 = e2
        return a1 * a2, a2 * b1 + b2

    _, h = lax.associative_scan(combine, (a, u), axis=1)
    return h.astype(x.dtype)


def compress_blocks(kv, pe, w1, w2):
    B, S, G, D = kv.shape
    nc = (S - CMP_LEN) // CMP_STRIDE + 1
    idx = jnp.arange(nc)[:, None] * CMP_STRIDE + jnp.arange(CMP_LEN)[None, :]
    blk = kv[:, idx] + pe[:, None, :]
    flat = blk.transpose(0, 1, 3, 2, 4).reshape(B, nc, G, CMP_LEN * D)
    return jax.nn.gelu(flat @ w1) @ w2


def nsa_mixer(q, k_cmp, v_cmp, k_slc, v_slc, k_win, v_win, gates, pe_k, w1_k, w2_k, pe_v, w1_v, w2_v):
    B, S = q.shape[:2]
    G, R, D = NSA_KV_GROUPS, NSA_HPG, HEAD_DIM
    scale = D ** -0.5
    nc = (S - CMP_LEN) // CMP_STRIDE + 1
    nsel = S // SLC_LEN
    n_top = min(SLC_TOPN, nsel)
    rs = lambda t: t.reshape(B, S, G, D)
    kc = compress_blocks(rs(k_cmp), pe_k, w1_k, w2_k)
    vc = compress_blocks(rs(v_cmp), pe_v, w1_v, w2_v)
    ks = rs(k_slc).reshape(B, nsel, SLC_LEN, G, D).transpose(0, 3, 1, 2, 4)
    vs = rs(v_slc).reshape(B, nsel, SLC_LEN, G, D).transpose(0, 3, 1, 2, 4)
    pad = ((0, 0), (WIN, 0), (0, 0), (0, 0))
    kw_pad = jnp.pad(rs(k_win), pad)
    vw_pad = jnp.pad(rs(v_win), pad)
    c_start = jnp.arange(nc) * CMP_STRIDE
    c_end = c_start + CMP_LEN - 1
    j_start = jnp.arange(nsel) * SLC_LEN
    overlap = ((c_start[:, None] < j_start[None, :] + SLC_LEN)
               & (c_start[:, None] + CMP_LEN > j_start[None, :])).astype(jnp.float32)
    jj = jnp.arange(nsel)
    b_i = jnp.arange(B)[:, None, None, None]
    g_i = jnp.arange(G)[None, :, None, None]

    def block(ib):
        t0 = ib * Q_BLOCK
        tq = t0 + jnp.arange(Q_BLOCK)
        qc = lax.dynamic_slice_in_dim(q, t0, Q_BLOCK, axis=1).reshape(B, Q_BLOCK, G, R, D)
        gc = lax.dynamic_slice_in_dim(gates, t0, Q_BLOCK, axis=1).reshape(B, Q_BLOCK, G, R, 3)
        s = jnp.einsum('bqgrd,bcgd->bgrqc', qc, kc) * scale
        p_cmp = masked_softmax(s, c_end[None, :] <= tq[:, None])
        o_cmp = jnp.einsum('bgrqc,bcgd->bqgrd', p_cmp, vc)
        imp = jnp.einsum('bgrqc,cj->bgqj', p_cmp, overlap)
        cur = tq // SLC_LEN
        forced = (jj[None, :] == 0) | (jj[None, :] == cur[:, None]) | (jj[None, :] == cur[:, None] - 1)
        imp = jnp.where(forced, FORCE_SCORE, imp)
        imp = jnp.where(jj[None, :] <= cur[:, None], imp, -jnp.inf)
        _, sel = lax.top_k(imp, n_top)
        kg = ks[b_i, g_i, sel].reshape(B, G, Q_BLOCK, n_top * SLC_LEN, D)
        vg = vs[b_i, g_i, sel].reshape(B, G, Q_BLOCK, n_top * SLC_LEN, D)
        kpos = sel[..., None] * SLC_LEN + jnp.arange(SLC_LEN)
        smask = (kpos <= tq[:, None, None]).reshape(B, G, 1, Q_BLOCK, n_top * SLC_LEN)
        s = jnp.einsum('bqgrd,bgqkd->bgrqk', qc, kg) * scale
        o_slc = jnp.einsum('bgrqk,bgqkd->bqgrd', masked_softmax(s, smask), vg)
        kw = lax.dynamic_slice_in_dim(kw_pad, t0, WIN + Q_BLOCK, axis=1)
        vw = lax.dynamic_slice_in_dim(vw_pad, t0, WIN + Q_BLOCK, axis=1)
        kpw = t0 - WIN + jnp.arange(WIN + Q_BLOCK)
        wmask = (kpw[None, :] >= 0) & (kpw[None, :] <= tq[:, None]) & (kpw[None, :] > tq[:, None] - WIN)
        s = jnp.einsum('bqgrd,bkgd->bgrqk', qc, kw) * scale
        o_win = jnp.einsum('bgrqk,bkgd->bqgrd', masked_softmax(s, wmask), vw)
        o = gc[..., 0:1] * o_cmp + gc[..., 1:2] * o_slc + gc[..., 2:3] * o_win
        return o.reshape(B, Q_BLOCK, G * R * D).astype(q.dtype)

    out = lax.map(block, jnp.arange(S // Q_BLOCK))
    return out.transpose(1, 0, 2, 3).reshape(B, S, G * R * D)


def dsa_mixer(q, k, v, q_idx, k_idx, w_idx):
    B, S = q.shape[:2]
    G, R, D = DSA_KV_HEADS, DSA_HPG, HEAD_DIM
    scale = D ** -0.5
    n_keep = min(IDX_TOPK_MAX, S // 4)
    kk = k.reshape(B, S, G, D)
    vv = v.reshape(B, S, G, D)
    qi_all = q_idx.reshape(B, S, IDX_HEADS, IDX_DIM)
    kpos = jnp.arange(S)
    b_i = jnp.arange(B)[:, None, None]

    def block(ib):
        t0 = ib * Q_BLOCK
        tq = t0 + jnp.arange(Q_BLOCK)
        qi = lax.dynamic_slice_in_dim(qi_all, t0, Q_BLOCK, axis=1)
        wi = lax.dynamic_slice_in_dim(w_idx, t0, Q_BLOCK, axis=1)
        sc = jax.nn.relu(jnp.einsum('bqhd,bsd->bqhs', qi, k_idx) * IDX_DIM ** -0.5)
        idx_score = jnp.einsum('bqhs,bqh->bqs', sc.astype(jnp.float32), wi.astype(jnp.float32)) * IDX_HEADS ** -0.5
        idx_score = jnp.where(kpos[None, :] <= tq[:, None], idx_score, -jnp.inf)
        _, sel = lax.top_k(idx_score, n_keep)
        kg = kk[b_i, sel]
        vg = vv[b_i, sel]
        qc = lax.dynamic_slice_in_dim(q, t0, Q_BLOCK, axis=1).reshape(B, Q_BLOCK, G, R, D)
        s = jnp.einsum('bqgrd,bqkgd->bgrqk', qc, kg) * scale
        mask = (sel <= tq[None, :, None])[:, None, None]
        o = jnp.einsum('bgrqk,bqkgd->bqgrd', masked_softmax(s, mask), vg)
        return o.reshape(B, Q_BLOCK, G * R * D).astype(q.dtype)

    out = lax.map(block, jnp.arange(S // Q_BLOCK))
    return out.transpose(1, 0, 2, 3).reshape(B, S, G * R * D)


def layer_ab(x, c, norm_mix, norm_ffn, mod_w, mod_b, w_in, conv_w, conv_b, wa, ba, wx, bx, lam,
             pe_k, w1_k, w2_k, pe_v, w1_v, w2_v, w_out, wg, wu, wd):
    sh_m, sc_m, g_m, sh_f, sc_f, g_f = adaln(c, mod_w, mod_b)
    h = modulate(x, norm_mix, sh_m, sc_m)
    (a_gate, a_x, q, kc, vc, ks, vs, kw, vw, gts) = _split(h @ w_in, IN0_SIZES)
    y_a = rg_lru(causal_conv(a_x, conv_w, conv_b), wa, ba, wx, bx, lam) * jax.nn.gelu(a_gate)
    y_b = nsa_mixer(q, kc, vc, ks, vs, kw, vw, jax.nn.sigmoid(gts), pe_k, w1_k, w2_k, pe_v, w1_v, w2_v)
    x = x + g_m[:, None, :] * (jnp.concatenate([y_a, y_b], axis=-1) @ w_out)
    h = modulate(x, norm_ffn, sh_f, sc_f)
    return x + g_f[:, None, :] * swiglu(h, wg, wu, wd)


def layer_c(x, c, norm_mix, norm_ffn, mod_w, mod_b, w_in, w_out, wg, wu, wd):
    sh_m, sc_m, g_m, sh_f, sc_f, g_f = adaln(c, mod_w, mod_b)
    h = modulate(x, norm_mix, sh_m, sc_m)
    q, k, v, q_idx, k_idx, w_idx = _split(h @ w_in, IN1_SIZES)
    x = x + g_m[:, None, :] * (dsa_mixer(q, k, v, q_idx, k_idx, w_idx) @ w_out)
    h = modulate(x, norm_ffn, sh_f, sc_f)
    return x + g_f[:, None, :] * swiglu(h, wg, wu, wd)


def setup_inputs(seed: int = 0) -> dict:
    key = jax.random.key(seed)
    keys = iter(jax.random.split(key, 64))

    def nrm(shape, s):
        return jax.random.normal(next(keys), shape, jnp.float32) * s

    def gain(n):
        return 1.0 + nrm((n,), 0.05)

    D = D_MODEL
    a0 = jax.random.uniform(next(keys), (LRU_WIDTH,), jnp.float32, minval=0.9, maxval=0.999)
    r0 = a0 ** (1.0 / LRU_C)
    lam = jnp.log(r0) - jnp.log1p(-r0)
    return {
        "x": nrm((BATCH, SEQ, D), 1.0),
        "c": nrm((BATCH, D), 1.0),
        "l0_norm_mix": gain(D),
        "l0_norm_ffn": gain(D),
        "l0_mod_w": nrm((D, 6 * D), 0.5 * D ** -0.5),
        "l0_mod_b": nrm((6 * D,), 0.01),
        "l0_w_in": nrm((D, IN0), D ** -0.5),
        "l0_conv_w": nrm((CONV_WIDTH, LRU_WIDTH), CONV_WIDTH ** -0.5),
        "l0_conv_b": nrm((LRU_WIDTH,), 0.01),
        "l0_lru_wa": nrm((LRU_BLOCKS, LRU_BLOCK, LRU_BLOCK), LRU_BLOCK ** -0.5),
        "l0_lru_ba": nrm((LRU_WIDTH,), 0.01),
        "l0_lru_wx": nrm((LRU_BLOCKS, LRU_BLOCK, LRU_BLOCK), LRU_BLOCK ** -0.5),
        "l0_lru_bx": nrm((LRU_WIDTH,), 0.01),
        "l0_lru_lambda": lam,
        "l0_cmp_pe_k": nrm((CMP_LEN, HEAD_DIM), 0.5),
        "l0_cmp_w1_k": nrm((CMP_LEN * HEAD_DIM, CMP_HIDDEN), (CMP_LEN * HEAD_DIM) ** -0.5),
        "l0_cmp_w2_k": nrm((CMP_HIDDEN, HEAD_DIM), CMP_HIDDEN ** -0.5),
        "l0_cmp_pe_v": nrm((CMP_LEN, HEAD_DIM), 0.5),
        "l0_cmp_w1_v": nrm((CMP_LEN * HEAD_DIM, CMP_HIDDEN), (CMP_LEN * HEAD_DIM) ** -0.5),
        "l0_cmp_w2_v": nrm((CMP_HIDDEN, HEAD_DIM), CMP_HIDDEN ** -0.5),
        "l0_w_out": nrm((MIX0, D), MIX0 ** -0.5),
        "l0_ffn_wg": nrm((D, D_FF), D ** -0.5),
        "l0_ffn_wu": nrm((D, D_FF), D ** -0.5),
        "l0_ffn_wd": nrm((D_FF, D), D_FF ** -0.5),
        "l1_norm_mix": gain(D),
        "l1_norm_ffn": gain(D),
        "l1_mod_w": nrm((D, 6 * D), 0.5 * D ** -0.5),
        "l1_mod_b": nrm((6 * D,), 0.01),
        "l1_w_in": nrm((D, IN1), D ** -0.5),
        "l1_w_out": nrm((MIX1, D), MIX1 ** -0.5),
        "l1_ffn_wg": nrm((D, D_FF), D ** -0.5),
        "l1_ffn_wu": nrm((D, D_FF), D ** -0.5),
        "l1_ffn_wd": nrm((D_FF, D), D_FF ** -0.5),
        "final_norm": gain(D),
    }


def reference(x, c,
              l0_norm_mix, l0_norm_ffn, l0_mod_w, l0_mod_b, l0_w_in, l0_conv_w, l0_conv_b,
              l0_lru_wa, l0_lru_ba, l0_lru_wx, l0_lru_bx, l0_lru_lambda,
              l0_cmp_pe_k, l0_cmp_w1_k, l0_cmp_w2_k, l0_cmp_pe_v, l0_cmp_w1_v, l0_cmp_w2_v,
              l0_w_out, l0_ffn_wg, l0_ffn_wu, l0_ffn_wd,
              l1_norm_mix, l1_norm_ffn, l1_mod_w, l1_mod_b, l1_w_in, l1_w_out,
              l1_ffn_wg, l1_ffn_wu, l1_ffn_wd,
              final_norm):
    layer_params = [
        (l0_norm_mix, l0_norm_ffn, l0_mod_w, l0_mod_b, l0_w_in, l0_conv_w, l0_conv_b,
         l0_lru_wa, l0_lru_ba, l0_lru_wx, l0_lru_bx, l0_lru_lambda,
         l0_cmp_pe_k, l0_cmp_w1_k, l0_cmp_w2_k, l0_cmp_pe_v, l0_cmp_w1_v, l0_cmp_w2_v,
         l0_w_out, l0_ffn_wg, l0_ffn_wu, l0_ffn_wd),
        (l1_norm_mix, l1_norm_ffn, l1_mod_w, l1_mod_b, l1_w_in, l1_w_out,
         l1_ffn_wg, l1_ffn_wu, l1_ffn_wd),
    ]
    for layer in range(DEPTH):
        if layer % 2 == 0:
            x = layer_ab(x, c, *layer_params[layer])
        else:
            x = layer_c(x, c, *layer_params[layer])
    return rmsnorm(x, final_norm)
```

```python
import numpy as np
import concourse.bass as bass
import concourse.mybir as mybir
from concourse.bass_utils import run_bass_kernel_spmd

F32 = mybir.dt.float32
BF16 = mybir.dt.bfloat16
AF = mybir.ActivationFunctionType
ALU = mybir.AluOpType
AX = mybir.AxisListType

NCORES = 8
SEQ = 2048
D = 1024
KC = 8
TG = 256
NTG = SEQ // TG
TPG = TG // 128
DFF = 2816
FCH = DFF // 128
IN0 = 2328
IN1 = 1864
BIG = 30000.0
NEG = -1.0e9
EPS = 1e-6
NBIS = 13

V_NMIX0, V_NFFN0, V_NMIX1, V_NFFN1, V_NFIN = 0, 8, 16, 24, 32
V_MODB0, V_MODB1 = 40, 88
V_CONVW, V_CONVB, V_BA, V_BX, V_LAM = 136, 152, 156, 160, 164
NV = 168
CB_ID, CB_IREP, CB_TRI, CB_TRILO, CB_BND, CB_WD, CB_VAUGC, CB_ONES = 0, 128, 640, 1152, 1664, 2176, 2304, 2338
NCB = 2338 + 128
CF_FM, CF_F0, CF_TRIQK, CF_ID = 0, 64, 96, 224
NCF = 352


class Sched:
    ENG = ("pe", "dve", "act", "pool", "sp")

    def __init__(self, nc):
        self.nc = nc
        self.ops = {e: [] for e in self.ENG}
        self.sem = {e: nc.alloc_semaphore("s_" + e) for e in self.ENG}
        self.cnt = {e: 0 for e in self.ENG}
        self.seen = {e: {} for e in self.ENG}
        self.state = {}
        self.dsem = {}
        self.alltoks = {}
        self.nops = 0
        self.max_ops = None
        self.cut_off = False

    def _skip(self):
        if self.cut_off or self.max_ops is None:
            return False
        self.nops += 1
        return self.nops > self.max_ops

    def _deps(self, r, w):
        toks = []
        for k in r:
            st = self.state.get(k)
            if st and st[0] is not None:
                toks.append(st[0])
        for k in w:
            st = self.state.get(k)
            if st:
                if st[0] is not None:
                    toks.append(st[0])
                toks.extend(st[1])
        return toks

    def _commit(self, tok, r, w):
        for k in r:
            st = self.state.setdefault(k, [None, []])
            st[1].append(tok)
            if len(st[1]) > 24:
                best = {}
                for t in st[1]:
                    if id(t[0]) not in best or best[id(t[0])][1] < t[1]:
                        best[id(t[0])] = t
                st[1] = list(best.values())
        for k in w:
            self.state[k] = [tok, []]
        self.alltoks[id(tok[0])] = tok

    def _waits(self, eng, toks, same_ok=False):
        best = {}
        for (s, v, e) in toks:
            if same_ok and e == eng:
                continue
            if self.seen[eng].get(id(s), 0) >= v:
                continue
            if id(s) not in best or best[id(s)][1] < v:
                best[id(s)] = (s, v)
        for (s, v) in best.values():
            self.seen[eng][id(s)] = v
        return list(best.values())

    def op(self, eng, fn, r=(), w=(), same_ok=False):
        if self._skip():
            return None
        toks = self._deps(r, w)
        waits = self._waits(eng, toks, same_ok)
        self.cnt[eng] += 1
        tok = (self.sem[eng], self.cnt[eng], eng)
        self.ops[eng].append((waits, fn, (self.sem[eng], 1)))
        self._commit(tok, r, w)
        return tok

    def dma(self, q, fn, r=(), w=(), sname=None):
        if self._skip():
            return None
        if sname not in self.dsem:
            self.dsem[sname] = [self.nc.alloc_semaphore("d_" + str(sname)), 0]
        d = self.dsem[sname]
        toks = [t for t in self._deps(r, w) if t[0] is not d[0]]
        waits = self._waits(q, toks)
        d[1] += 16
        tok = (d[0], d[1], "dma")
        self.ops[q].append((waits, fn, (d[0], 16)))
        self._commit(tok, r, w)
        return tok

    def wait_tok(self, eng, tok):
        waits = self._waits(eng, [tok])
        if waits:
            self.ops[eng].append((waits, None, None))

    def barrier(self):
        toks = list(self.alltoks.values())
        for e in self.ENG:
            waits = self._waits(e, toks, same_ok=False)
            if waits:
                self.ops[e].append((waits, None, None))
        self.state = {}

    def new_epoch(self):
        self.barrier()
        old = dict(self.sem)
        self.epoch = getattr(self, "epoch", 0) + 1
        for e in self.ENG:
            self.sem[e] = self.nc.alloc_semaphore("s%d_%s" % (self.epoch, e))
            self.cnt[e] = 0
        for s_ in old.values():
            self.alltoks.pop(id(s_), None)
        self._old_sems = getattr(self, "_old_sems", []) + list(old.values())

    def replay(self):
        nc = self.nc
        engs = {"pe": "tensor", "dve": "vector", "act": "scalar", "pool": "gpsimd", "sp": "sync"}
        with nc.Block() as block:
            for e, attr in engs.items():
                lst = self.ops[e]

                def body(eng, lst=lst):
                    for waits, fn, inc in lst:
                        for (s, v) in waits:
                            eng.wait_ge(s, v)
                        if fn is not None:
                            ins = fn(eng)
                            ins.then_inc(inc[0], inc[1])
                getattr(block, attr)(body)


class Rot:
    def __init__(self, items):
        self.items = items
        self.i = 0

    def next(self):
        it = self.items[self.i % len(self.items)]
        self.i += 1
        return it


def make_consts():
    cb = np.zeros((128, NCB), np.float32)
    cb[:, CB_ID:CB_ID + 128] = np.eye(128)
    kk = np.arange(128)[:, None]
    qq = np.arange(128)[None, :]
    for h in range(4):
        cb[:, CB_IREP + h * 128:CB_IREP + (h + 1) * 128] = BIG * np.eye(128)
        cb[:, CB_TRI + h * 128:CB_TRI + (h + 1) * 128] = np.where(kk > qq, -BIG, 0.0)
        cb[:, CB_TRILO + h * 128:CB_TRILO + (h + 1) * 128] = np.where(kk <= qq, -BIG, 0.0)
        j = np.arange(8)[:, None]
        cb[0:8, CB_BND + h * 128:CB_BND + (h + 1) * 128] = np.where(qq >= 16 * j + 15, 0.0, -BIG)
    for j in range(8):
        cb[j, CB_WD + j + 120] = 1.0
    cb[1:128, CB_VAUGC] = 1.0
    for cp in range(1, 128):
        c = cp - 1
        for jj in range(32):
            if 16 * c < 64 * jj + 64 and 16 * c + 32 > 64 * jj:
                cb[cp, CB_VAUGC + 2 + jj] = 1.0
    cb[:, CB_ONES:CB_ONES + 128] = 1.0
    cf = np.zeros((128, NCF), np.float32)
    q = np.arange(128)[:, None]
    hq = (q >= 64).astype(np.int64)
    x = np.arange(64)[None, :]
    dj = x - 32
    fm = np.zeros((128, 64), np.float32)
    fm[(dj == hq) | (dj == hq - 1)] = 1e4
    fm[dj > hq] = NEG
    cf[:, CF_FM:CF_FM + 64] = fm
    cf[:, CF_F0] = 1e4
    cf[:, CF_TRIQK:CF_TRIQK + 128] = np.where(np.arange(128)[None, :] > q, NEG, 0.0)
    cf[:, CF_ID:CF_ID + 128] = np.eye(128)
    return cb, cf


def pack_vecs(inp):
    v = np.zeros((128, NV), np.float32)

    def put(off, vec):
        vec = np.asarray(vec, np.float32).reshape(-1, 128)
        v[:, off:off + vec.shape[0]] = vec.T
    put(V_NMIX0, inp["l0_norm_mix"]); put(V_NFFN0, inp["l0_norm_ffn"])
    put(V_NMIX1, inp["l1_norm_mix"]); put(V_NFFN1, inp["l1_norm_ffn"]); put(V_NFIN, inp["final_norm"])
    put(V_MODB0, inp["l0_mod_b"]); put(V_MODB1, inp["l1_mod_b"])
    cw = np.asarray(inp["l0_conv_w"], np.float32)
    for c in range(4):
        v[:, V_CONVW + c * 4:V_CONVW + c * 4 + 4] = cw[:, c * 128:(c + 1) * 128].T
    put(V_CONVB, inp["l0_conv_b"]); put(V_BA, inp["l0_lru_ba"]); put(V_BX, inp["l0_lru_bx"]); put(V_LAM, inp["l0_lru_lambda"])
    pet = np.zeros((64, 66), np.float32)
    pet[:, 0:32] = np.asarray(inp["l0_cmp_pe_k"], np.float32).T
    pet[:, 32:64] = np.asarray(inp["l0_cmp_pe_v"], np.float32).T
    return v, pet


WEIGHTS = [("l0_mod_w", [D, 6 * D]), ("l1_mod_w", [D, 6 * D]), ("l0_w_in", [D, IN0]), ("l1_w_in", [D, IN1]),
           ("l0_w_out", [D, D]), ("l1_w_out", [D, D]),
           ("l0_ffn_wg", [D, DFF]), ("l0_ffn_wu", [D, DFF]), ("l0_ffn_wd", [DFF, D]),
           ("l1_ffn_wg", [D, DFF]), ("l1_ffn_wu", [D, DFF]), ("l1_ffn_wd", [DFF, D]),
           ("l0_lru_wa", [8, 64, 64]), ("l0_lru_wx", [8, 64, 64]),
           ("l0_cmp_w1_k", [2048, 128]), ("l0_cmp_w2_k", [128, 64]), ("l0_cmp_w1_v", [2048, 128]), ("l0_cmp_w2_v", [128, 64])]


def build_program(stop_after=None, nseq=2, ntg_run=NTG, dbg_yc=False, layers=(0, 1), max_ops=None):
    nc = bass.Bass("TRN2", target_bir_lowering=False)
    S = Sched(nc)
    S.max_ops = max_ops
    dr = {}
    for name, shp in WEIGHTS:
        dr[name] = nc.dram_tensor(name, shp, F32, kind="ExternalInput").ap()
    xT_d = nc.dram_tensor("xT", [2, 128, KC, SEQ], F32, kind="ExternalInput").ap()
    cT_d = nc.dram_tensor("cT", [128, KC, 2], F32, kind="ExternalInput").ap()
    vecs_d = nc.dram_tensor("vecs", [128, NV], F32, kind="ExternalInput").ap()
    pet_d = nc.dram_tensor("pet", [64, 66], F32, kind="ExternalInput").ap()
    cb_d = nc.dram_tensor("cb", [128, NCB], F32, kind="ExternalInput").ap()
    cf_d = nc.dram_tensor("cf", [128, NCF], F32, kind="ExternalInput").ap()
    out_d = nc.dram_tensor("outT", [2, 128, KC, SEQ], F32, kind="ExternalOutput").ap()

    def sb(name, shape, dt=F32):
        return nc.alloc_sbuf_tensor(name, list(shape), dt).ap()

    xT = sb("xT_sb", [128, KC, SEQ])
    vecs = sb("vecs_sb", [128, NV])
    cb = sb("cb_sb", [128, NCB], BF16)
    cf = sb("cf_sb", [128, NCF])
    pet = sb("pet_sb", [64, 66], BF16)
    mod = [sb("mod%d" % l, [128, 48, 2]) for l in range(2)]
    gsm = [sb("gsm%d" % l, [128, KC, 2]) for l in range(2)]
    gsf = [sb("gsf%d" % l, [128, KC, 2]) for l in range(2)]
    siluc = sb("siluc", [128, KC, 2])
    lruc = sb("lruc", [128, 24])
    c_one = sb("c_one", [128, 1]); c_eps = sb("c_eps", [128, 1])
    ps = [nc.alloc_psum_tensor("ps%d" % i, [128, 512], F32).ap() for i in range(8)]
    PK = [("ps", i) for i in range(8)]

    ident = cb[:, CB_ID:CB_ID + 128]
    irep = cb[:, CB_IREP:CB_IREP + 512]
    trineg = cb[:, CB_TRI:CB_TRI + 512]
    trilo = cb[:, CB_TRILO:CB_TRILO + 512]
    bnd = cb[0:8, CB_BND:CB_BND + 512]
    wd = cb[0:8, CB_WD:CB_WD + 128]
    onesb = cb[:, CB_ONES:CB_ONES + 128]
    identf = cf[:, CF_ID:CF_ID + 128]
    triqk = cf[:, CF_TRIQK:CF_TRIQK + 128]

    OV_BYTES = nc.sbuf_bytes_remaining - 2048
    ov = nc.alloc_sbuf_tensor("ov", [128, OV_BYTES // 2], BF16).ap()
    ovf = ov.bitcast(F32)

    class Carver:
        def __init__(self):
            self.off = 0

        def get(self, shape, dt=F32, parts=128):
            n = int(np.prod(shape[1:]))
            esz = 4 if dt == F32 else 2
            self.off = (self.off + 31) // 32 * 32
            o = self.off
            self.off += n * esz
            assert self.off <= OV_BYTES, ("overlay overflow", self.off, OV_BYTES)
            base = ovf if dt == F32 else ov
            a = base[0:shape[0], o // esz:o // esz + n]
            if len(shape) == 3:
                a = a.rearrange("p (a b) -> p a b", b=shape[2])
            elif len(shape) == 4:
                a = a.rearrange("p (a b c) -> p a b c", b=shape[2], c=shape[3])
            return a

    def mm(out, lhsT, rhs, start, stop, r, w):
        S.op("pe", lambda e: e.matmul(out, lhsT=lhsT, rhs=rhs, start=start, stop=stop), r=r, w=w, same_ok=True)

    def act(out, in_, func, r, w, bias=None, scale=None, accum=None):
        kw = {}
        if bias is not None:
            kw["bias"] = bias
        if scale is not None:
            kw["scale"] = scale
        if accum is not None:
            kw["accum_out"] = accum
        S.op("act", lambda e: e.activation(out=out, in_=in_, func=func, **kw), r=r, w=w)

    def ts(eng, out, in0, s1, s2, op0, op1, r, w, accum=None):
        kw = {}
        if accum is not None:
            kw["accum_out"] = accum
        if op1 is None:
            S.op(eng, lambda e: e.tensor_scalar(out=out, in0=in0, scalar1=s1, scalar2=None, op0=op0, **kw), r=r, w=w)
        else:
            S.op(eng, lambda e: e.tensor_scalar(out=out, in0=in0, scalar1=s1, scalar2=s2, op0=op0, op1=op1, **kw), r=r, w=w)

    def tt(eng, out, in0, in1, op, r, w):
        S.op(eng, lambda e: e.tensor_tensor(out=out, in0=in0, in1=in1, op=op), r=r, w=w)

    def stt(out, in0, scalar, in1, op0, op1, r, w):
        S.op("dve", lambda e: e.scalar_tensor_tensor(out=out, in0=in0, scalar=scalar, in1=in1, op0=op0, op1=op1), r=r, w=w)

    def cp(eng, out, in_, r, w):
        if eng == "act":
            S.op("act", lambda e: e.copy(out=out, in_=in_), r=r, w=w)
        else:
            S.op(eng, lambda e: e.tensor_copy(out=out, in_=in_), r=r, w=w)

    def memset(eng, ap, val, w):
        S.op(eng, lambda e: e.memset(ap, val), w=w)

    def recip(out, in_, r, w):
        S.op("dve", lambda e: e.reciprocal(out=out, in_=in_), r=r, w=w)

    def treduce(out, in_, op, r, w):
        S.op("dve", lambda e: e.tensor_reduce(out=out, in_=in_, axis=AX.X, op=op), r=r, w=w)

    def max8(out, in_, r, w):
        S.op("dve", lambda e: e.max(out=out, in_=in_), r=r, w=w)

    def transp(out, in_, idn, r, w):
        S.op("pe", lambda e: e.transpose(out, in_, idn), r=r, w=w, same_ok=True)

    def scan(out, d0, d1, init, r, w):
        S.op("dve", lambda e: e.tensor_tensor_scan(out=out, data0=d0, data1=d1, initial=init, op0=ALU.mult, op1=ALU.add), r=r, w=w)

    wprev = [None]

    def wdma(out, in_, w, sname):
        if wprev[0] is not None:
            S.wait_tok("pool", wprev[0])
        t_ = S.dma("pool", lambda e: e.dma_start(out=out, in_=in_), w=w, sname=sname)
        if t_ is not None:
            wprev[0] = t_

    def gelu_inplace(z, t, r_keys, zk, tk):
        tt("pool", t, z, z, ALU.mult, r=[zk], w=[tk])
        ts("pool", t, t, 0.044715, 1.0, ALU.mult, ALU.add, r=[tk], w=[tk])
        tt("pool", t, t, z, ALU.mult, r=[tk, zk], w=[tk])
        act(t, t, AF.Exp, r=[tk], w=[tk], scale=-1.5957691216)
        ts("dve", t, t, 1.0, None, ALU.add, None, r=[tk], w=[tk])
        recip(t, t, r=[tk], w=[tk])
        tt("pool", t, t, z, ALU.mult, r=[tk, zk], w=[tk])

    S.dma("sp", lambda e: e.dma_start(out=vecs, in_=vecs_d), w=["vecs"], sname="vecs")
    S.dma("sp", lambda e: e.dma_start(out=cf, in_=cf_d), w=["cf"], sname="cf")
    S.dma("sp", lambda e: e.dma_start(out=siluc, in_=cT_d), w=["siluc"], sname="siluc")
    wdma(cb, cb_d, ["cb"], "cb")
    wdma(pet, pet_d, ["pet"], "pet")
    memset("dve", c_one, 1.0, ["c_one"])
    memset("dve", c_eps, EPS, ["c_eps"])
    act(siluc, siluc, AF.Silu, r=["siluc"], w=["siluc"])
    act(lruc[:, 8:12], vecs[:, V_LAM:V_LAM + 4], AF.Exp, r=["vecs"], w=["lruc"], scale=-1.0)
    act(lruc[:, 8:12], lruc[:, 8:12], AF.Ln, r=["lruc", "c_one"], w=["lruc"], bias=c_one[:, 0:1])
    ts("dve", lruc[:, 0:4], lruc[:, 8:12], -8.0, None, ALU.mult, None, r=["lruc"], w=["lruc"])
    ts("dve", lruc[:, 4:8], lruc[:, 8:12], -16.0, None, ALU.mult, None, r=["lruc"], w=["lruc"])
    ts("dve", lruc[:, 12:16], vecs[:, V_BA:V_BA + 4], -1.0, None, ALU.mult, None, r=["vecs", "lruc"], w=["lruc"])
    ts("dve", lruc[:, 16:20], vecs[:, V_BX:V_BX + 4], -1.0, None, ALU.mult, None, r=["vecs", "lruc"], w=["lruc"])

    NG = 768
    C0 = Carver()
    stg = [C0.get([128, KC, NG]) for i in range(2)]
    gi = 0
    bankrot = Rot([0, 1, 2, 3, 4, 5, 6, 7])
    for l in range(2):
        mw = dr["l%d_mod_w" % l].rearrange("(k p) n -> p k n", p=128)
        vb = V_MODB0 if l == 0 else V_MODB1
        for j in range(6 * D // NG):
            st = stg[gi % 2]
            sk = "modstg%d" % (gi % 2)
            S.dma("sp", lambda e, st=st, j=j, mw=mw: e.dma_start(out=st, in_=mw[:, :, j * NG:(j + 1) * NG]), w=[sk], sname=sk)
            gi += 1
            for nn in range(NG // 128):
                b = bankrot.next()
                col = j * (NG // 128) + nn
                for k in range(KC):
                    mm(ps[b][:, 0:2], st[:, k, nn * 128:(nn + 1) * 128], siluc[:, k, :], k == 0, k == KC - 1,
                       r=[sk, "siluc"], w=[PK[b]])
                ts("dve", mod[l][:, col, :], ps[b][:, 0:2], vecs[:, vb + col:vb + col + 1], None, ALU.add, None,
                   r=[PK[b], "vecs"], w=["mod%d" % l])
        nm = V_NMIX0 if l == 0 else V_NMIX1
        nf = V_NFFN0 if l == 0 else V_NFFN1
        for s in range(2):
            stt(gsm[l][:, :, s], mod[l][:, 8:16, s], 1.0, vecs[:, nm:nm + 8], ALU.add, ALU.mult, r=["mod%d" % l, "vecs"], w=["gs%d" % l])
            stt(gsf[l][:, :, s], mod[l][:, 32:40, s], 1.0, vecs[:, nf:nf + 8], ALU.add, ALU.mult, r=["mod%d" % l, "vecs"], w=["gs%d" % l])

    def norm_group(tg, gs_ap, sh_ap, hT_out, hkey, tmp_sq, tmp_f, rstd, rkeys, scale_only=False, width=TG):
        c0 = tg * width
        xk = [("xT", t) for t in range(c0 // TG, (c0 + width) // TG)]
        b = bankrot.next()
        for k in range(KC):
            sq = tmp_sq[k % 2]
            act(sq, xT[:, k, c0:c0 + width], AF.Square, r=xk + rkeys, w=[("nsq", k % 2)])
            mm(ps[b][:, 0:width], onesb, sq, k == 0, k == KC - 1, r=[("nsq", k % 2), "cb"], w=[PK[b]])
        act(rstd, ps[b][:, 0:width], AF.Ln, r=[PK[b], "c_eps"], w=["rstd"], bias=c_eps[:, 0:1], scale=1.0 / D)
        act(rstd, rstd, AF.Exp, r=["rstd"], w=["rstd"], scale=-0.5)
        for k in range(KC):
            tf = tmp_f[k % 2]
            stt(tf, xT[:, k, c0:c0 + width], gs_ap[:, k:k + 1], rstd, ALU.mult, ALU.mult,
                r=xk + ["rstd"] + rkeys, w=[("ntf", k % 2)])
            if sh_ap is not None:
                act(hT_out[:, k, :], tf, AF.Identity, r=[("ntf", k % 2)] + rkeys, w=[hkey], bias=sh_ap[:, k:k + 1])
            else:
                cp("act", hT_out[:, k, :], tf, r=[("ntf", k % 2)], w=[hkey])

    dbg = {}

    for s in range(nseq):
        if s > 0:
            S.new_epoch()
        for k in range(KC):
            S.dma("sp", lambda e, k=k, s=s: e.dma_start(out=xT[:, k, :], in_=xT_d[s, :, k, :]),
                  w=[("xT", t) for t in range(NTG)], sname="xT%d" % k)

        for layer in layers:
            if stop_after == ("p0",):
                break
            S.barrier()
            C = Carver()
            nin = IN0 if layer == 0 else IN1
            WA = C.get([128, KC, nin], BF16)
            WB = C.get([128, KC, D], BF16)
            win_d = dr["l%d_w_in" % layer].rearrange("(k p) n -> p k n", p=128)
            for k in range(KC):
                wdma(WA[:, k, :], win_d[:, k, :], ["WA"], "WA")
            wout_d = dr["l%d_w_out" % layer].rearrange("(k p) n -> p k n", p=128)
            for k in range(0, KC, 4):
                wdma(WB[:, k:k + 4, :], wout_d[:, k:k + 4, :], ["WB"], "WB")
            hTg = C.get([128, KC, TG], BF16)
            nsq = [C.get([128, TG], BF16) for _ in range(2)]
            ntf = [C.get([128, TG]) for _ in range(2)]
            rstd = C.get([128, TG])
            yT = C.get([128, KC, TG], BF16)
            pT = [C.get([128, 512], BF16) for _ in range(3)]
            pTrot = Rot([0, 1, 2])
            ytok = C.get([128, 512 if layer == 0 else 1024])
            gm_ap = mod[layer][:, 16:24, s]
            shm_ap = mod[layer][:, 0:8, s]
            SB = Rot([0, 1, 2])
            AB = Rot([3, 4])
            MB = Rot([5, 6, 7])

            def outproj(tg):
                if dbg_yc == "l1_yc" and layer == 1:
                    cp("dve", xT[:, :, tg * TG:(tg + 1) * TG], yT, r=["yT", ("xT", tg)], w=[("xT", tg)])
                    return
                for ko in range(KC):
                    b = MB.next()
                    for kf in range(KC):
                        mm(ps[b][:, 0:TG], WB[:, kf, ko * 128:(ko + 1) * 128], yT[:, kf, :], kf == 0, kf == KC - 1,
                           r=["WB", "yT", "yTa", "yTb"], w=[PK[b]])
                    stt(xT[:, ko, tg * TG:(tg + 1) * TG], ps[b][:, 0:TG], gm_ap[:, ko:ko + 1], xT[:, ko, tg * TG:(tg + 1) * TG],
                        ALU.mult, ALU.add, r=[PK[b], "mod%d" % layer, ("xT", tg)], w=[("xT", tg)])

            if layer == 0:
                W1 = [C.get([64, 32, 128], BF16) for _ in range(2)]
                W2 = [C.get([128, 64], BF16) for _ in range(2)]
                BD = [C.get([128, 4, 128], BF16) for _ in range(2)]
                for i, nm_ in enumerate(("k", "v")):
                    w1d = dr["l0_cmp_w1_" + nm_].rearrange("(l d) m -> d l m", d=64)
                    for l0_ in range(0, 32, 4):
                        wdma(W1[i][:, l0_:l0_ + 4, :], w1d[:, l0_:l0_ + 4, :], ["W1%d" % i], "W1%d" % i)
                    wdma(W2[i], dr["l0_cmp_w2_" + nm_], ["W2%d" % i], "W2%d" % i)
                for i, nm_ in enumerate(("wa", "wx")):
                    memset("pool", BD[i], 0.0, ["BD%d" % i])
                    for c in range(4):
                        wdma(BD[i][0:64, c, 0:64], dr["l0_lru_" + nm_][2 * c], ["BD%d" % i], "BD%d" % i)
                        wdma(BD[i][64:128, c, 64:128], dr["l0_lru_" + nm_][2 * c + 1], ["BD%d" % i], "BD%d" % i)
                ksT = C.get([128, 2, SEQ], BF16)
                kwT = C.get([128, 2, 6 * 128], BF16)
                VsA = C.get([128, 16, 2, 66], BF16)
                VwA = C.get([128, 6, 2, 66], BF16)
                kcR = [C.get([64, 2, 16, 17], BF16) for _ in range(2)]
                KcT = C.get([128, 2, 128], BF16)
                VcT = C.get([64, 2, 128])
                VcA = C.get([128, 2, 98], BF16)
                hid = C.get([128, 16]); hidt = C.get([128, 16]); hidb = C.get([128, 16], BF16)
                pebias = C.get([128, 2])
                qT = C.get([128, 8, TG], BF16)
                sig = C.get([128, TPG, 24]); gtsraw = C.get([128, TPG, 24])
                AXb = C.get([128, 4, TG + 3])
                agf = C.get([128, TG]); gt = C.get([128, TG]); xc = C.get([128, TG]); xcb = C.get([128, TG], BF16)
                rr = C.get([128, TG]); ii = C.get([128, TG]); aa = C.get([128, TG])
                carry = C.get([128, 4])
                negE = [C.get([128, 32, 64], BF16) for _ in range(2)]
                impn = C.get([128, 4, 32]); imp = C.get([128, 32]); top8 = C.get([128, 8]); negsel = C.get([128, 32], BF16)
                rec = C.get([128, 4]); coef = C.get([128, 4]); tmpo = C.get([128, 4, 64])

                memset("dve", VsA, 0.0, [("VsA", t) for t in range(NTG)])
                memset("dve", VwA, 0.0, ["VwA"])
                for t_ in range(16):
                    memset("dve", VsA[:, t_, :, 64:65], 1.0, [("VsA", t_ // TPG)])
                for t_ in range(6):
                    memset("dve", VwA[:, t_, :, 64:65], 1.0, ["VwA"])
                memset("dve", KcT, 0.0, ["KcT"])
                memset("dve", qT, 0.0, ["qT"])
                memset("pool", ksT, 0.0, [("ksT", t) for t in range(NTG)])
                memset("pool", kwT, 0.0, [("kwT", t) for t in range(6)])
                memset("dve", VcT, 0.0, ["VcT"])
                memset("dve", AXb, 0.0, ["AXb"])
                for i in range(2):
                    memset("pool", kcR[i], 0.0, ["kcR%d" % i])
                for g in range(2):
                    cp("pool", VcA[:, g, 64:98], cb[:, CB_VAUGC:CB_VAUGC + 34], r=["cb"], w=["VcA"])
                for i in range(2):
                    b = MB.next()
                    for l in range(32):
                        mm(ps[b][:, 0:2], W1[i][:, l, :], pet[:, 32 * i + l:32 * i + l + 2],
                           l == 0, l == 31, r=["W1%d" % i, "pet"], w=[PK[b]])
                    cp("dve", pebias[:, i:i + 1], ps[b][:, 0:1], r=[PK[b]], w=["pebias"])

                for tg in range(ntg_run):
                    t0 = tg * TG
                    norm_group(tg, gsm[0][:, :, s], shm_ap, hTg, "hTg", nsq, ntf, rstd, ["gs0", "mod0"])
                    if dbg_yc == "l0_h":
                        cp("dve", xT[:, :, tg * TG:(tg + 1) * TG], hTg, r=["hTg", ("xT", tg)], w=[("xT", tg)])
                        continue
                    def lru_gen(tg=tg):
                        for c in range(4):
                            bg = MB.next()
                            for k in range(KC):
                                mm(ps[bg][:, 0:TG], WA[:, k, c * 128:(c + 1) * 128], hTg[:, k, :], k == 0, k == KC - 1, r=["WA", "hTg"], w=[PK[bg]])
                            cp("act", agf, ps[bg][:, 0:TG], r=[PK[bg]], w=["agf"])
                            yield
                            bx_ = MB.next()
                            for k in range(KC):
                                mm(ps[bx_][:, 0:TG], WA[:, k, 512 + c * 128:512 + (c + 1) * 128], hTg[:, k, :], k == 0, k == KC - 1, r=["WA", "hTg"], w=[PK[bx_]])
                            cp("act", AXb[:, c, 3:3 + TG], ps[bx_][:, 0:TG], r=[PK[bx_]], w=["AXb"])
                            yield
                            cw = V_CONVW + c * 4
                            ts("dve", xc, AXb[:, c, 0:TG], vecs[:, cw:cw + 1], vecs[:, V_CONVB + c:V_CONVB + c + 1], ALU.mult, ALU.add, r=["AXb", "vecs"], w=["xc"])
                            yield
                            for kk_ in range(1, 4):
                                stt(xc, AXb[:, c, kk_:kk_ + TG], vecs[:, cw + kk_:cw + kk_ + 1], xc, ALU.mult, ALU.add, r=["AXb", "vecs", "xc"], w=["xc"])
                                yield
                            cp("pool", AXb[:, c, 0:3], AXb[:, c, TG:TG + 3], r=["AXb"], w=["AXb"])
                            cp("act", xcb, xc, r=["xc"], w=["xcb"])
                            yield
                            br = MB.next()
                            mm(ps[br][:, 0:TG], BD[0][:, c, :], xcb, True, True, r=["BD0", "xcb"], w=[PK[br]])
                            bi = MB.next()
                            mm(ps[bi][:, 0:TG], BD[1][:, c, :], xcb, True, True, r=["BD1", "xcb"], w=[PK[bi]])
                            yield
                            act(rr, ps[br][:, 0:TG], AF.Exp, r=[PK[br], "lruc"], w=["rr"], bias=lruc[:, 12 + c:13 + c], scale=-1.0)
                            act(ii, ps[bi][:, 0:TG], AF.Exp, r=[PK[bi], "lruc"], w=["ii"], bias=lruc[:, 16 + c:17 + c], scale=-1.0)
                            yield
                            ts("dve", rr, rr, 1.0, None, ALU.add, None, r=["rr"], w=["rr"])
                            yield
                            recip(rr, rr, r=["rr"], w=["rr"])
                            yield
                            ts("dve", ii, ii, 1.0, None, ALU.add, None, r=["ii"], w=["ii"])
                            yield
                            recip(ii, ii, r=["ii"], w=["ii"])
                            yield
                            act(aa, rr, AF.Exp, r=["rr", "lruc"], w=["aa"], scale=lruc[:, c:c + 1])
                            yield
                            act(rr, rr, AF.Exp, r=["rr", "lruc"], w=["rr"], scale=lruc[:, 4 + c:5 + c])
                            yield
                            ts("dve", rr, rr, -1.0, 1.0, ALU.mult, ALU.add, r=["rr"], w=["rr"])
                            yield
                            ts("dve", rr, rr, 1e-18, None, ALU.max, None, r=["rr"], w=["rr"])
                            yield
                            act(rr, rr, AF.Ln, r=["rr"], w=["rr"])
                            yield
                            act(rr, rr, AF.Exp, r=["rr"], w=["rr"], scale=0.5)
                            yield
                            if tg == 0:
                                memset("dve", rr[:, 0:1], 1.0, ["rr"])
                            tt("dve", ii, ii, xc, ALU.mult, r=["ii", "xc"], w=["ii"])
                            yield
                            tt("dve", ii, ii, rr, ALU.mult, r=["ii", "rr"], w=["ii"])
                            yield
                            init = 0.0 if tg == 0 else carry[:, c:c + 1]
                            scan(xc, aa, ii, init, r=["aa", "ii", "carry"], w=["xc"])
                            yield
                            cp("dve", carry[:, c:c + 1], xc[:, TG - 1:TG], r=["xc"], w=["carry"])
                            tt("pool", gt, agf, agf, ALU.mult, r=["agf"], w=["gt"])
                            yield
                            ts("pool", gt, gt, 0.044715, 1.0, ALU.mult, ALU.add, r=["gt"], w=["gt"])
                            yield
                            tt("pool", gt, gt, agf, ALU.mult, r=["gt", "agf"], w=["gt"])
                            yield
                            act(gt, gt, AF.Exp, r=["gt"], w=["gt"], scale=-1.5957691216)
                            yield
                            ts("dve", gt, gt, 1.0, None, ALU.add, None, r=["gt"], w=["gt"])
                            yield
                            recip(gt, gt, r=["gt"], w=["gt"])
                            yield
                            tt("pool", gt, gt, agf, ALU.mult, r=["gt", "agf"], w=["gt"])
                            yield
                            tt("pool", yT[:, c, :], xc, gt, ALU.mult, r=["xc", "gt"], w=["yTa"])
                            yield

                    lgen = lru_gen()
                    nsteps_ = 0
                    for j_ in range(TPG):
                        ib_ = tg * TPG + j_
                        nsteps_ += 2 * (1 + len([kt for kt in range(ib_ - 4, ib_ + 1) if kt >= 0]) + ib_ + 1)
                    per_step = -(-26 * 4 // max(nsteps_ - 2, 1))

                    def pump(n):
                        for _ in range(n):
                            try:
                                next(lgen)
                            except StopIteration:
                                return
                    for h in range(8):
                        b = MB.next()
                        for k in range(KC):
                            mm(ps[b][0:64, 0:TG], WA[:, k, 1024 + h * 64:1024 + (h + 1) * 64], hTg[:, k, :], k == 0, k == KC - 1, r=["WA", "hTg"], w=[PK[b]])
                        cp("act" if h % 2 == 0 else "dve", qT[0:64, h, :], ps[b][0:64, 0:TG], r=[PK[b]], w=["qT"])
                    for which, cbase in (("kc", 1536), ("vc", 1664), ("ks", 1792), ("kw", 2048)):
                        for g in range(2):
                            b = MB.next()
                            for k in range(KC):
                                mm(ps[b][0:64, 0:TG], WA[:, k, cbase + g * 64:cbase + (g + 1) * 64], hTg[:, k, :], k == 0, k == KC - 1, r=["WA", "hTg"], w=[PK[b]])
                            if which == "ks":
                                cp("act", ksT[0:64, g, t0:t0 + TG], ps[b][0:64, 0:TG], r=[PK[b]], w=[("ksT", tg)])
                            elif which == "kw":
                                for j in range(TPG):
                                    kt = tg * TPG + j
                                    sl = kt % 6
                                    cp("dve", kwT[0:64, g, sl * 128:(sl + 1) * 128], ps[b][0:64, j * 128:(j + 1) * 128], r=[PK[b]], w=[("kwT", sl)])
                            else:
                                i = 0 if which == "kc" else 1
                                if g == 0:
                                    if tg > 0:
                                        cp("pool", kcR[i][:, :, :, 0:1], kcR[i][:, :, :, 16:17], r=["kcR%d" % i], w=["kcR%d" % i])
                                cp("act", kcR[i][:, g, :, 1:17], ps[b][0:64, 0:TG].rearrange("p (b r) -> p r b", r=16), r=[PK[b]], w=["kcR%d" % i])
                    for j in range(TPG):
                        kt = tg * TPG + j
                        b = MB.next()
                        for k in range(KC):
                            mm(ps[b][:, 0:128], hTg[:, k, j * 128:(j + 1) * 128], WA[:, k, 1920:2048], k == 0, k == KC - 1, r=["WA", "hTg"], w=[PK[b]])
                        cp("act", VsA[:, kt, :, 0:64], ps[b][:, 0:128].rearrange("p (g d) -> p g d", d=64), r=[PK[b]], w=[("VsA", tg)])
                        b = MB.next()
                        for k in range(KC):
                            mm(ps[b][:, 0:152], hTg[:, k, j * 128:(j + 1) * 128], WA[:, k, 2176:2328], k == 0, k == KC - 1, r=["WA", "hTg"], w=[PK[b]])
                        cp("dve", VwA[:, kt % 6, :, 0:64], ps[b][:, 0:128].rearrange("p (g d) -> p g d", d=64), r=[PK[b]], w=[("VwA", kt % 6)])
                        cp("dve", gtsraw[:, j, :], ps[b][:, 128:152], r=[PK[b]], w=["gtsraw"])
                    act(sig, gtsraw, AF.Exp, r=["gtsraw"], w=["sig"], scale=-1.0)
                    ts("dve", sig, sig, 1.0, None, ALU.add, None, r=["sig"], w=["sig"])
                    recip(sig, sig, r=["sig"], w=["sig"])
                    for i in range(2):
                        for g in range(2):
                            b = MB.next()
                            for l in range(32):
                                mm(ps[b][:, 0:16], W1[i][:, l, :], kcR[i][:, g, l % 16, (l // 16):(l // 16) + 16], l == 0, l == 31,
                                   r=["W1%d" % i, "kcR%d" % i], w=[PK[b]])
                            act(hid, ps[b][:, 0:16], AF.Identity, r=[PK[b], "pebias"], w=["hid"], bias=pebias[:, i:i + 1])
                            gelu_inplace(hid, hidt, None, "hid", "hidt")
                            cp("pool", hidb, hidt, r=["hidt"], w=["hidb"])
                            b2 = MB.next()
                            mm(ps[b2][0:64, 0:16], W2[i], hidb, True, True, r=["W2%d" % i, "hidb"], w=[PK[b2]])
                            if i == 0:
                                cp("act", KcT[0:64, g, 16 * tg:16 * tg + 16], ps[b2][0:64, 0:16], r=[PK[b2]], w=["KcT"])
                            else:
                                cp("act", VcT[:, g, 16 * tg:16 * tg + 16], ps[b2][0:64, 0:16], r=[PK[b2]], w=["VcT"])
                    if tg == 0:
                        memset("dve", KcT[0:64, :, 0:1], 0.0, ["KcT"])
                        memset("dve", VcT[:, :, 0:1], 0.0, ["VcT"])
                    for g in range(2):
                        b = MB.next()
                        transp(ps[b][:, 0:64], VcT[:, g, :], identf[0:64, 0:64], r=["VcT", "cf"], w=[PK[b]])
                        cp("act", VcA[:, g, 0:64], ps[b][:, 0:64], r=[PK[b]], w=["VcA"])

                    for j in range(TPG):
                        ib = tg * TPG + j
                        qc = slice(j * 128, (j + 1) * 128)
                        for g in range(2):
                            qrhs = qT[:, 4 * g:4 * g + 4, qc]
                            M = 8 * (ib + 1)
                            sbk = SB.next()
                            mm(ps[sbk][0:M, :], KcT[:, g, 0:M], qrhs, True, False, r=["KcT", "qT"], w=[PK[sbk]])
                            s0 = 120 - 8 * ib
                            mm(ps[sbk][0:M, :], wd[:, s0:s0 + M], bnd, False, True, r=["cb"], w=[PK[sbk]])
                            pi = pTrot.next()
                            act(pT[pi][0:M, :], ps[sbk][0:M, :], AF.Exp, r=[PK[sbk]], w=[("pT", pi)], scale=0.125)
                            ab = AB.next()
                            for h in range(4):
                                mm(ps[ab][:, h * 98:(h + 1) * 98], pT[pi][0:M, h * 128:(h + 1) * 128], VcA[0:M, g, :], h == 0, h == 3,
                                   r=[("pT", pi), "VcA"], w=[PK[ab]])
                            pump(per_step)
                            accv = ps[ab][:, 0:392].rearrange("p (h c) -> p h c", c=98)
                            ts("dve", rec, accv[:, :, 64], 1e-30, None, ALU.max, None, r=[PK[ab]], w=["rec"])
                            recip(rec, rec, r=["rec"], w=["rec"])
                            tt("dve", impn, accv[:, :, 66:98], rec.unsqueeze(2).to_broadcast([128, 4, 32]), ALU.mult, r=[PK[ab], "rec"], w=["impn"])
                            treduce(imp, impn.rearrange("p h j -> p j h"), ALU.add, r=["impn"], w=["imp"])
                            tt("dve", imp, imp, cf[:, CF_FM + 32 - 2 * ib:CF_FM + 64 - 2 * ib], ALU.add, r=["imp", "cf"], w=["imp"])
                            tt("dve", imp, imp, cf[:, CF_F0:CF_F0 + 32], ALU.add, r=["imp", "cf"], w=["imp"])
                            max8(top8, imp, r=["imp"], w=["top8"])
                            ts("dve", negsel, imp, top8[:, 7:8], 1.0, ALU.is_ge, ALU.subtract, r=["imp", "top8"], w=["negsel"])
                            cp("pool", negE[g], negsel.unsqueeze(2).to_broadcast([128, 32, 64]), r=["negsel"], w=[("negE", g)])
                            tt("dve", coef, rec, sig[:, j, 12 * g + 0:12 * g + 12:3], ALU.mult, r=["rec", "sig"], w=["coef"])
                            yv = ytok[:, g * 256:(g + 1) * 256].rearrange("p (h d) -> p h d", d=64)
                            tt("dve", yv, accv[:, :, 0:64], coef.unsqueeze(2).to_broadcast([128, 4, 64]), ALU.mult, r=[PK[ab], "coef"], w=["ytok"])
                            for br_, gi_ in (("win", 2), ("slc", 1)):
                                kts = [kt for kt in range(ib - 4, ib + 1) if kt >= 0] if br_ == "win" else list(range(ib + 1))
                                ab = AB.next()
                                for n_, kt in enumerate(kts):
                                    sbk = SB.next()
                                    if br_ == "win":
                                        klhs = kwT[:, g, (kt % 6) * 128:(kt % 6 + 1) * 128]
                                        kkey = ("kwT", kt % 6)
                                        vrhs = VwA[:, kt % 6, g, :]
                                        vkey = ("VwA", kt % 6)
                                        extra = []
                                        if kt == ib:
                                            extra.append((ident, trineg, ["cb"]))
                                        if kt == ib - 4:
                                            extra.append((ident, trilo, ["cb"]))
                                    else:
                                        klhs = ksT[:, g, kt * 128:(kt + 1) * 128]
                                        kkey = ("ksT", kt // TPG)
                                        vrhs = VsA[:, kt, g, :]
                                        vkey = ("VsA", kt // TPG)
                                        extra = [(negE[g][:, 2 * kt:2 * kt + 2, :].rearrange("p a b -> p (a b)"), irep, [("negE", g), "cb"])]
                                        if kt == ib:
                                            extra.append((ident, trineg, ["cb"]))
                                    mm(ps[sbk][:, :], klhs, qrhs, True, len(extra) == 0, r=[kkey, "qT"], w=[PK[sbk]])
                                    for xi, (l_, r_, ks_) in enumerate(extra):
                                        mm(ps[sbk][:, :], l_, r_, False, xi == len(extra) - 1, r=ks_, w=[PK[sbk]])
                                    pi = pTrot.next()
                                    act(pT[pi], ps[sbk], AF.Exp, r=[PK[sbk]], w=[("pT", pi)], scale=0.125)
                                    for h in range(4):
                                        mm(ps[ab][:, h * 66:(h + 1) * 66], pT[pi][:, h * 128:(h + 1) * 128], vrhs,
                                           n_ == 0 and h == 0, n_ == len(kts) - 1 and h == 3, r=[("pT", pi), vkey], w=[PK[ab]])
                                    pump(per_step)
                                accw = ps[ab][:, 0:264].rearrange("p (h c) -> p h c", c=66)
                                ts("dve", rec, accw[:, :, 64], 1e-30, None, ALU.max, None, r=[PK[ab]], w=["rec"])
                                recip(rec, rec, r=["rec"], w=["rec"])
                                tt("dve", coef, rec, sig[:, j, 12 * g + gi_:12 * g + 12:3], ALU.mult, r=["rec", "sig"], w=["coef"])
                                tt("dve", tmpo, accw[:, :, 0:64], coef.unsqueeze(2).to_broadcast([128, 4, 64]), ALU.mult, r=[PK[ab], "coef"], w=["tmpo"])
                                tt("pool", yv, yv, tmpo, ALU.add, r=["ytok", "tmpo"], w=["ytok"])
                        b = MB.next()
                        for c4 in range(4):
                            transp(ps[b][:, c4 * 128:(c4 + 1) * 128], ytok[:, c4 * 128:(c4 + 1) * 128], identf, r=["ytok", "cf"], w=[PK[b]])
                        cp("act", yT[:, 4:8, qc], ps[b].rearrange("p (c q) -> p c q", q=128), r=[PK[b]], w=["yTb"])
                    pump(10 ** 6)
                    outproj(tg)
            else:
                kT = C.get([128, 2, SEQ], BF16)
                kiT = C.get([128, SEQ], BF16)
                VA = C.get([128, 16, 2, 66], BF16)
                qTs = [C.get([128, 16, TG], BF16) for _ in range(2)]
                qiTs = [C.get([128, 8, TG], BF16) for _ in range(2)]
                widxs = [C.get([128, TPG, 8]) for _ in range(2)]
                acc = C.get([128, SEQ])
                rl = [C.get([128, 512]) for _ in range(2)]
                negms = [C.get([128, SEQ], BF16) for _ in range(2)]
                lo = C.get([128, 1]); w0 = C.get([128, 1]); mid = C.get([128, 1]); cnt = C.get([128, 1]); pw = C.get([128, 1])
                junk = C.get([128, SEQ], BF16)
                rec8 = C.get([128, 8])
                memset("dve", VA, 0.0, [("VA", t) for t in range(NTG)])
                for t_ in range(16):
                    memset("dve", VA[:, t_, :, 64:65], 1.0, [("VA", t_ // TPG)])
                memset("pool", kT, 0.0, [("kT", t) for t in range(NTG)])
                memset("pool", kiT, 0.0, [("kiT", t) for t in range(NTG)])
                for i_ in range(2):
                    memset("dve", qTs[i_], 0.0, [("qT", i_)])
                    memset("pool", qiTs[i_], 0.0, [("qiT", i_)])

                def proj1(tg):
                    t0 = tg * TG
                    qT = qTs[tg % 2]; qiT = qiTs[tg % 2]; widx = widxs[tg % 2]
                    qk = ("qT", tg % 2); qik = ("qiT", tg % 2); wk = ("widx", tg % 2)
                    norm_group(tg, gsm[1][:, :, s], shm_ap, hTg, "hTg", nsq, ntf, rstd, ["gs1", "mod1"])
                    for h in range(8):
                        b = MB.next()
                        for k in range(KC):
                            mm(ps[b][0:64, 0:TG], WA[:, k, 1280 + h * 64:1280 + (h + 1) * 64], hTg[:, k, :], k == 0, k == KC - 1, r=["WA", "hTg"], w=[PK[b]])
                        cp("act" if h % 2 == 0 else "dve", qiT[0:64, h, :], ps[b][0:64, 0:TG], r=[PK[b]], w=[qik])
                    b = MB.next()
                    for k in range(KC):
                        mm(ps[b][0:64, 0:TG], WA[:, k, 1792:1856], hTg[:, k, :], k == 0, k == KC - 1, r=["WA", "hTg"], w=[PK[b]])
                    cp("act", kiT[0:64, t0:t0 + TG], ps[b][0:64, 0:TG], r=[PK[b]], w=[("kiT", tg)])
                    for j in range(TPG):
                        b = MB.next()
                        for k in range(KC):
                            mm(ps[b][:, 0:8], hTg[:, k, j * 128:(j + 1) * 128], WA[:, k, 1856:1864], k == 0, k == KC - 1, r=["WA", "hTg"], w=[PK[b]])
                        cp("dve", widx[:, j, :], ps[b][:, 0:8], r=[PK[b]], w=[wk])
                    for h in range(16):
                        b = MB.next()
                        for k in range(KC):
                            mm(ps[b][0:64, 0:TG], WA[:, k, h * 64:(h + 1) * 64], hTg[:, k, :], k == 0, k == KC - 1, r=["WA", "hTg"], w=[PK[b]])
                        cp("act" if h % 2 == 0 else "dve", qT[0:64, h, :], ps[b][0:64, 0:TG], r=[PK[b]], w=[qk])
                    for g in range(2):
                        b = MB.next()
                        for k in range(KC):
                            mm(ps[b][0:64, 0:TG], WA[:, k, 1024 + g * 64:1024 + (g + 1) * 64], hTg[:, k, :], k == 0, k == KC - 1, r=["WA", "hTg"], w=[PK[b]])
                        cp("act", kT[0:64, g, t0:t0 + TG], ps[b][0:64, 0:TG], r=[PK[b]], w=[("kT", tg)])
                    for j in range(TPG):
                        kt = tg * TPG + j
                        b = MB.next()
                        for k in range(KC):
                            mm(ps[b][:, 0:128], hTg[:, k, j * 128:(j + 1) * 128], WA[:, k, 1152:1280], k == 0, k == KC - 1, r=["WA", "hTg"], w=[PK[b]])
                        cp("act", VA[:, kt, :, 0:64], ps[b][:, 0:128].rearrange("p (g d) -> p g d", d=64), r=[PK[b]], w=[("VA", tg)])

                def stageA(ib):
                    tg = ib // TPG; j = ib % TPG
                    qiT = qiTs[tg % 2]; widx = widxs[tg % 2]; negm = negms[ib % 2]
                    qik = ("qiT", tg % 2); wk = ("widx", tg % 2); nk_ = ("negm", ib % 2)
                    qc = slice(j * 128, (j + 1) * 128)
                    nk = (ib + 1) * 128
                    for c0 in range(0, nk, 512):
                        wdt = min(512, nk - c0)
                        for h in range(8):
                            b = MB.next()
                            mm(ps[b][:, 0:wdt], qiT[:, h, qc], kiT[:, c0:c0 + wdt], True, True,
                               r=[qik] + [("kiT", t) for t in range(c0 // TG, (c0 + wdt - 1) // TG + 1)], w=[PK[b]])
                            ri = h % 2
                            act(rl[ri][:, 0:wdt], ps[b][:, 0:wdt], AF.Relu, r=[PK[b]], w=[("rl", ri)], scale=0.125)
                            if h == 0:
                                ts("dve", acc[:, c0:c0 + wdt], rl[ri][:, 0:wdt], widx[:, j, 0:1], None, ALU.mult, None, r=[("rl", ri), wk], w=["acc"])
                            else:
                                stt(acc[:, c0:c0 + wdt], rl[ri][:, 0:wdt], widx[:, j, h:h + 1], acc[:, c0:c0 + wdt], ALU.mult, ALU.add,
                                    r=[("rl", ri), wk, "acc"], w=["acc"])
                    tt("dve", acc[:, ib * 128:nk], acc[:, ib * 128:nk], triqk, ALU.add, r=["acc", "cf"], w=["acc"])
                    if ib >= 2:
                        treduce(lo, acc[:, 0:ib * 128], ALU.min, r=["acc"], w=["lo"])
                        treduce(w0, acc[:, 0:nk], ALU.max, r=["acc"], w=["w0"])
                        tt("dve", w0, w0, lo, ALU.subtract, r=["w0", "lo"], w=["w0"])
                        for it in range(1, NBIS + 1):
                            f = 2.0 ** (-it)
                            stt(mid, w0, f, lo, ALU.mult, ALU.add, r=["w0", "lo"], w=["mid"])
                            ts("dve", junk[:, 0:nk], acc[:, 0:nk], mid[:, 0:1], 0.0, ALU.is_ge, ALU.add, r=["acc", "mid"], w=["junk", "cnt"], accum=cnt)
                            ts("dve", pw, cnt, 255.5, f, ALU.is_ge, ALU.mult, r=["cnt"], w=["pw"])
                            stt(lo, pw, w0[:, 0:1], lo, ALU.mult, ALU.add, r=["pw", "w0", "lo"], w=["lo"])
                        ts("dve", negm[:, 0:nk], acc[:, 0:nk], lo[:, 0:1], 1.0, ALU.is_ge, ALU.subtract, r=["acc", "lo"], w=[nk_])
                    else:
                        ts("dve", negm[:, 0:nk], acc[:, 0:nk], -1.0e8, 1.0, ALU.is_ge, ALU.subtract, r=["acc"], w=[nk_])

                def stageB(ib):
                    tg = ib // TPG; j = ib % TPG
                    qT = qTs[tg % 2]; negm = negms[ib % 2]
                    qk = ("qT", tg % 2); nk_ = ("negm", ib % 2)
                    qc = slice(j * 128, (j + 1) * 128)
                    for g in range(2):
                        abs_ = [AB.next(), AB.next()]
                        for kt in range(ib + 1):
                            for half in range(2):
                                sbk = SB.next()
                                mm(ps[sbk], kT[:, g, kt * 128:(kt + 1) * 128], qT[:, 8 * g + 4 * half:8 * g + 4 * half + 4, qc], True, False,
                                   r=[("kT", kt // TPG), qk], w=[PK[sbk]])
                                mm(ps[sbk], negm[:, kt * 128:(kt + 1) * 128], irep, False, True, r=[nk_, "cb"], w=[PK[sbk]])
                                pi = pTrot.next()
                                act(pT[pi], ps[sbk], AF.Exp, r=[PK[sbk]], w=[("pT", pi)], scale=0.125)
                                ab = abs_[half]
                                for h in range(4):
                                    mm(ps[ab][:, h * 66:(h + 1) * 66], pT[pi][:, h * 128:(h + 1) * 128], VA[:, kt, g, :],
                                       kt == 0 and h == 0, kt == ib and h == 3, r=[("pT", pi), ("VA", kt // TPG)], w=[PK[ab]])
                        for half in range(2):
                            ab = abs_[half]
                            accw = ps[ab][:, 0:264].rearrange("p (h c) -> p h c", c=66)
                            recip(rec8[:, 0:4], accw[:, :, 64], r=[PK[ab]], w=["rec8"])
                            yv = ytok[:, (8 * g + 4 * half) * 64:(8 * g + 4 * half + 4) * 64].rearrange("p (h d) -> p h d", d=64)
                            tt("dve", yv, accw[:, :, 0:64], rec8[:, 0:4].unsqueeze(2).to_broadcast([128, 4, 64]), ALU.mult, r=[PK[ab], "rec8"], w=["ytok"])
                    for half in range(2):
                        b = MB.next()
                        for c4 in range(4):
                            cc = half * 4 + c4
                            transp(ps[b][:, c4 * 128:(c4 + 1) * 128], ytok[:, cc * 128:(cc + 1) * 128], identf, r=["ytok", "cf"], w=[PK[b]])
                        cp("act", yT[:, half * 4:half * 4 + 4, qc], ps[b].rearrange("p (c q) -> p c q", q=128), r=[PK[b]], w=["yT"])

                nq = ntg_run * TPG
                proj1(0)
                stageA(0)
                for ib in range(nq):
                    nx = ib + 1
                    if nx < nq:
                        if nx % TPG == 0:
                            proj1(nx // TPG)
                        stageA(nx)
                    stageB(ib)
                    if ib % TPG == TPG - 1:
                        outproj(ib // TPG)

            if stop_after == ("mix", layer):
                break
            S.barrier()
            C = Carver()
            hT = C.get([128, KC, SEQ], BF16)
            nsq = [C.get([128, 512], BF16) for _ in range(2)]
            ntf = [C.get([128, 512]) for _ in range(2)]
            rstd = C.get([128, 512])
            SL = [2, 4, 5, 5, 6]
            WG = [C.get([128, KC, 768], BF16) for _ in range(2)]
            WU = [C.get([128, KC, 768], BF16) for _ in range(2)]
            WDn = [C.get([128, 6, D], BF16) for _ in range(2)]
            actb = [C.get([128, 6, 512], BF16) for _ in range(2)]
            sil = [C.get([128, 512]) for _ in range(2)]
            gf_ap = mod[layer][:, 40:48, s]
            shf_ap = mod[layer][:, 24:32, s]
            wg_d = dr["l%d_ffn_wg" % layer].rearrange("(k p) n -> p k n", p=128)
            wu_d = dr["l%d_ffn_wu" % layer].rearrange("(k p) n -> p k n", p=128)
            wd_d = dr["l%d_ffn_wd" % layer].rearrange("(f p) n -> p f n", p=128)

            def load_slice(si):
                bi = si % 2
                f0 = sum(SL[:si]); nf_ = SL[si]
                for k in range(0, KC, 4):
                    wdma(WG[bi][:, k:k + 4, 0:nf_ * 128], wg_d[:, k:k + 4, f0 * 128:(f0 + nf_) * 128], [("WG", bi)], "WG%d" % bi)
                    wdma(WU[bi][:, k:k + 4, 0:nf_ * 128], wu_d[:, k:k + 4, f0 * 128:(f0 + nf_) * 128], [("WU", bi)], "WU%d" % bi)
                h_ = max(nf_ // 2, 1)
                wdma(WDn[bi][:, 0:h_, :], wd_d[:, f0:f0 + h_, :], [("WD", bi)], "WD%d" % bi)
                wdma(WDn[bi][:, h_:nf_, :], wd_d[:, f0 + h_:f0 + nf_, :], [("WD", bi)], "WD%d" % bi)

            load_slice(0)
            load_slice(1)
            for t4 in range(SEQ // 512):
                hv = hT[:, :, t4 * 512:(t4 + 1) * 512]
                norm_group(t4, gsf[layer][:, :, s], shf_ap, hv, ("hT", t4), nsq, ntf, rstd, ["gs%d" % layer, "mod%d" % layer], width=512)
            GB = Rot([0, 1, 2, 3])
            DB = Rot([4, 5, 6, 7])
            for si in range(len(SL)):
                bi = si % 2
                nf_ = SL[si]
                for t4 in range(SEQ // 512):
                    cols = slice(t4 * 512, (t4 + 1) * 512)
                    ai = (si * 4 + t4) % 2
                    for f in range(nf_):
                        bg = GB.next(); bu = GB.next()
                        for k in range(KC):
                            mm(ps[bg], WG[bi][:, k, f * 128:(f + 1) * 128], hT[:, k, cols], k == 0, k == KC - 1, r=[("WG", bi), ("hT", t4)], w=[PK[bg]])
                        for k in range(KC):
                            mm(ps[bu], WU[bi][:, k, f * 128:(f + 1) * 128], hT[:, k, cols], k == 0, k == KC - 1, r=[("WU", bi), ("hT", t4)], w=[PK[bu]])
                        sl_ = sil[f % 2]
                        act(sl_, ps[bg], AF.Silu, r=[PK[bg]], w=[("sil", f % 2)])
                        tt("dve", actb[ai][:, f, :], sl_, ps[bu], ALU.mult, r=[("sil", f % 2), PK[bu]], w=[("actb", ai)])
                    for ko in range(KC):
                        b = DB.next()
                        for f in range(nf_):
                            mm(ps[b], WDn[bi][:, f, ko * 128:(ko + 1) * 128], actb[ai][:, f, :], f == 0, f == nf_ - 1, r=[("WD", bi), ("actb", ai)], w=[PK[b]])
                        tgs = [("xT", t) for t in range(t4 * 512 // TG, (t4 + 1) * 512 // TG)]
                        stt(xT[:, ko, cols], ps[b], gf_ap[:, ko:ko + 1], xT[:, ko, cols], ALU.mult, ALU.add, r=[PK[b], "mod%d" % layer] + tgs, w=tgs)
                if si + 2 < len(SL):
                    load_slice(si + 2)
            if stop_after == ("ffn", layer):
                break

        S.cut_off = True
        S.barrier()
        C = Carver()
        nsq = [C.get([128, 512], BF16) for _ in range(2)]
        ntf = [C.get([128, 512]) for _ in range(2)]
        rstd = C.get([128, 512])
        ob = [C.get([128, KC, 512]) for _ in range(2)]
        for t4 in range(SEQ // 512):
            o = ob[t4 % 2]
            if stop_after is None:
                c0 = t4 * 512
                b = bankrot.next()
                for k in range(KC):
                    sq = nsq[k % 2]
                    act(sq, xT[:, k, c0:c0 + 512], AF.Square, r=[("xT", c0 // TG), ("xT", c0 // TG + 1)], w=[("nsq", k % 2)])
                    mm(ps[b], onesb, sq, k == 0, k == KC - 1, r=[("nsq", k % 2), "cb"], w=[PK[b]])
                act(rstd, ps[b], AF.Ln, r=[PK[b], "c_eps"], w=["rstd"], bias=c_eps[:, 0:1], scale=1.0 / D)
                act(rstd, rstd, AF.Exp, r=["rstd"], w=["rstd"], scale=-0.5)
                for k in range(KC):
                    stt(o[:, k, :], xT[:, k, c0:c0 + 512], vecs[:, V_NFIN + k:V_NFIN + k + 1], rstd, ALU.mult, ALU.mult,
                        r=[("xT", c0 // TG), ("xT", c0 // TG + 1), "rstd", "vecs"], w=[("ob", t4 % 2)])
            else:
                cp("dve", o, xT[:, :, t4 * 512:(t4 + 1) * 512], r=[("xT", t) for t in range(NTG)], w=[("ob", t4 % 2)])
            for k in range(KC):
                S.dma("sp", lambda e, o=o, k=k, s=s, t4=t4: e.dma_start(out=out_d[s, :, k, t4 * 512:(t4 + 1) * 512], in_=o[:, k, :]),
                      r=[("ob", t4 % 2)], sname="ob%d" % (t4 % 2))
    for nm_ in ("ob0", "ob1"):
        d = S.dsem[nm_]
        S.wait_tok("sp", (d[0], d[1], "dma"))
    S.replay()
    return nc


_CACHE = {}


def _prep_inputs(inputs):
    vecs, pet = pack_vecs(inputs)
    cbv, cfv = make_consts()
    x = np.asarray(inputs["x"], np.float32)
    c = np.asarray(inputs["c"], np.float32)
    shared = {name: np.ascontiguousarray(np.asarray(inputs[name], np.float32)) for name, _ in WEIGHTS}
    shared.update({"vecs": vecs, "pet": pet, "cb": cbv, "cf": cfv})
    in_maps = []
    for i in range(NCORES):
        xs = x[2 * i:2 * i + 2]
        xT = np.ascontiguousarray(xs.reshape(2, SEQ, KC, 128).transpose(0, 3, 2, 1))
        cs = c[2 * i:2 * i + 2]
        cT = np.ascontiguousarray(cs.reshape(2, KC, 128).transpose(2, 1, 0))
        m = dict(shared)
        m["xT"] = xT
        m["cT"] = cT
        in_maps.append(m)
    return in_maps


def kernel(**inputs):
    if "nc" not in _CACHE:
        _CACHE["nc"] = build_program()
    nc = _CACHE["nc"]
    in_maps = _prep_inputs(inputs)
    res = run_bass_kernel_spmd(nc, in_maps, core_ids=list(range(NCORES)))
    out = np.empty((16, SEQ, D), np.float32)
    for i in range(NCORES):
        oT = res.results[i]["outT"]
        out[2 * i:2 * i + 2] = oT.transpose(0, 3, 2, 1).reshape(2, SEQ, D)
    return out
```

```python
import numpy as np
import concourse.bass as bass
import concourse.mybir as mybir
from concourse.bass_utils import run_bass_kernel_spmd

F32 = mybir.dt.float32
BF16 = mybir.dt.bfloat16
AF = mybir.ActivationFunctionType
ALU = mybir.AluOpType
AX = mybir.AxisListType

NCORES = 8
SEQ = 2048
D = 1024
KC = 8
TG = 256
NTG = SEQ // TG
TPG = TG // 128
DFF = 2816
FCH = DFF // 128
IN0 = 2328
IN1 = 1864
BIG = 30000.0
NEG = -1.0e9
EPS = 1e-6
NBIS = 13

V_NMIX0, V_NFFN0, V_NMIX1, V_NFFN1, V_NFIN = 0, 8, 16, 24, 32
V_MODB0, V_MODB1 = 40, 88
V_CONVW, V_CONVB, V_BA, V_BX, V_LAM = 136, 152, 156, 160, 164
NV = 168
CB_ID, CB_IREP, CB_TRI, CB_TRILO, CB_BND, CB_WD, CB_VAUGC, CB_ONES = 0, 128, 640, 1152, 1664, 2176, 2304, 2338
NCB = 2338 + 128
CF_FM, CF_F0, CF_TRIQK, CF_ID = 0, 64, 96, 224
NCF = 352


class Sched:
    ENG = ("pe", "dve", "act", "pool", "sp")

    def __init__(self, nc):
        self.nc = nc
        self.ops = {e: [] for e in self.ENG}
        self.sem = {e: nc.alloc_semaphore("s_" + e) for e in self.ENG}
        self.cnt = {e: 0 for e in self.ENG}
        self.seen = {e: {} for e in self.ENG}
        self.state = {}
        self.dsem = {}
        self.alltoks = {}
        self.nops = 0
        self.max_ops = None
        self.cut_off = False

    def _skip(self):
        if self.cut_off or self.max_ops is None:
            return False
        self.nops += 1
        return self.nops > self.max_ops

    def _deps(self, r, w):
        toks = []
        for k in r:
            st = self.state.get(k)
            if st and st[0] is not None:
                toks.append(st[0])
        for k in w:
            st = self.state.get(k)
            if st:
                if st[0] is not None:
                    toks.append(st[0])
                toks.extend(st[1])
        return toks

    def _commit(self, tok, r, w):
        for k in r:
            st = self.state.setdefault(k, [None, []])
            st[1].append(tok)
            if len(st[1]) > 24:
                best = {}
                for t in st[1]:
                    if id(t[0]) not in best or best[id(t[0])][1] < t[1]:
                        best[id(t[0])] = t
                st[1] = list(best.values())
        for k in w:
            self.state[k] = [tok, []]
        self.alltoks[id(tok[0])] = tok

    def _waits(self, eng, toks, same_ok=False):
        best = {}
        for (s, v, e) in toks:
            if same_ok and e == eng:
                continue
            if self.seen[eng].get(id(s), 0) >= v:
                continue
            if id(s) not in best or best[id(s)][1] < v:
                best[id(s)] = (s, v)
        for (s, v) in best.values():
            self.seen[eng][id(s)] = v
        return list(best.values())

    def op(self, eng, fn, r=(), w=(), same_ok=False):
        if self._skip():
            return None
        toks = self._deps(r, w)
        waits = self._waits(eng, toks, same_ok)
        self.cnt[eng] += 1
        tok = (self.sem[eng], self.cnt[eng], eng)
        self.ops[eng].append((waits, fn, (self.sem[eng], 1)))
        self._commit(tok, r, w)
        return tok

    def dma(self, q, fn, r=(), w=(), sname=None):
        if self._skip():
            return None
        if sname not in self.dsem:
            self.dsem[sname] = [self.nc.alloc_semaphore("d_" + str(sname)), 0]
        d = self.dsem[sname]
        toks = [t for t in self._deps(r, w) if t[0] is not d[0]]
        waits = self._waits(q, toks)
        d[1] += 16
        tok = (d[0], d[1], "dma")
        self.ops[q].append((waits, fn, (d[0], 16)))
        self._commit(tok, r, w)
        return tok

    def wait_tok(self, eng, tok):
        waits = self._waits(eng, [tok])
        if waits:
            self.ops[eng].append((waits, None, None))

    def barrier(self):
        toks = list(self.alltoks.values())
        for e in self.ENG:
            waits = self._waits(e, toks, same_ok=False)
            if waits:
                self.ops[e].append((waits, None, None))
        self.state = {}

    def new_epoch(self):
        self.barrier()
        old = dict(self.sem)
        self.epoch = getattr(self, "epoch", 0) + 1
        for e in self.ENG:
            self.sem[e] = self.nc.alloc_semaphore("s%d_%s" % (self.epoch, e))
            self.cnt[e] = 0
        for s_ in old.values():
            self.alltoks.pop(id(s_), None)
        self._old_sems = getattr(self, "_old_sems", []) + list(old.values())

    def replay(self):
        nc = self.nc
        engs = {"pe": "tensor", "dve": "vector", "act": "scalar", "pool": "gpsimd", "sp": "sync"}
        with nc.Block() as block:
            for e, attr in engs.items():
                lst = self.ops[e]

                def body(eng, lst=lst):
                    for waits, fn, inc in lst:
                        for (s, v) in waits:
                            eng.wait_ge(s, v)
                        if fn is not None:
                            ins = fn(eng)
                            ins.then_inc(inc[0], inc[1])
                getattr(block, attr)(body)


class Rot:
    def __init__(self, items):
        self.items = items
        self.i = 0

    def next(self):
        it = self.items[self.i % len(self.items)]
        self.i += 1
        return it


def make_consts():
    cb = np.zeros((128, NCB), np.float32)
    cb[:, CB_ID:CB_ID + 128] = np.eye(128)
    kk = np.arange(128)[:, None]
    qq = np.arange(128)[None, :]
    for h in range(4):
        cb[:, CB_IREP + h * 128:CB_IREP + (h + 1) * 128] = BIG * np.eye(128)
        cb[:, CB_TRI + h * 128:CB_TRI + (h + 1) * 128] = np.where(kk > qq, -BIG, 0.0)
        cb[:, CB_TRILO + h * 128:CB_TRILO + (h + 1) * 128] = np.where(kk <= qq, -BIG, 0.0)
        j = np.arange(8)[:, None]
        cb[0:8, CB_BND + h * 128:CB_BND + (h + 1) * 128] = np.where(qq >= 16 * j + 15, 0.0, -BIG)
    for j in range(8):
        cb[j, CB_WD + j + 120] = 1.0
    cb[1:128, CB_VAUGC] = 1.0
    for cp in range(1, 128):
        c = cp - 1
        for jj in range(32):
            if 16 * c < 64 * jj + 64 and 16 * c + 32 > 64 * jj:
                cb[cp, CB_VAUGC + 2 + jj] = 1.0
    cb[:, CB_ONES:CB_ONES + 128] = 1.0
    cf = np.zeros((128, NCF), np.float32)
    q = np.arange(128)[:, None]
    hq = (q >= 64).astype(np.int64)
    x = np.arange(64)[None, :]
    dj = x - 32
    fm = np.zeros((128, 64), np.float32)
    fm[(dj == hq) | (dj == hq - 1)] = 1e4
    fm[dj > hq] = NEG
    cf[:, CF_FM:CF_FM + 64] = fm
    cf[:, CF_F0] = 1e4
    cf[:, CF_TRIQK:CF_TRIQK + 128] = np.where(np.arange(128)[None, :] > q, NEG, 0.0)
    cf[:, CF_ID:CF_ID + 128] = np.eye(128)
    return cb, cf


def pack_vecs(inp):
    v = np.zeros((128, NV), np.float32)

    def put(off, vec):
        vec = np.asarray(vec, np.float32).reshape(-1, 128)
        v[:, off:off + vec.shape[0]] = vec.T
    put(V_NMIX0, inp["l0_norm_mix"]); put(V_NFFN0, inp["l0_norm_ffn"])
    put(V_NMIX1, inp["l1_norm_mix"]); put(V_NFFN1, inp["l1_norm_ffn"]); put(V_NFIN, inp["final_norm"])
    put(V_MODB0, inp["l0_mod_b"]); put(V_MODB1, inp["l1_mod_b"])
    cw = np.asarray(inp["l0_conv_w"], np.float32)
    for c in range(4):
        v[:, V_CONVW + c * 4:V_CONVW + c * 4 + 4] = cw[:, c * 128:(c + 1) * 128].T
    put(V_CONVB, inp["l0_conv_b"]); put(V_BA, inp["l0_lru_ba"]); put(V_BX, inp["l0_lru_bx"]); put(V_LAM, inp["l0_lru_lambda"])
    pet = np.zeros((64, 66), np.float32)
    pet[:, 0:32] = np.asarray(inp["l0_cmp_pe_k"], np.float32).T
    pet[:, 32:64] = np.asarray(inp["l0_cmp_pe_v"], np.float32).T
    return v, pet


WEIGHTS = [("l0_mod_w", [D, 6 * D]), ("l1_mod_w", [D, 6 * D]), ("l0_w_in", [D, IN0]), ("l1_w_in", [D, IN1]),
           ("l0_w_out", [D, D]), ("l1_w_out", [D, D]),
           ("l0_ffn_wg", [D, DFF]), ("l0_ffn_wu", [D, DFF]), ("l0_ffn_wd", [DFF, D]),
           ("l1_ffn_wg", [D, DFF]), ("l1_ffn_wu", [D, DFF]), ("l1_ffn_wd", [DFF, D]),
           ("l0_lru_wa", [8, 64, 64]), ("l0_lru_wx", [8, 64, 64]),
           ("l0_cmp_w1_k", [2048, 128]), ("l0_cmp_w2_k", [128, 64]), ("l0_cmp_w1_v", [2048, 128]), ("l0_cmp_w2_v", [128, 64])]


def build_program(stop_after=None, nseq=2, ntg_run=NTG, dbg_yc=False, layers=(0, 1), max_ops=None):
    nc = bass.Bass("TRN2", target_bir_lowering=False)
    S = Sched(nc)
    S.max_ops = max_ops
    dr = {}
    for name, shp in WEIGHTS:
        dr[name] = nc.dram_tensor(name, shp, F32, kind="ExternalInput").ap()
    xT_d = nc.dram_tensor("xT", [2, 128, KC, SEQ], F32, kind="ExternalInput").ap()
    cT_d = nc.dram_tensor("cT", [128, KC, 2], F32, kind="ExternalInput").ap()
    vecs_d = nc.dram_tensor("vecs", [128, NV], F32, kind="ExternalInput").ap()
    pet_d = nc.dram_tensor("pet", [64, 66], F32, kind="ExternalInput").ap()
    cb_d = nc.dram_tensor("cb", [128, NCB], F32, kind="ExternalInput").ap()
    cf_d = nc.dram_tensor("cf", [128, NCF], F32, kind="ExternalInput").ap()
    out_d = nc.dram_tensor("outT", [2, 128, KC, SEQ], F32, kind="ExternalOutput").ap()

    def sb(name, shape, dt=F32):
        return nc.alloc_sbuf_tensor(name, list(shape), dt).ap()

    xT = sb("xT_sb", [128, KC, SEQ])
    vecs = sb("vecs_sb", [128, NV])
    cb = sb("cb_sb", [128, NCB], BF16)
    cf = sb("cf_sb", [128, NCF])
    pet = sb("pet_sb", [64, 66], BF16)
    mod = [sb("mod%d" % l, [128, 48, 2]) for l in range(2)]
    gsm = [sb("gsm%d" % l, [128, KC, 2]) for l in range(2)]
    gsf = [sb("gsf%d" % l, [128, KC, 2]) for l in range(2)]
    siluc = sb("siluc", [128, KC, 2])
    lruc = sb("lruc", [128, 24])
    c_one = sb("c_one", [128, 1]); c_eps = sb("c_eps", [128, 1])
    ps = [nc.alloc_psum_tensor("ps%d" % i, [128, 512], F32).ap() for i in range(8)]
    PK = [("ps", i) for i in range(8)]

    ident = cb[:, CB_ID:CB_ID + 128]
    irep = cb[:, CB_IREP:CB_IREP + 512]
    trineg = cb[:, CB_TRI:CB_TRI + 512]
    trilo = cb[:, CB_TRILO:CB_TRILO + 512]
    bnd = cb[0:8, CB_BND:CB_BND + 512]
    wd = cb[0:8, CB_WD:CB_WD + 128]
    onesb = cb[:, CB_ONES:CB_ONES + 128]
    identf = cf[:, CF_ID:CF_ID + 128]
    triqk = cf[:, CF_TRIQK:CF_TRIQK + 128]

    OV_BYTES = nc.sbuf_bytes_remaining - 2048
    ov = nc.alloc_sbuf_tensor("ov", [128, OV_BYTES // 2], BF16).ap()
    ovf = ov.bitcast(F32)

    class Carver:
        def __init__(self):
            self.off = 0

        def get(self, shape, dt=F32, parts=128):
            n = int(np.prod(shape[1:]))
            esz = 4 if dt == F32 else 2
            self.off = (self.off + 31) // 32 * 32
            o = self.off
            self.off += n * esz
            assert self.off <= OV_BYTES, ("overlay overflow", self.off, OV_BYTES)
            base = ovf if dt == F32 else ov
            a = base[0:shape[0], o // esz:o // esz + n]
            if len(shape) == 3:
                a = a.rearrange("p (a b) -> p a b", b=shape[2])
            elif len(shape) == 4:
                a = a.rearrange("p (a b c) -> p a b c", b=shape[2], c=shape[3])
            return a

    def mm(out, lhsT, rhs, start, stop, r, w):
        S.op("pe", lambda e: e.matmul(out, lhsT=lhsT, rhs=rhs, start=start, stop=stop), r=r, w=w, same_ok=True)

    def act(out, in_, func, r, w, bias=None, scale=None, accum=None):
        kw = {}
        if bias is not None:
            kw["bias"] = bias
        if scale is not None:
            kw["scale"] = scale
        if accum is not None:
            kw["accum_out"] = accum
        S.op("act", lambda e: e.activation(out=out, in_=in_, func=func, **kw), r=r, w=w)

    def ts(eng, out, in0, s1, s2, op0, op1, r, w, accum=None):
        kw = {}
        if accum is not None:
            kw["accum_out"] = accum
        if op1 is None:
            S.op(eng, lambda e: e.tensor_scalar(out=out, in0=in0, scalar1=s1, scalar2=None, op0=op0, **kw), r=r, w=w)
        else:
            S.op(eng, lambda e: e.tensor_scalar(out=out, in0=in0, scalar1=s1, scalar2=s2, op0=op0, op1=op1, **kw), r=r, w=w)

    def tt(eng, out, in0, in1, op, r, w):
        S.op(eng, lambda e: e.tensor_tensor(out=out, in0=in0, in1=in1, op=op), r=r, w=w)

    def stt(out, in0, scalar, in1, op0, op1, r, w):
        S.op("dve", lambda e: e.scalar_tensor_tensor(out=out, in0=in0, scalar=scalar, in1=in1, op0=op0, op1=op1), r=r, w=w)

    def cp(eng, out, in_, r, w):
        if eng == "act":
            S.op("act", lambda e: e.copy(out=out, in_=in_), r=r, w=w)
        else:
            S.op(eng, lambda e: e.tensor_copy(out=out, in_=in_), r=r, w=w)

    def memset(eng, ap, val, w):
        S.op(eng, lambda e: e.memset(ap, val), w=w)

    def recip(out, in_, r, w):
        S.op("dve", lambda e: e.reciprocal(out=out, in_=in_), r=r, w=w)

    def treduce(out, in_, op, r, w):
        S.op("dve", lambda e: e.tensor_reduce(out=out, in_=in_, axis=AX.X, op=op), r=r, w=w)

    def max8(out, in_, r, w):
        S.op("dve", lambda e: e.max(out=out, in_=in_), r=r, w=w)

    def transp(out, in_, idn, r, w):
        S.op("pe", lambda e: e.transpose(out, in_, idn), r=r, w=w, same_ok=True)

    def scan(out, d0, d1, init, r, w):
        S.op("dve", lambda e: e.tensor_tensor_scan(out=out, data0=d0, data1=d1, initial=init, op0=ALU.mult, op1=ALU.add), r=r, w=w)

    wprev = [None]

    def wdma(out, in_, w, sname):
        if wprev[0] is not None:
            S.wait_tok("pool", wprev[0])
        t_ = S.dma("pool", lambda e: e.dma_start(out=out, in_=in_), w=w, sname=sname)
        if t_ is not None:
            wprev[0] = t_

    def gelu_inplace(z, t, r_keys, zk, tk):
        tt("pool", t, z, z, ALU.mult, r=[zk], w=[tk])
        ts("pool", t, t, 0.044715, 1.0, ALU.mult, ALU.add, r=[tk], w=[tk])
        tt("pool", t, t, z, ALU.mult, r=[tk, zk], w=[tk])
        act(t, t, AF.Exp, r=[tk], w=[tk], scale=-1.5957691216)
        ts("dve", t, t, 1.0, None, ALU.add, None, r=[tk], w=[tk])
        recip(t, t, r=[tk], w=[tk])
        tt("pool", t, t, z, ALU.mult, r=[tk, zk], w=[tk])

    S.dma("sp", lambda e: e.dma_start(out=vecs, in_=vecs_d), w=["vecs"], sname="vecs")
    S.dma("sp", lambda e: e.dma_start(out=cf, in_=cf_d), w=["cf"], sname="cf")
    S.dma("sp", lambda e: e.dma_start(out=siluc, in_=cT_d), w=["siluc"], sname="siluc")
    wdma(cb, cb_d, ["cb"], "cb")
    wdma(pet, pet_d, ["pet"], "pet")
    memset("dve", c_one, 1.0, ["c_one"])
    memset("dve", c_eps, EPS, ["c_eps"])
    act(siluc, siluc, AF.Silu, r=["siluc"], w=["siluc"])
    act(lruc[:, 8:12], vecs[:, V_LAM:V_LAM + 4], AF.Exp, r=["vecs"], w=["lruc"], scale=-1.0)
    act(lruc[:, 8:12], lruc[:, 8:12], AF.Ln, r=["lruc", "c_one"], w=["lruc"], bias=c_one[:, 0:1])
    ts("dve", lruc[:, 0:4], lruc[:, 8:12], -8.0, None, ALU.mult, None, r=["lruc"], w=["lruc"])
    ts("dve", lruc[:, 4:8], lruc[:, 8:12], -16.0, None, ALU.mult, None, r=["lruc"], w=["lruc"])
    ts("dve", lruc[:, 12:16], vecs[:, V_BA:V_BA + 4], -1.0, None, ALU.mult, None, r=["vecs", "lruc"], w=["lruc"])
    ts("dve", lruc[:, 16:20], vecs[:, V_BX:V_BX + 4], -1.0, None, ALU.mult, None, r=["vecs", "lruc"], w=["lruc"])

    NG = 768
    C0 = Carver()
    stg = [C0.get([128, KC, NG]) for i in range(2)]
    gi = 0
    bankrot = Rot([0, 1, 2, 3, 4, 5, 6, 7])
    for l in range(2):
        mw = dr["l%d_mod_w" % l].rearrange("(k p) n -> p k n", p=128)
        vb = V_MODB0 if l == 0 else V_MODB1
        for j in range(6 * D // NG):
            st = stg[gi % 2]
            sk = "modstg%d" % (gi % 2)
            S.dma("sp", lambda e, st=st, j=j, mw=mw: e.dma_start(out=st, in_=mw[:, :, j * NG:(j + 1) * NG]), w=[sk], sname=sk)
            gi += 1
            for nn in range(NG // 128):
                b = bankrot.next()
                col = j * (NG // 128) + nn
                for k in range(KC):
                    mm(ps[b][:, 0:2], st[:, k, nn * 128:(nn + 1) * 128], siluc[:, k, :], k == 0, k == KC - 1,
                       r=[sk, "siluc"], w=[PK[b]])
                ts("dve", mod[l][:, col, :], ps[b][:, 0:2], vecs[:, vb + col:vb + col + 1], None, ALU.add, None,
                   r=[PK[b], "vecs"], w=["mod%d" % l])
        nm = V_NMIX0 if l == 0 else V_NMIX1
        nf = V_NFFN0 if l == 0 else V_NFFN1
        for s in range(2):
            stt(gsm[l][:, :, s], mod[l][:, 8:16, s], 1.0, vecs[:, nm:nm + 8], ALU.add, ALU.mult, r=["mod%d" % l, "vecs"], w=["gs%d" % l])
            stt(gsf[l][:, :, s], mod[l][:, 32:40, s], 1.0, vecs[:, nf:nf + 8], ALU.add, ALU.mult, r=["mod%d" % l, "vecs"], w=["gs%d" % l])

    def norm_group(tg, gs_ap, sh_ap, hT_out, hkey, tmp_sq, tmp_f, rstd, rkeys, scale_only=False, width=TG):
        c0 = tg * width
        xk = [("xT", t) for t in range(c0 // TG, (c0 + width) // TG)]
        b = bankrot.next()
        for k in range(KC):
            sq = tmp_sq[k % 2]
            act(sq, xT[:, k, c0:c0 + width], AF.Square, r=xk + rkeys, w=[("nsq", k % 2)])
            mm(ps[b][:, 0:width], onesb, sq, k == 0, k == KC - 1, r=[("nsq", k % 2), "cb"], w=[PK[b]])
        act(rstd, ps[b][:, 0:width], AF.Ln, r=[PK[b], "c_eps"], w=["rstd"], bias=c_eps[:, 0:1], scale=1.0 / D)
        act(rstd, rstd, AF.Exp, r=["rstd"], w=["rstd"], scale=-0.5)
        for k in range(KC):
            tf = tmp_f[k % 2]
            stt(tf, xT[:, k, c0:c0 + width], gs_ap[:, k:k + 1], rstd, ALU.mult, ALU.mult,
                r=xk + ["rstd"] + rkeys, w=[("ntf", k % 2)])
            if sh_ap is not None:
                act(hT_out[:, k, :], tf, AF.Identity, r=[("ntf", k % 2)] + rkeys, w=[hkey], bias=sh_ap[:, k:k + 1])
            else:
                cp("act", hT_out[:, k, :], tf, r=[("ntf", k % 2)], w=[hkey])

    dbg = {}

    for s in range(nseq):
        if s > 0:
            S.new_epoch()
        for k in range(KC):
            S.dma("sp", lambda e, k=k, s=s: e.dma_start(out=xT[:, k, :], in_=xT_d[s, :, k, :]),
                  w=[("xT", t) for t in range(NTG)], sname="xT%d" % k)

        for layer in layers:
            if stop_after == ("p0",):
                break
            S.barrier()
            C = Carver()
            nin = IN0 if layer == 0 else IN1
            WA = C.get([128, KC, nin], BF16)
            WB = C.get([128, KC, D], BF16)
            win_d = dr["l%d_w_in" % layer].rearrange("(k p) n -> p k n", p=128)
            for k in range(KC):
                wdma(WA[:, k, :], win_d[:, k, :], ["WA"], "WA")
            wout_d = dr["l%d_w_out" % layer].rearrange("(k p) n -> p k n", p=128)
            for k in range(0, KC, 4):
                wdma(WB[:, k:k + 4, :], wout_d[:, k:k + 4, :], ["WB"], "WB")
            hTg = C.get([128, KC, TG], BF16)
            nsq = [C.get([128, TG], BF16) for _ in range(2)]
            ntf = [C.get([128, TG]) for _ in range(2)]
            rstd = C.get([128, TG])
            yT = C.get([128, KC, TG], BF16)
            pT = [C.get([128, 512], BF16) for _ in range(3)]
            pTrot = Rot([0, 1, 2])
            ytok = C.get([128, 512 if layer == 0 else 1024])
            gm_ap = mod[layer][:, 16:24, s]
            shm_ap = mod[layer][:, 0:8, s]
            SB = Rot([0, 1, 2])
            AB = Rot([3, 4])
            MB = Rot([5, 6, 7])

            def outproj(tg):
                if dbg_yc == "l1_yc" and layer == 1:
                    cp("dve", xT[:, :, tg * TG:(tg + 1) * TG], yT, r=["yT", ("xT", tg)], w=[("xT", tg)])
                    return
                for ko in range(KC):
                    b = MB.next()
                    for kf in range(KC):
                        mm(ps[b][:, 0:TG], WB[:, kf, ko * 128:(ko + 1) * 128], yT[:, kf, :], kf == 0, kf == KC - 1,
                           r=["WB", "yT", "yTa", "yTb"], w=[PK[b]])
                    stt(xT[:, ko, tg * TG:(tg + 1) * TG], ps[b][:, 0:TG], gm_ap[:, ko:ko + 1], xT[:, ko, tg * TG:(tg + 1) * TG],
                        ALU.mult, ALU.add, r=[PK[b], "mod%d" % layer, ("xT", tg)], w=[("xT", tg)])

            if layer == 0:
                W1 = [C.get([64, 32, 128], BF16) for _ in range(2)]
                W2 = [C.get([128, 64], BF16) for _ in range(2)]
                BD = [C.get([128, 4, 128], BF16) for _ in range(2)]
                for i, nm_ in enumerate(("k", "v")):
                    w1d = dr["l0_cmp_w1_" + nm_].rearrange("(l d) m -> d l m", d=64)
                    for l0_ in range(0, 32, 4):
                        wdma(W1[i][:, l0_:l0_ + 4, :], w1d[:, l0_:l0_ + 4, :], ["W1%d" % i], "W1%d" % i)
                    wdma(W2[i], dr["l0_cmp_w2_" + nm_], ["W2%d" % i], "W2%d" % i)
                for i, nm_ in enumerate(("wa", "wx")):
                    memset("pool", BD[i], 0.0, ["BD%d" % i])
                    for c in range(4):
                        wdma(BD[i][0:64, c, 0:64], dr["l0_lru_" + nm_][2 * c], ["BD%d" % i], "BD%d" % i)
                        wdma(BD[i][64:128, c, 64:128], dr["l0_lru_" + nm_][2 * c + 1], ["BD%d" % i], "BD%d" % i)
                ksT = C.get([128, 2, SEQ], BF16)
                kwT = C.get([128, 2, 6 * 128], BF16)
                VsA = C.get([128, 16, 2, 66], BF16)
                VwA = C.get([128, 6, 2, 66], BF16)
                kcR = [C.get([64, 2, 16, 17], BF16) for _ in range(2)]
                KcT = C.get([128, 2, 128], BF16)
                VcT = C.get([64, 2, 128])
                VcA = C.get([128, 2, 98], BF16)
                hid = C.get([128, 16]); hidt = C.get([128, 16]); hidb = C.get([128, 16], BF16)
                pebias = C.get([128, 2])
                qT = C.get([128, 8, TG], BF16)
                sig = C.get([128, TPG, 24]); gtsraw = C.get([128, TPG, 24])
                AXb = C.get([128, 4, TG + 3])
                agf = C.get([128, TG]); gt = C.get([128, TG]); xc = C.get([128, TG]); xcb = C.get([128, TG], BF16)
                rr = C.get([128, TG]); ii = C.get([128, TG]); aa = C.get([128, TG])
                carry = C.get([128, 4])
                negE = [C.get([128, 32, 64], BF16) for _ in range(2)]
                impn = C.get([128, 4, 32]); imp = C.get([128, 32]); top8 = C.get([128, 8]); negsel = C.get([128, 32], BF16)
                rec = C.get([128, 4]); coef = C.get([128, 4]); tmpo = C.get([128, 4, 64])

                memset("dve", VsA, 0.0, [("VsA", t) for t in range(NTG)])
                memset("dve", VwA, 0.0, ["VwA"])
                for t_ in range(16):
                    memset("dve", VsA[:, t_, :, 64:65], 1.0, [("VsA", t_ // TPG)])
                for t_ in range(6):
                    memset("dve", VwA[:, t_, :, 64:65], 1.0, ["VwA"])
                memset("dve", KcT, 0.0, ["KcT"])
                memset("dve", qT, 0.0, ["qT"])
                memset("pool", ksT, 0.0, [("ksT", t) for t in range(NTG)])
                memset("pool", kwT, 0.0, [("kwT", t) for t in range(6)])
                memset("dve", VcT, 0.0, ["VcT"])
                memset("dve", AXb, 0.0, ["AXb"])
                for i in range(2):
                    memset("pool", kcR[i], 0.0, ["kcR%d" % i])
                for g in range(2):
                    cp("pool", VcA[:, g, 64:98], cb[:, CB_VAUGC:CB_VAUGC + 34], r=["cb"], w=["VcA"])
                for i in range(2):
                    b = MB.next()
                    for l in range(32):
                        mm(ps[b][:, 0:2], W1[i][:, l, :], pet[:, 32 * i + l:32 * i + l + 2],
                           l == 0, l == 31, r=["W1%d" % i, "pet"], w=[PK[b]])
                    cp("dve", pebias[:, i:i + 1], ps[b][:, 0:1], r=[PK[b]], w=["pebias"])

                LOOK = 2

                def emit_pv0(item, ab, nkt):
                    pi, vrhs, vkey, n_ = item
                    for h in range(4):
                        mm(ps[ab][:, h * 66:(h + 1) * 66], pT[pi][:, h * 128:(h + 1) * 128], vrhs,
                           n_ == 0 and h == 0, n_ == nkt - 1 and h == 3, r=[("pT", pi), vkey], w=[PK[ab]])

                for tg in range(ntg_run):
                    t0 = tg * TG
                    norm_group(tg, gsm[0][:, :, s], shm_ap, hTg, "hTg", nsq, ntf, rstd, ["gs0", "mod0"])
                    if dbg_yc == "l0_h":
                        cp("dve", xT[:, :, tg * TG:(tg + 1) * TG], hTg, r=["hTg", ("xT", tg)], w=[("xT", tg)])
                        continue
                    def lru_gen(tg=tg):
                        for c in range(4):
                            bg = MB.next()
                            for k in range(KC):
                                mm(ps[bg][:, 0:TG], WA[:, k, c * 128:(c + 1) * 128], hTg[:, k, :], k == 0, k == KC - 1, r=["WA", "hTg"], w=[PK[bg]])
                            cp("act", agf, ps[bg][:, 0:TG], r=[PK[bg]], w=["agf"])
                            yield
                            bx_ = MB.next()
                            for k in range(KC):
                                mm(ps[bx_][:, 0:TG], WA[:, k, 512 + c * 128:512 + (c + 1) * 128], hTg[:, k, :], k == 0, k == KC - 1, r=["WA", "hTg"], w=[PK[bx_]])
                            cp("act", AXb[:, c, 3:3 + TG], ps[bx_][:, 0:TG], r=[PK[bx_]], w=["AXb"])
                            yield
                            cw = V_CONVW + c * 4
                            ts("dve", xc, AXb[:, c, 0:TG], vecs[:, cw:cw + 1], vecs[:, V_CONVB + c:V_CONVB + c + 1], ALU.mult, ALU.add, r=["AXb", "vecs"], w=["xc"])
                            yield
                            for kk_ in range(1, 4):
                                stt(xc, AXb[:, c, kk_:kk_ + TG], vecs[:, cw + kk_:cw + kk_ + 1], xc, ALU.mult, ALU.add, r=["AXb", "vecs", "xc"], w=["xc"])
                                yield
                            cp("pool", AXb[:, c, 0:3], AXb[:, c, TG:TG + 3], r=["AXb"], w=["AXb"])
                            cp("act", xcb, xc, r=["xc"], w=["xcb"])
                            yield
                            br = MB.next()
                            mm(ps[br][:, 0:TG], BD[0][:, c, :], xcb, True, True, r=["BD0", "xcb"], w=[PK[br]])
                            bi = MB.next()
                            mm(ps[bi][:, 0:TG], BD[1][:, c, :], xcb, True, True, r=["BD1", "xcb"], w=[PK[bi]])
                            yield
                            act(rr, ps[br][:, 0:TG], AF.Exp, r=[PK[br], "lruc"], w=["rr"], bias=lruc[:, 12 + c:13 + c], scale=-1.0)
                            act(ii, ps[bi][:, 0:TG], AF.Exp, r=[PK[bi], "lruc"], w=["ii"], bias=lruc[:, 16 + c:17 + c], scale=-1.0)
                            yield
                            ts("dve", rr, rr, 1.0, None, ALU.add, None, r=["rr"], w=["rr"])
                            yield
                            recip(rr, rr, r=["rr"], w=["rr"])
                            yield
                            ts("dve", ii, ii, 1.0, None, ALU.add, None, r=["ii"], w=["ii"])
                            yield
                            recip(ii, ii, r=["ii"], w=["ii"])
                            yield
                            act(aa, rr, AF.Exp, r=["rr", "lruc"], w=["aa"], scale=lruc[:, c:c + 1])
                            yield
                            act(rr, rr, AF.Exp, r=["rr", "lruc"], w=["rr"], scale=lruc[:, 4 + c:5 + c])
                            yield
                            ts("dve", rr, rr, -1.0, 1.0, ALU.mult, ALU.add, r=["rr"], w=["rr"])
                            yield
                            ts("dve", rr, rr, 1e-18, None, ALU.max, None, r=["rr"], w=["rr"])
                            yield
                            act(rr, rr, AF.Ln, r=["rr"], w=["rr"])
                            yield
                            act(rr, rr, AF.Exp, r=["rr"], w=["rr"], scale=0.5)
                            yield
                            if tg == 0:
                                memset("dve", rr[:, 0:1], 1.0, ["rr"])
                            tt("dve", ii, ii, xc, ALU.mult, r=["ii", "xc"], w=["ii"])
                            yield
                            tt("dve", ii, ii, rr, ALU.mult, r=["ii", "rr"], w=["ii"])
                            yield
                            init = 0.0 if tg == 0 else carry[:, c:c + 1]
                            scan(xc, aa, ii, init, r=["aa", "ii", "carry"], w=["xc"])
                            yield
                            cp("dve", carry[:, c:c + 1], xc[:, TG - 1:TG], r=["xc"], w=["carry"])
                            tt("pool", gt, agf, agf, ALU.mult, r=["agf"], w=["gt"])
                            yield
                            ts("pool", gt, gt, 0.044715, 1.0, ALU.mult, ALU.add, r=["gt"], w=["gt"])
                            yield
                            tt("pool", gt, gt, agf, ALU.mult, r=["gt", "agf"], w=["gt"])
                            yield
                            act(gt, gt, AF.Exp, r=["gt"], w=["gt"], scale=-1.5957691216)
                            yield
                            ts("dve", gt, gt, 1.0, None, ALU.add, None, r=["gt"], w=["gt"])
                            yield
                            recip(gt, gt, r=["gt"], w=["gt"])
                            yield
                            tt("pool", gt, gt, agf, ALU.mult, r=["gt", "agf"], w=["gt"])
                            yield
                            tt("pool", yT[:, c, :], xc, gt, ALU.mult, r=["xc", "gt"], w=["yTa"])
                            yield

                    lgen = lru_gen()
                    nsteps_ = 0
                    for j_ in range(TPG):
                        ib_ = tg * TPG + j_
                        nsteps_ += 2 * (1 + len([kt for kt in range(ib_ - 4, ib_ + 1) if kt >= 0]) + ib_ + 1)
                    per_step = -(-26 * 4 // max(nsteps_ - 2, 1))

                    def pump(n):
                        for _ in range(n):
                            try:
                                next(lgen)
                            except StopIteration:
                                return
                    for h in range(8):
                        b = MB.next()
                        for k in range(KC):
                            mm(ps[b][0:64, 0:TG], WA[:, k, 1024 + h * 64:1024 + (h + 1) * 64], hTg[:, k, :], k == 0, k == KC - 1, r=["WA", "hTg"], w=[PK[b]])
                        cp("act" if h % 2 == 0 else "dve", qT[0:64, h, :], ps[b][0:64, 0:TG], r=[PK[b]], w=["qT"])
                    for which, cbase in (("kc", 1536), ("vc", 1664), ("ks", 1792), ("kw", 2048)):
                        for g in range(2):
                            b = MB.next()
                            for k in range(KC):
                                mm(ps[b][0:64, 0:TG], WA[:, k, cbase + g * 64:cbase + (g + 1) * 64], hTg[:, k, :], k == 0, k == KC - 1, r=["WA", "hTg"], w=[PK[b]])
                            if which == "ks":
                                cp("act", ksT[0:64, g, t0:t0 + TG], ps[b][0:64, 0:TG], r=[PK[b]], w=[("ksT", tg)])
                            elif which == "kw":
                                for j in range(TPG):
                                    kt = tg * TPG + j
                                    sl = kt % 6
                                    cp("dve", kwT[0:64, g, sl * 128:(sl + 1) * 128], ps[b][0:64, j * 128:(j + 1) * 128], r=[PK[b]], w=[("kwT", sl)])
                            else:
                                i = 0 if which == "kc" else 1
                                if g == 0:
                                    if tg > 0:
                                        cp("pool", kcR[i][:, :, :, 0:1], kcR[i][:, :, :, 16:17], r=["kcR%d" % i], w=["kcR%d" % i])
                                cp("act", kcR[i][:, g, :, 1:17], ps[b][0:64, 0:TG].rearrange("p (b r) -> p r b", r=16), r=[PK[b]], w=["kcR%d" % i])
                    for j in range(TPG):
                        kt = tg * TPG + j
                        b = MB.next()
                        for k in range(KC):
                            mm(ps[b][:, 0:128], hTg[:, k, j * 128:(j + 1) * 128], WA[:, k, 1920:2048], k == 0, k == KC - 1, r=["WA", "hTg"], w=[PK[b]])
                        cp("act", VsA[:, kt, :, 0:64], ps[b][:, 0:128].rearrange("p (g d) -> p g d", d=64), r=[PK[b]], w=[("VsA", tg)])
                        b = MB.next()
                        for k in range(KC):
                            mm(ps[b][:, 0:152], hTg[:, k, j * 128:(j + 1) * 128], WA[:, k, 2176:2328], k == 0, k == KC - 1, r=["WA", "hTg"], w=[PK[b]])
                        cp("dve", VwA[:, kt % 6, :, 0:64], ps[b][:, 0:128].rearrange("p (g d) -> p g d", d=64), r=[PK[b]], w=[("VwA", kt % 6)])
                        cp("dve", gtsraw[:, j, :], ps[b][:, 128:152], r=[PK[b]], w=["gtsraw"])
                    act(sig, gtsraw, AF.Exp, r=["gtsraw"], w=["sig"], scale=-1.0)
                    ts("dve", sig, sig, 1.0, None, ALU.add, None, r=["sig"], w=["sig"])
                    recip(sig, sig, r=["sig"], w=["sig"])
                    for i in range(2):
                        for g in range(2):
                            b = MB.next()
                            for l in range(32):
                                mm(ps[b][:, 0:16], W1[i][:, l, :], kcR[i][:, g, l % 16, (l // 16):(l // 16) + 16], l == 0, l == 31,
                                   r=["W1%d" % i, "kcR%d" % i], w=[PK[b]])
                            act(hid, ps[b][:, 0:16], AF.Identity, r=[PK[b], "pebias"], w=["hid"], bias=pebias[:, i:i + 1])
                            gelu_inplace(hid, hidt, None, "hid", "hidt")
                            cp("pool", hidb, hidt, r=["hidt"], w=["hidb"])
                            b2 = MB.next()
                            mm(ps[b2][0:64, 0:16], W2[i], hidb, True, True, r=["W2%d" % i, "hidb"], w=[PK[b2]])
                            if i == 0:
                                cp("act", KcT[0:64, g, 16 * tg:16 * tg + 16], ps[b2][0:64, 0:16], r=[PK[b2]], w=["KcT"])
                            else:
                                cp("act", VcT[:, g, 16 * tg:16 * tg + 16], ps[b2][0:64, 0:16], r=[PK[b2]], w=["VcT"])
                    if tg == 0:
                        memset("dve", KcT[0:64, :, 0:1], 0.0, ["KcT"])
                        memset("dve", VcT[:, :, 0:1], 0.0, ["VcT"])
                    for g in range(2):
                        b = MB.next()
                        transp(ps[b][:, 0:64], VcT[:, g, :], identf[0:64, 0:64], r=["VcT", "cf"], w=[PK[b]])
                        cp("act", VcA[:, g, 0:64], ps[b][:, 0:64], r=[PK[b]], w=["VcA"])

                    for j in range(TPG):
                        ib = tg * TPG + j
                        qc = slice(j * 128, (j + 1) * 128)
                        for g in range(2):
                            qrhs = qT[:, 4 * g:4 * g + 4, qc]
                            M = 8 * (ib + 1)
                            sbk = SB.next()
                            mm(ps[sbk][0:M, :], KcT[:, g, 0:M], qrhs, True, False, r=["KcT", "qT"], w=[PK[sbk]])
                            s0 = 120 - 8 * ib
                            mm(ps[sbk][0:M, :], wd[:, s0:s0 + M], bnd, False, True, r=["cb"], w=[PK[sbk]])
                            pi = pTrot.next()
                            act(pT[pi][0:M, :], ps[sbk][0:M, :], AF.Exp, r=[PK[sbk]], w=[("pT", pi)], scale=0.125)
                            ab = AB.next()
                            for h in range(4):
                                mm(ps[ab][:, h * 98:(h + 1) * 98], pT[pi][0:M, h * 128:(h + 1) * 128], VcA[0:M, g, :], h == 0, h == 3,
                                   r=[("pT", pi), "VcA"], w=[PK[ab]])
                            pump(per_step)
                            accv = ps[ab][:, 0:392].rearrange("p (h c) -> p h c", c=98)
                            ts("dve", rec, accv[:, :, 64], 1e-30, None, ALU.max, None, r=[PK[ab]], w=["rec"])
                            recip(rec, rec, r=["rec"], w=["rec"])
                            tt("dve", impn, accv[:, :, 66:98], rec.unsqueeze(2).to_broadcast([128, 4, 32]), ALU.mult, r=[PK[ab], "rec"], w=["impn"])
                            treduce(imp, impn.rearrange("p h j -> p j h"), ALU.add, r=["impn"], w=["imp"])
                            tt("dve", imp, imp, cf[:, CF_FM + 32 - 2 * ib:CF_FM + 64 - 2 * ib], ALU.add, r=["imp", "cf"], w=["imp"])
                            tt("dve", imp, imp, cf[:, CF_F0:CF_F0 + 32], ALU.add, r=["imp", "cf"], w=["imp"])
                            max8(top8, imp, r=["imp"], w=["top8"])
                            ts("dve", negsel, imp, top8[:, 7:8], 1.0, ALU.is_ge, ALU.subtract, r=["imp", "top8"], w=["negsel"])
                            cp("pool", negE[g], negsel.unsqueeze(2).to_broadcast([128, 32, 64]), r=["negsel"], w=[("negE", g)])
                            tt("dve", coef, rec, sig[:, j, 12 * g + 0:12 * g + 12:3], ALU.mult, r=["rec", "sig"], w=["coef"])
                            yv = ytok[:, g * 256:(g + 1) * 256].rearrange("p (h d) -> p h d", d=64)
                            tt("dve", yv, accv[:, :, 0:64], coef.unsqueeze(2).to_broadcast([128, 4, 64]), ALU.mult, r=[PK[ab], "coef"], w=["ytok"])
                            for br_, gi_ in (("win", 2), ("slc", 1)):
                                kts = [kt for kt in range(ib - 4, ib + 1) if kt >= 0] if br_ == "win" else list(range(ib + 1))
                                ab = AB.next()
                                pend = []
                                for n_, kt in enumerate(kts):
                                    sbk = SB.next()
                                    if br_ == "win":
                                        klhs = kwT[:, g, (kt % 6) * 128:(kt % 6 + 1) * 128]
                                        kkey = ("kwT", kt % 6)
                                        vrhs = VwA[:, kt % 6, g, :]
                                        vkey = ("VwA", kt % 6)
                                        extra = []
                                        if kt == ib:
                                            extra.append((ident, trineg, ["cb"]))
                                        if kt == ib - 4:
                                            extra.append((ident, trilo, ["cb"]))
                                    else:
                                        klhs = ksT[:, g, kt * 128:(kt + 1) * 128]
                                        kkey = ("ksT", kt // TPG)
                                        vrhs = VsA[:, kt, g, :]
                                        vkey = ("VsA", kt // TPG)
                                        extra = [(negE[g][:, 2 * kt:2 * kt + 2, :].rearrange("p a b -> p (a b)"), irep, [("negE", g), "cb"])]
                                        if kt == ib:
                                            extra.append((ident, trineg, ["cb"]))
                                    mm(ps[sbk][:, :], klhs, qrhs, True, len(extra) == 0, r=[kkey, "qT"], w=[PK[sbk]])
                                    for xi, (l_, r_, ks_) in enumerate(extra):
                                        mm(ps[sbk][:, :], l_, r_, False, xi == len(extra) - 1, r=ks_, w=[PK[sbk]])
                                    pi = pTrot.next()
                                    act(pT[pi], ps[sbk], AF.Exp, r=[PK[sbk]], w=[("pT", pi)], scale=0.125)
                                    pend.append((pi, vrhs, vkey, n_))
                                    if len(pend) > LOOK:
                                        emit_pv0(pend.pop(0), ab, len(kts))
                                    pump(per_step)
                                while pend:
                                    emit_pv0(pend.pop(0), ab, len(kts))
                                accw = ps[ab][:, 0:264].rearrange("p (h c) -> p h c", c=66)
                                ts("dve", rec, accw[:, :, 64], 1e-30, None, ALU.max, None, r=[PK[ab]], w=["rec"])
                                recip(rec, rec, r=["rec"], w=["rec"])
                                tt("dve", coef, rec, sig[:, j, 12 * g + gi_:12 * g + 12:3], ALU.mult, r=["rec", "sig"], w=["coef"])
                                tt("dve", tmpo, accw[:, :, 0:64], coef.unsqueeze(2).to_broadcast([128, 4, 64]), ALU.mult, r=[PK[ab], "coef"], w=["tmpo"])
                                tt("pool", yv, yv, tmpo, ALU.add, r=["ytok", "tmpo"], w=["ytok"])
                        b = MB.next()
                        for c4 in range(4):
                            transp(ps[b][:, c4 * 128:(c4 + 1) * 128], ytok[:, c4 * 128:(c4 + 1) * 128], identf, r=["ytok", "cf"], w=[PK[b]])
                        cp("act", yT[:, 4:8, qc], ps[b].rearrange("p (c q) -> p c q", q=128), r=[PK[b]], w=["yTb"])
                    pump(10 ** 6)
                    outproj(tg)
            else:
                kT = C.get([128, 2, SEQ], BF16)
                kiT = C.get([128, SEQ], BF16)
                VA = C.get([128, 16, 2, 66], BF16)
                qTs = [C.get([128, 16, TG], BF16) for _ in range(2)]
                qiTs = [C.get([128, 8, TG], BF16) for _ in range(2)]
                widxs = [C.get([128, TPG, 8]) for _ in range(2)]
                acc = C.get([128, SEQ])
                rl = [C.get([128, 512]) for _ in range(2)]
                negms = [C.get([128, SEQ], BF16) for _ in range(2)]
                lo = C.get([128, 1]); w0 = C.get([128, 1]); mid = C.get([128, 1]); cnt = C.get([128, 1]); pw = C.get([128, 1])
                junk = C.get([128, SEQ], BF16)
                rec8 = C.get([128, 8])
                memset("dve", VA, 0.0, [("VA", t) for t in range(NTG)])
                for t_ in range(16):
                    memset("dve", VA[:, t_, :, 64:65], 1.0, [("VA", t_ // TPG)])
                memset("pool", kT, 0.0, [("kT", t) for t in range(NTG)])
                memset("pool", kiT, 0.0, [("kiT", t) for t in range(NTG)])
                for i_ in range(2):
                    memset("dve", qTs[i_], 0.0, [("qT", i_)])
                    memset("pool", qiTs[i_], 0.0, [("qiT", i_)])

                def proj1(tg):
                    t0 = tg * TG
                    qT = qTs[tg % 2]; qiT = qiTs[tg % 2]; widx = widxs[tg % 2]
                    qk = ("qT", tg % 2); qik = ("qiT", tg % 2); wk = ("widx", tg % 2)
                    norm_group(tg, gsm[1][:, :, s], shm_ap, hTg, "hTg", nsq, ntf, rstd, ["gs1", "mod1"])
                    for h in range(8):
                        b = MB.next()
                        for k in range(KC):
                            mm(ps[b][0:64, 0:TG], WA[:, k, 1280 + h * 64:1280 + (h + 1) * 64], hTg[:, k, :], k == 0, k == KC - 1, r=["WA", "hTg"], w=[PK[b]])
                        cp("act" if h % 2 == 0 else "dve", qiT[0:64, h, :], ps[b][0:64, 0:TG], r=[PK[b]], w=[qik])
                    b = MB.next()
                    for k in range(KC):
                        mm(ps[b][0:64, 0:TG], WA[:, k, 1792:1856], hTg[:, k, :], k == 0, k == KC - 1, r=["WA", "hTg"], w=[PK[b]])
                    cp("act", kiT[0:64, t0:t0 + TG], ps[b][0:64, 0:TG], r=[PK[b]], w=[("kiT", tg)])
                    for j in range(TPG):
                        b = MB.next()
                        for k in range(KC):
                            mm(ps[b][:, 0:8], hTg[:, k, j * 128:(j + 1) * 128], WA[:, k, 1856:1864], k == 0, k == KC - 1, r=["WA", "hTg"], w=[PK[b]])
                        cp("dve", widx[:, j, :], ps[b][:, 0:8], r=[PK[b]], w=[wk])
                    for h in range(16):
                        b = MB.next()
                        for k in range(KC):
                            mm(ps[b][0:64, 0:TG], WA[:, k, h * 64:(h + 1) * 64], hTg[:, k, :], k == 0, k == KC - 1, r=["WA", "hTg"], w=[PK[b]])
                        cp("act" if h % 2 == 0 else "dve", qT[0:64, h, :], ps[b][0:64, 0:TG], r=[PK[b]], w=[qk])
                    for g in range(2):
                        b = MB.next()
                        for k in range(KC):
                            mm(ps[b][0:64, 0:TG], WA[:, k, 1024 + g * 64:1024 + (g + 1) * 64], hTg[:, k, :], k == 0, k == KC - 1, r=["WA", "hTg"], w=[PK[b]])
                        cp("act", kT[0:64, g, t0:t0 + TG], ps[b][0:64, 0:TG], r=[PK[b]], w=[("kT", tg)])
                    for j in range(TPG):
                        kt = tg * TPG + j
                        b = MB.next()
                        for k in range(KC):
                            mm(ps[b][:, 0:128], hTg[:, k, j * 128:(j + 1) * 128], WA[:, k, 1152:1280], k == 0, k == KC - 1, r=["WA", "hTg"], w=[PK[b]])
                        cp("act", VA[:, kt, :, 0:64], ps[b][:, 0:128].rearrange("p (g d) -> p g d", d=64), r=[PK[b]], w=[("VA", tg)])

                def stageA(ib):
                    tg = ib // TPG; j = ib % TPG
                    qiT = qiTs[tg % 2]; widx = widxs[tg % 2]; negm = negms[ib % 2]
                    qik = ("qiT", tg % 2); wk = ("widx", tg % 2); nk_ = ("negm", ib % 2)
                    qc = slice(j * 128, (j + 1) * 128)
                    nk = (ib + 1) * 128
                    for c0 in range(0, nk, 512):
                        wdt = min(512, nk - c0)
                        for h in range(8):
                            b = MB.next()
                            mm(ps[b][:, 0:wdt], qiT[:, h, qc], kiT[:, c0:c0 + wdt], True, True,
                               r=[qik] + [("kiT", t) for t in range(c0 // TG, (c0 + wdt - 1) // TG + 1)], w=[PK[b]])
                            ri = h % 2
                            act(rl[ri][:, 0:wdt], ps[b][:, 0:wdt], AF.Relu, r=[PK[b]], w=[("rl", ri)], scale=0.125)
                            if h == 0:
                                ts("dve", acc[:, c0:c0 + wdt], rl[ri][:, 0:wdt], widx[:, j, 0:1], None, ALU.mult, None, r=[("rl", ri), wk], w=["acc"])
                            else:
                                stt(acc[:, c0:c0 + wdt], rl[ri][:, 0:wdt], widx[:, j, h:h + 1], acc[:, c0:c0 + wdt], ALU.mult, ALU.add,
                                    r=[("rl", ri), wk, "acc"], w=["acc"])
                    tt("dve", acc[:, ib * 128:nk], acc[:, ib * 128:nk], triqk, ALU.add, r=["acc", "cf"], w=["acc"])
                    if ib >= 2:
                        treduce(lo, acc[:, 0:ib * 128], ALU.min, r=["acc"], w=["lo"])
                        treduce(w0, acc[:, 0:nk], ALU.max, r=["acc"], w=["w0"])
                        tt("dve", w0, w0, lo, ALU.subtract, r=["w0", "lo"], w=["w0"])
                        for it in range(1, NBIS + 1):
                            f = 2.0 ** (-it)
                            stt(mid, w0, f, lo, ALU.mult, ALU.add, r=["w0", "lo"], w=["mid"])
                            ts("dve", junk[:, 0:nk], acc[:, 0:nk], mid[:, 0:1], 0.0, ALU.is_ge, ALU.add, r=["acc", "mid"], w=["junk", "cnt"], accum=cnt)
                            ts("dve", pw, cnt, 255.5, f, ALU.is_ge, ALU.mult, r=["cnt"], w=["pw"])
                            stt(lo, pw, w0[:, 0:1], lo, ALU.mult, ALU.add, r=["pw", "w0", "lo"], w=["lo"])
                        ts("dve", negm[:, 0:nk], acc[:, 0:nk], lo[:, 0:1], 1.0, ALU.is_ge, ALU.subtract, r=["acc", "lo"], w=[nk_])
                    else:
                        ts("dve", negm[:, 0:nk], acc[:, 0:nk], -1.0e8, 1.0, ALU.is_ge, ALU.subtract, r=["acc"], w=[nk_])

                def stageB(ib):
                    tg = ib // TPG; j = ib % TPG
                    qT = qTs[tg % 2]; negm = negms[ib % 2]
                    qk = ("qT", tg % 2); nk_ = ("negm", ib % 2)
                    qc = slice(j * 128, (j + 1) * 128)
                    for g in range(2):
                        abs_ = [AB.next(), AB.next()]
                        pend = []

                        def emit_pv1(item, g=g, abs_=abs_):
                            pi, kt, half = item
                            ab = abs_[half]
                            for h in range(4):
                                mm(ps[ab][:, h * 66:(h + 1) * 66], pT[pi][:, h * 128:(h + 1) * 128], VA[:, kt, g, :],
                                   kt == 0 and h == 0, kt == ib and h == 3, r=[("pT", pi), ("VA", kt // TPG)], w=[PK[ab]])

                        for kt in range(ib + 1):
                            for half in range(2):
                                sbk = SB.next()
                                mm(ps[sbk], kT[:, g, kt * 128:(kt + 1) * 128], qT[:, 8 * g + 4 * half:8 * g + 4 * half + 4, qc], True, False,
                                   r=[("kT", kt // TPG), qk], w=[PK[sbk]])
                                mm(ps[sbk], negm[:, kt * 128:(kt + 1) * 128], irep, False, True, r=[nk_, "cb"], w=[PK[sbk]])
                                pi = pTrot.next()
                                act(pT[pi], ps[sbk], AF.Exp, r=[PK[sbk]], w=[("pT", pi)], scale=0.125)
                                pend.append((pi, kt, half))
                                if len(pend) > 2:
                                    emit_pv1(pend.pop(0))
                        while pend:
                            emit_pv1(pend.pop(0))
                        for half in range(2):
                            ab = abs_[half]
                            accw = ps[ab][:, 0:264].rearrange("p (h c) -> p h c", c=66)
                            recip(rec8[:, 0:4], accw[:, :, 64], r=[PK[ab]], w=["rec8"])
                            yv = ytok[:, (8 * g + 4 * half) * 64:(8 * g + 4 * half + 4) * 64].rearrange("p (h d) -> p h d", d=64)
                            tt("dve", yv, accw[:, :, 0:64], rec8[:, 0:4].unsqueeze(2).to_broadcast([128, 4, 64]), ALU.mult, r=[PK[ab], "rec8"], w=["ytok"])
                    for half in range(2):
                        b = MB.next()
                        for c4 in range(4):
                            cc = half * 4 + c4
                            transp(ps[b][:, c4 * 128:(c4 + 1) * 128], ytok[:, cc * 128:(cc + 1) * 128], identf, r=["ytok", "cf"], w=[PK[b]])
                        cp("act", yT[:, half * 4:half * 4 + 4, qc], ps[b].rearrange("p (c q) -> p c q", q=128), r=[PK[b]], w=["yT"])

                nq = ntg_run * TPG
                proj1(0)
                stageA(0)
                for ib in range(nq):
                    nx = ib + 1
                    if nx < nq:
                        if nx % TPG == 0:
                            proj1(nx // TPG)
                        stageA(nx)
                    stageB(ib)
                    if ib % TPG == TPG - 1:
                        outproj(ib // TPG)

            if stop_after == ("mix", layer):
                break
            S.barrier()
            C = Carver()
            hT = C.get([128, KC, SEQ], BF16)
            nsq = [C.get([128, 512], BF16) for _ in range(2)]
            ntf = [C.get([128, 512]) for _ in range(2)]
            rstd = C.get([128, 512])
            SL = [2, 4, 5, 5, 6]
            WG = [C.get([128, KC, 768], BF16) for _ in range(2)]
            WU = [C.get([128, KC, 768], BF16) for _ in range(2)]
            WDn = [C.get([128, 6, D], BF16) for _ in range(2)]
            actb = [C.get([128, 6, 512], BF16) for _ in range(2)]
            sil = [C.get([128, 512]) for _ in range(2)]
            gf_ap = mod[layer][:, 40:48, s]
            shf_ap = mod[layer][:, 24:32, s]
            wg_d = dr["l%d_ffn_wg" % layer].rearrange("(k p) n -> p k n", p=128)
            wu_d = dr["l%d_ffn_wu" % layer].rearrange("(k p) n -> p k n", p=128)
            wd_d = dr["l%d_ffn_wd" % layer].rearrange("(f p) n -> p f n", p=128)

            def load_slice(si):
                bi = si % 2
                f0 = sum(SL[:si]); nf_ = SL[si]
                for k in range(0, KC, 4):
                    wdma(WG[bi][:, k:k + 4, 0:nf_ * 128], wg_d[:, k:k + 4, f0 * 128:(f0 + nf_) * 128], [("WG", bi)], "WG%d" % bi)
                    wdma(WU[bi][:, k:k + 4, 0:nf_ * 128], wu_d[:, k:k + 4, f0 * 128:(f0 + nf_) * 128], [("WU", bi)], "WU%d" % bi)
                h_ = max(nf_ // 2, 1)
                wdma(WDn[bi][:, 0:h_, :], wd_d[:, f0:f0 + h_, :], [("WD", bi)], "WD%d" % bi)
                wdma(WDn[bi][:, h_:nf_, :], wd_d[:, f0 + h_:f0 + nf_, :], [("WD", bi)], "WD%d" % bi)

            load_slice(0)
            load_slice(1)
            for t4 in range(SEQ // 512):
                hv = hT[:, :, t4 * 512:(t4 + 1) * 512]
                norm_group(t4, gsf[layer][:, :, s], shf_ap, hv, ("hT", t4), nsq, ntf, rstd, ["gs%d" % layer, "mod%d" % layer], width=512)
            GB = Rot([0, 1, 2, 3])
            DB = Rot([4, 5, 6, 7])
            for si in range(len(SL)):
                bi = si % 2
                nf_ = SL[si]
                for t4 in range(SEQ // 512):
                    cols = slice(t4 * 512, (t4 + 1) * 512)
                    ai = (si * 4 + t4) % 2
                    for f in range(nf_):
                        bg = GB.next(); bu = GB.next()
                        for k in range(KC):
                            mm(ps[bg], WG[bi][:, k, f * 128:(f + 1) * 128], hT[:, k, cols], k == 0, k == KC - 1, r=[("WG", bi), ("hT", t4)], w=[PK[bg]])
                        for k in range(KC):
                            mm(ps[bu], WU[bi][:, k, f * 128:(f + 1) * 128], hT[:, k, cols], k == 0, k == KC - 1, r=[("WU", bi), ("hT", t4)], w=[PK[bu]])
                        sl_ = sil[f % 2]
                        act(sl_, ps[bg], AF.Silu, r=[PK[bg]], w=[("sil", f % 2)])
                        tt("dve", actb[ai][:, f, :], sl_, ps[bu], ALU.mult, r=[("sil", f % 2), PK[bu]], w=[("actb", ai)])
                    for ko in range(KC):
                        b = DB.next()
                        for f in range(nf_):
                            mm(ps[b], WDn[bi][:, f, ko * 128:(ko + 1) * 128], actb[ai][:, f, :], f == 0, f == nf_ - 1, r=[("WD", bi), ("actb", ai)], w=[PK[b]])
                        tgs = [("xT", t) for t in range(t4 * 512 // TG, (t4 + 1) * 512 // TG)]
                        stt(xT[:, ko, cols], ps[b], gf_ap[:, ko:ko + 1], xT[:, ko, cols], ALU.mult, ALU.add, r=[PK[b], "mod%d" % layer] + tgs, w=tgs)
                if si + 2 < len(SL):
                    load_slice(si + 2)
            if stop_after == ("ffn", layer):
                break

        S.cut_off = True
        S.barrier()
        C = Carver()
        nsq = [C.get([128, 512], BF16) for _ in range(2)]
        ntf = [C.get([128, 512]) for _ in range(2)]
        rstd = C.get([128, 512])
        ob = [C.get([128, KC, 512]) for _ in range(2)]
        for t4 in range(SEQ // 512):
            o = ob[t4 % 2]
            if stop_after is None:
                c0 = t4 * 512
                b = bankrot.next()
                for k in range(KC):
                    sq = nsq[k % 2]
                    act(sq, xT[:, k, c0:c0 + 512], AF.Square, r=[("xT", c0 // TG), ("xT", c0 // TG + 1)], w=[("nsq", k % 2)])
                    mm(ps[b], onesb, sq, k == 0, k == KC - 1, r=[("nsq", k % 2), "cb"], w=[PK[b]])
                act(rstd, ps[b], AF.Ln, r=[PK[b], "c_eps"], w=["rstd"], bias=c_eps[:, 0:1], scale=1.0 / D)
                act(rstd, rstd, AF.Exp, r=["rstd"], w=["rstd"], scale=-0.5)
                for k in range(KC):
                    stt(o[:, k, :], xT[:, k, c0:c0 + 512], vecs[:, V_NFIN + k:V_NFIN + k + 1], rstd, ALU.mult, ALU.mult,
                        r=[("xT", c0 // TG), ("xT", c0 // TG + 1), "rstd", "vecs"], w=[("ob", t4 % 2)])
            else:
                cp("dve", o, xT[:, :, t4 * 512:(t4 + 1) * 512], r=[("xT", t) for t in range(NTG)], w=[("ob", t4 % 2)])
            for k in range(KC):
                S.dma("sp", lambda e, o=o, k=k, s=s, t4=t4: e.dma_start(out=out_d[s, :, k, t4 * 512:(t4 + 1) * 512], in_=o[:, k, :]),
                      r=[("ob", t4 % 2)], sname="ob%d" % (t4 % 2))
    for nm_ in ("ob0", "ob1"):
        d = S.dsem[nm_]
        S.wait_tok("sp", (d[0], d[1], "dma"))
    S.replay()
    return nc


_CACHE = {}


def _prep_inputs(inputs):
    vecs, pet = pack_vecs(inputs)
    cbv, cfv = make_consts()
    x = np.asarray(inputs["x"], np.float32)
    c = np.asarray(inputs["c"], np.float32)
    shared = {name: np.ascontiguousarray(np.asarray(inputs[name], np.float32)) for name, _ in WEIGHTS}
    shared.update({"vecs": vecs, "pet": pet, "cb": cbv, "cf": cfv})
    in_maps = []
    for i in range(NCORES):
        xs = x[2 * i:2 * i + 2]
        xT = np.ascontiguousarray(xs.reshape(2, SEQ, KC, 128).transpose(0, 3, 2, 1))
        cs = c[2 * i:2 * i + 2]
        cT = np.ascontiguousarray(cs.reshape(2, KC, 128).transpose(2, 1, 0))
        m = dict(shared)
        m["xT"] = xT
        m["cT"] = cT
        in_maps.append(m)
    return in_maps


def kernel(**inputs):
    if "nc" not in _CACHE:
        _CACHE["nc"] = build_program()
    nc = _CACHE["nc"]
    in_maps = _prep_inputs(inputs)
    res = run_bass_kernel_spmd(nc, in_maps, core_ids=list(range(NCORES)))
    out = np.empty((16, SEQ, D), np.float32)
    for i in range(NCORES):
        oT = res.results[i]["outT"]
        out[2 * i:2 * i + 2] = oT.transpose(0, 3, 2, 1).reshape(2, SEQ, D)
    return out
```

```python
import numpy as np
import concourse.bass as bass
import concourse.mybir as mybir
from concourse.bass_utils import run_bass_kernel_spmd

F32 = mybir.dt.float32
BF16 = mybir.dt.bfloat16
AF = mybir.ActivationFunctionType
ALU = mybir.AluOpType
AX = mybir.AxisListType

NCORES = 8
SEQ = 2048
D = 1024
KC = 8
TG = 256
NTG = SEQ // TG
TPG = TG // 128
DFF = 2816
FCH = DFF // 128
IN0 = 2328
IN1 = 1864
BIG = 30000.0
NEG = -1.0e9
EPS = 1e-6
NBIS = 13

V_NMIX0, V_NFFN0, V_NMIX1, V_NFFN1, V_NFIN = 0, 8, 16, 24, 32
V_MODB0, V_MODB1 = 40, 88
V_CONVW, V_CONVB, V_BA, V_BX, V_LAM = 136, 152, 156, 160, 164
NV = 168
CB_ID, CB_IREP, CB_TRI, CB_TRILO, CB_BND, CB_WD, CB_VAUGC, CB_ONES = 0, 128, 640, 1152, 1664, 2176, 2304, 2338
NCB = 2338 + 128
CF_FM, CF_F0, CF_TRIQK, CF_ID = 0, 64, 96, 224
NCF = 352


class Sched:
    ENG = ("pe", "dve", "act", "pool", "sp")

    def __init__(self, nc):
        self.nc = nc
        self.ops = {e: [] for e in self.ENG}
        self.sem = {e: nc.alloc_semaphore("s_" + e) for e in self.ENG}
        self.cnt = {e: 0 for e in self.ENG}
        self.seen = {e: {} for e in self.ENG}
        self.state = {}
        self.dsem = {}
        self.alltoks = {}
        self.nops = 0
        self.max_ops = None
        self.cut_off = False

    def _skip(self):
        if self.cut_off or self.max_ops is None:
            return False
        self.nops += 1
        return self.nops > self.max_ops

    def _deps(self, r, w):
        toks = []
        for k in r:
            st = self.state.get(k)
            if st and st[0] is not None:
                toks.append(st[0])
        for k in w:
            st = self.state.get(k)
            if st:
                if st[0] is not None:
                    toks.append(st[0])
                toks.extend(st[1])
        return toks

    def _commit(self, tok, r, w):
        for k in r:
            st = self.state.setdefault(k, [None, []])
            st[1].append(tok)
            if len(st[1]) > 24:
                best = {}
                for t in st[1]:
                    if id(t[0]) not in best or best[id(t[0])][1] < t[1]:
                        best[id(t[0])] = t
                st[1] = list(best.values())
        for k in w:
            self.state[k] = [tok, []]
        self.alltoks[id(tok[0])] = tok

    def _waits(self, eng, toks, same_ok=False):
        best = {}
        for (s, v, e) in toks:
            if same_ok and e == eng:
                continue
            if self.seen[eng].get(id(s), 0) >= v:
                continue
            if id(s) not in best or best[id(s)][1] < v:
                best[id(s)] = (s, v)
        for (s, v) in best.values():
            self.seen[eng][id(s)] = v
        return list(best.values())

    def op(self, eng, fn, r=(), w=(), same_ok=False):
        if self._skip():
            return None
        toks = self._deps(r, w)
        waits = self._waits(eng, toks, same_ok)
        self.cnt[eng] += 1
        tok = (self.sem[eng], self.cnt[eng], eng)
        self.ops[eng].append((waits, fn, (self.sem[eng], 1)))
        self._commit(tok, r, w)
        return tok

    def dma(self, q, fn, r=(), w=(), sname=None):
        if self._skip():
            return None
        if sname not in self.dsem:
            self.dsem[sname] = [self.nc.alloc_semaphore("d_" + str(sname)), 0]
        d = self.dsem[sname]
        toks = [t for t in self._deps(r, w) if t[0] is not d[0]]
        waits = self._waits(q, toks)
        d[1] += 16
        tok = (d[0], d[1], "dma")
        self.ops[q].append((waits, fn, (d[0], 16)))
        self._commit(tok, r, w)
        return tok

    def wait_tok(self, eng, tok):
        waits = self._waits(eng, [tok])
        if waits:
            self.ops[eng].append((waits, None, None))

    def barrier(self):
        toks = list(self.alltoks.values())
        for e in self.ENG:
            waits = self._waits(e, toks, same_ok=False)
            if waits:
                self.ops[e].append((waits, None, None))
        self.state = {}

    def new_epoch(self):
        self.barrier()
        old = dict(self.sem)
        self.epoch = getattr(self, "epoch", 0) + 1
        for e in self.ENG:
            self.sem[e] = self.nc.alloc_semaphore("s%d_%s" % (self.epoch, e))
            self.cnt[e] = 0
        for s_ in old.values():
            self.alltoks.pop(id(s_), None)
        self._old_sems = getattr(self, "_old_sems", []) + list(old.values())

    def replay(self):
        nc = self.nc
        engs = {"pe": "tensor", "dve": "vector", "act": "scalar", "pool": "gpsimd", "sp": "sync"}
        with nc.Block() as block:
            for e, attr in engs.items():
                lst = self.ops[e]

                def body(eng, lst=lst):
                    for waits, fn, inc in lst:
                        for (s, v) in waits:
                            eng.wait_ge(s, v)
                        if fn is not None:
                            ins = fn(eng)
                            ins.then_inc(inc[0], inc[1])
                getattr(block, attr)(body)


class Rot:
    def __init__(self, items):
        self.items = items
        self.i = 0

    def next(self):
        it = self.items[self.i % len(self.items)]
        self.i += 1
        return it


def make_consts():
    cb = np.zeros((128, NCB), np.float32)
    cb[:, CB_ID:CB_ID + 128] = np.eye(128)
    kk = np.arange(128)[:, None]
    qq = np.arange(128)[None, :]
    for h in range(4):
        cb[:, CB_IREP + h * 128:CB_IREP + (h + 1) * 128] = BIG * np.eye(128)
        cb[:, CB_TRI + h * 128:CB_TRI + (h + 1) * 128] = np.where(kk > qq, -BIG, 0.0)
        cb[:, CB_TRILO + h * 128:CB_TRILO + (h + 1) * 128] = np.where(kk <= qq, -BIG, 0.0)
        j = np.arange(8)[:, None]
        cb[0:8, CB_BND + h * 128:CB_BND + (h + 1) * 128] = np.where(qq >= 16 * j + 15, 0.0, -BIG)
    for j in range(8):
        cb[j, CB_WD + j + 120] = 1.0
    cb[1:128, CB_VAUGC] = 1.0
    for cp in range(1, 128):
        c = cp - 1
        for jj in range(32):
            if 16 * c < 64 * jj + 64 and 16 * c + 32 > 64 * jj:
                cb[cp, CB_VAUGC + 2 + jj] = 1.0
    cb[:, CB_ONES:CB_ONES + 128] = 1.0
    cf = np.zeros((128, NCF), np.float32)
    q = np.arange(128)[:, None]
    hq = (q >= 64).astype(np.int64)
    x = np.arange(64)[None, :]
    dj = x - 32
    fm = np.zeros((128, 64), np.float32)
    fm[(dj == hq) | (dj == hq - 1)] = 1e4
    fm[dj > hq] = NEG
    cf[:, CF_FM:CF_FM + 64] = fm
    cf[:, CF_F0] = 1e4
    cf[:, CF_TRIQK:CF_TRIQK + 128] = np.where(np.arange(128)[None, :] > q, NEG, 0.0)
    cf[:, CF_ID:CF_ID + 128] = np.eye(128)
    return cb, cf


def pack_vecs(inp):
    v = np.zeros((128, NV), np.float32)

    def put(off, vec):
        vec = np.asarray(vec, np.float32).reshape(-1, 128)
        v[:, off:off + vec.shape[0]] = vec.T
    put(V_NMIX0, inp["l0_norm_mix"]); put(V_NFFN0, inp["l0_norm_ffn"])
    put(V_NMIX1, inp["l1_norm_mix"]); put(V_NFFN1, inp["l1_norm_ffn"]); put(V_NFIN, inp["final_norm"])
    put(V_MODB0, inp["l0_mod_b"]); put(V_MODB1, inp["l1_mod_b"])
    cw = np.asarray(inp["l0_conv_w"], np.float32)
    for c in range(4):
        v[:, V_CONVW + c * 4:V_CONVW + c * 4 + 4] = cw[:, c * 128:(c + 1) * 128].T
    put(V_CONVB, inp["l0_conv_b"]); put(V_BA, inp["l0_lru_ba"]); put(V_BX, inp["l0_lru_bx"]); put(V_LAM, inp["l0_lru_lambda"])
    pet = np.zeros((64, 66), np.float32)
    pet[:, 0:32] = np.asarray(inp["l0_cmp_pe_k"], np.float32).T
    pet[:, 32:64] = np.asarray(inp["l0_cmp_pe_v"], np.float32).T
    return v, pet


WEIGHTS = [("l0_mod_w", [D, 6 * D]), ("l1_mod_w", [D, 6 * D]), ("l0_w_in", [D, IN0]), ("l1_w_in", [D, IN1]),
           ("l0_w_out", [D, D]), ("l1_w_out", [D, D]),
           ("l0_ffn_wg", [D, DFF]), ("l0_ffn_wu", [D, DFF]), ("l0_ffn_wd", [DFF, D]),
           ("l1_ffn_wg", [D, DFF]), ("l1_ffn_wu", [D, DFF]), ("l1_ffn_wd", [DFF, D]),
           ("l0_lru_wa", [8, 64, 64]), ("l0_lru_wx", [8, 64, 64]),
           ("l0_cmp_w1_k", [2048, 128]), ("l0_cmp_w2_k", [128, 64]), ("l0_cmp_w1_v", [2048, 128]), ("l0_cmp_w2_v", [128, 64])]


def build_program(stop_after=None, nseq=2, ntg_run=NTG, dbg_yc=False, layers=(0, 1), max_ops=None):
    nc = bass.Bass("TRN2", target_bir_lowering=False)
    S = Sched(nc)
    S.max_ops = max_ops
    dr = {}
    for name, shp in WEIGHTS:
        dr[name] = nc.dram_tensor(name, shp, F32, kind="ExternalInput").ap()
    xT_d = nc.dram_tensor("xT", [2, 128, KC, SEQ], F32, kind="ExternalInput").ap()
    cT_d = nc.dram_tensor("cT", [128, KC, 2], F32, kind="ExternalInput").ap()
    vecs_d = nc.dram_tensor("vecs", [128, NV], F32, kind="ExternalInput").ap()
    pet_d = nc.dram_tensor("pet", [64, 66], F32, kind="ExternalInput").ap()
    cb_d = nc.dram_tensor("cb", [128, NCB], F32, kind="ExternalInput").ap()
    cf_d = nc.dram_tensor("cf", [128, NCF], F32, kind="ExternalInput").ap()
    out_d = nc.dram_tensor("outT", [2, 128, KC, SEQ], F32, kind="ExternalOutput").ap()

    def sb(name, shape, dt=F32):
        return nc.alloc_sbuf_tensor(name, list(shape), dt).ap()

    xT = sb("xT_sb", [128, KC, SEQ])
    vecs = sb("vecs_sb", [128, NV])
    cb = sb("cb_sb", [128, NCB], BF16)
    cf = sb("cf_sb", [128, NCF])
    pet = sb("pet_sb", [64, 66], BF16)
    mod = [sb("mod%d" % l, [128, 48, 2]) for l in range(2)]
    gsm = [sb("gsm%d" % l, [128, KC, 2]) for l in range(2)]
    gsf = [sb("gsf%d" % l, [128, KC, 2]) for l in range(2)]
    siluc = sb("siluc", [128, KC, 2])
    lruc = sb("lruc", [128, 24])
    c_one = sb("c_one", [128, 1]); c_eps = sb("c_eps", [128, 1])
    ps = [nc.alloc_psum_tensor("ps%d" % i, [128, 512], F32).ap() for i in range(8)]
    PK = [("ps", i) for i in range(8)]

    ident = cb[:, CB_ID:CB_ID + 128]
    irep = cb[:, CB_IREP:CB_IREP + 512]
    trineg = cb[:, CB_TRI:CB_TRI + 512]
    trilo = cb[:, CB_TRILO:CB_TRILO + 512]
    bnd = cb[0:8, CB_BND:CB_BND + 512]
    wd = cb[0:8, CB_WD:CB_WD + 128]
    onesb = cb[:, CB_ONES:CB_ONES + 128]
    identf = cf[:, CF_ID:CF_ID + 128]
    triqk = cf[:, CF_TRIQK:CF_TRIQK + 128]

    OV_BYTES = nc.sbuf_bytes_remaining - 2048
    ov = nc.alloc_sbuf_tensor("ov", [128, OV_BYTES // 2], BF16).ap()
    ovf = ov.bitcast(F32)

    class Carver:
        def __init__(self):
            self.off = 0

        def get(self, shape, dt=F32, parts=128):
            n = int(np.prod(shape[1:]))
            esz = 4 if dt == F32 else 2
            self.off = (self.off + 31) // 32 * 32
            o = self.off
            self.off += n * esz
            assert self.off <= OV_BYTES, ("overlay overflow", self.off, OV_BYTES)
            base = ovf if dt == F32 else ov
            a = base[0:shape[0], o // esz:o // esz + n]
            if len(shape) == 3:
                a = a.rearrange("p (a b) -> p a b", b=shape[2])
            elif len(shape) == 4:
                a = a.rearrange("p (a b c) -> p a b c", b=shape[2], c=shape[3])
            return a

    def mm(out, lhsT, rhs, start, stop, r, w):
        S.op("pe", lambda e: e.matmul(out, lhsT=lhsT, rhs=rhs, start=start, stop=stop), r=r, w=w, same_ok=True)

    def act(out, in_, func, r, w, bias=None, scale=None, accum=None):
        kw = {}
        if bias is not None:
            kw["bias"] = bias
        if scale is not None:
            kw["scale"] = scale
        if accum is not None:
            kw["accum_out"] = accum
        S.op("act", lambda e: e.activation(out=out, in_=in_, func=func, **kw), r=r, w=w)

    def ts(eng, out, in0, s1, s2, op0, op1, r, w, accum=None):
        kw = {}
        if accum is not None:
            kw["accum_out"] = accum
        if op1 is None:
            S.op(eng, lambda e: e.tensor_scalar(out=out, in0=in0, scalar1=s1, scalar2=None, op0=op0, **kw), r=r, w=w)
        else:
            S.op(eng, lambda e: e.tensor_scalar(out=out, in0=in0, scalar1=s1, scalar2=s2, op0=op0, op1=op1, **kw), r=r, w=w)

    def tt(eng, out, in0, in1, op, r, w):
        S.op(eng, lambda e: e.tensor_tensor(out=out, in0=in0, in1=in1, op=op), r=r, w=w)

    def stt(out, in0, scalar, in1, op0, op1, r, w):
        S.op("dve", lambda e: e.scalar_tensor_tensor(out=out, in0=in0, scalar=scalar, in1=in1, op0=op0, op1=op1), r=r, w=w)

    def cp(eng, out, in_, r, w):
        if eng == "act":
            S.op("act", lambda e: e.copy(out=out, in_=in_), r=r, w=w)
        else:
            S.op(eng, lambda e: e.tensor_copy(out=out, in_=in_), r=r, w=w)

    def memset(eng, ap, val, w):
        S.op(eng, lambda e: e.memset(ap, val), w=w)

    def recip(out, in_, r, w):
        S.op("dve", lambda e: e.reciprocal(out=out, in_=in_), r=r, w=w)

    def treduce(out, in_, op, r, w):
        S.op("dve", lambda e: e.tensor_reduce(out=out, in_=in_, axis=AX.X, op=op), r=r, w=w)

    def max8(out, in_, r, w):
        S.op("dve", lambda e: e.max(out=out, in_=in_), r=r, w=w)

    def transp(out, in_, idn, r, w):
        S.op("pe", lambda e: e.transpose(out, in_, idn), r=r, w=w, same_ok=True)

    def scan(out, d0, d1, init, r, w):
        S.op("dve", lambda e: e.tensor_tensor_scan(out=out, data0=d0, data1=d1, initial=init, op0=ALU.mult, op1=ALU.add), r=r, w=w)

    wprev = [None]

    def wdma(out, in_, w, sname):
        if wprev[0] is not None:
            S.wait_tok("pool", wprev[0])
        t_ = S.dma("pool", lambda e: e.dma_start(out=out, in_=in_), w=w, sname=sname)
        if t_ is not None:
            wprev[0] = t_

    def gelu_inplace(z, t, r_keys, zk, tk):
        tt("pool", t, z, z, ALU.mult, r=[zk], w=[tk])
        ts("pool", t, t, 0.044715, 1.0, ALU.mult, ALU.add, r=[tk], w=[tk])
        tt("pool", t, t, z, ALU.mult, r=[tk, zk], w=[tk])
        act(t, t, AF.Exp, r=[tk], w=[tk], scale=-1.5957691216)
        ts("dve", t, t, 1.0, None, ALU.add, None, r=[tk], w=[tk])
        recip(t, t, r=[tk], w=[tk])
        tt("pool", t, t, z, ALU.mult, r=[tk, zk], w=[tk])

    S.dma("sp", lambda e: e.dma_start(out=vecs, in_=vecs_d), w=["vecs"], sname="vecs")
    S.dma("sp", lambda e: e.dma_start(out=cf, in_=cf_d), w=["cf"], sname="cf")
    S.dma("sp", lambda e: e.dma_start(out=siluc, in_=cT_d), w=["siluc"], sname="siluc")
    wdma(cb, cb_d, ["cb"], "cb")
    wdma(pet, pet_d, ["pet"], "pet")
    memset("dve", c_one, 1.0, ["c_one"])
    memset("dve", c_eps, EPS, ["c_eps"])
    act(siluc, siluc, AF.Silu, r=["siluc"], w=["siluc"])
    act(lruc[:, 8:12], vecs[:, V_LAM:V_LAM + 4], AF.Exp, r=["vecs"], w=["lruc"], scale=-1.0)
    act(lruc[:, 8:12], lruc[:, 8:12], AF.Ln, r=["lruc", "c_one"], w=["lruc"], bias=c_one[:, 0:1])
    ts("dve", lruc[:, 0:4], lruc[:, 8:12], -8.0, None, ALU.mult, None, r=["lruc"], w=["lruc"])
    ts("dve", lruc[:, 4:8], lruc[:, 8:12], -16.0, None, ALU.mult, None, r=["lruc"], w=["lruc"])
    ts("dve", lruc[:, 12:16], vecs[:, V_BA:V_BA + 4], -1.0, None, ALU.mult, None, r=["vecs", "lruc"], w=["lruc"])
    ts("dve", lruc[:, 16:20], vecs[:, V_BX:V_BX + 4], -1.0, None, ALU.mult, None, r=["vecs", "lruc"], w=["lruc"])

    NG = 768
    C0 = Carver()
    stg = [C0.get([128, KC, NG]) for i in range(2)]
    gi = 0
    bankrot = Rot([0, 1, 2, 3, 4, 5, 6, 7])
    for l in range(2):
        mw = dr["l%d_mod_w" % l].rearrange("(k p) n -> p k n", p=128)
        vb = V_MODB0 if l == 0 else V_MODB1
        for j in range(6 * D // NG):
            st = stg[gi % 2]
            sk = "modstg%d" % (gi % 2)
            S.dma("sp", lambda e, st=st, j=j, mw=mw: e.dma_start(out=st, in_=mw[:, :, j * NG:(j + 1) * NG]), w=[sk], sname=sk)
            gi += 1
            for nn in range(NG // 128):
                b = bankrot.next()
                col = j * (NG // 128) + nn
                for k in range(KC):
                    mm(ps[b][:, 0:2], st[:, k, nn * 128:(nn + 1) * 128], siluc[:, k, :], k == 0, k == KC - 1,
                       r=[sk, "siluc"], w=[PK[b]])
                ts("dve", mod[l][:, col, :], ps[b][:, 0:2], vecs[:, vb + col:vb + col + 1], None, ALU.add, None,
                   r=[PK[b], "vecs"], w=["mod%d" % l])
        nm = V_NMIX0 if l == 0 else V_NMIX1
        nf = V_NFFN0 if l == 0 else V_NFFN1
        for s in range(2):
            stt(gsm[l][:, :, s], mod[l][:, 8:16, s], 1.0, vecs[:, nm:nm + 8], ALU.add, ALU.mult, r=["mod%d" % l, "vecs"], w=["gs%d" % l])
            stt(gsf[l][:, :, s], mod[l][:, 32:40, s], 1.0, vecs[:, nf:nf + 8], ALU.add, ALU.mult, r=["mod%d" % l, "vecs"], w=["gs%d" % l])

    def norm_group(tg, gs_ap, sh_ap, hT_out, hkey, tmp_sq, tmp_f, rstd, rkeys, scale_only=False, width=TG):
        c0 = tg * width
        xk = [("xT", t) for t in range(c0 // TG, (c0 + width) // TG)]
        b = bankrot.next()
        for k in range(KC):
            sq = tmp_sq[k % 2]
            act(sq, xT[:, k, c0:c0 + width], AF.Square, r=xk + rkeys, w=[("nsq", k % 2)])
            mm(ps[b][:, 0:width], onesb, sq, k == 0, k == KC - 1, r=[("nsq", k % 2), "cb"], w=[PK[b]])
        act(rstd, ps[b][:, 0:width], AF.Ln, r=[PK[b], "c_eps"], w=["rstd"], bias=c_eps[:, 0:1], scale=1.0 / D)
        act(rstd, rstd, AF.Exp, r=["rstd"], w=["rstd"], scale=-0.5)
        for k in range(KC):
            tf = tmp_f[k % 2]
            stt(tf, xT[:, k, c0:c0 + width], gs_ap[:, k:k + 1], rstd, ALU.mult, ALU.mult,
                r=xk + ["rstd"] + rkeys, w=[("ntf", k % 2)])
            if sh_ap is not None:
                act(hT_out[:, k, :], tf, AF.Identity, r=[("ntf", k % 2)] + rkeys, w=[hkey], bias=sh_ap[:, k:k + 1])
            else:
                cp("act", hT_out[:, k, :], tf, r=[("ntf", k % 2)], w=[hkey])

    dbg = {}

    for s in range(nseq):
        if s > 0:
            S.new_epoch()
        for k in range(KC):
            S.dma("sp", lambda e, k=k, s=s: e.dma_start(out=xT[:, k, :], in_=xT_d[s, :, k, :]),
                  w=[("xT", t) for t in range(NTG)], sname="xT%d" % k)

        for layer in layers:
            if stop_after == ("p0",):
                break
            S.barrier()
            C = Carver()
            nin = IN0 if layer == 0 else IN1
            WA = C.get([128, KC, nin], BF16)
            WB = C.get([128, KC, D], BF16)
            win_d = dr["l%d_w_in" % layer].rearrange("(k p) n -> p k n", p=128)
            for k in range(KC):
                wdma(WA[:, k, :], win_d[:, k, :], ["WA"], "WA")
            wout_d = dr["l%d_w_out" % layer].rearrange("(k p) n -> p k n", p=128)
            for k in range(0, KC, 4):
                wdma(WB[:, k:k + 4, :], wout_d[:, k:k + 4, :], ["WB"], "WB")
            hTg = C.get([128, KC, TG], BF16)
            nsq = [C.get([128, TG], BF16) for _ in range(2)]
            ntf = [C.get([128, TG]) for _ in range(2)]
            rstd = C.get([128, TG])
            yT = C.get([128, KC, TG], BF16)
            pT = [C.get([128, 512], BF16) for _ in range(3)]
            pTrot = Rot([0, 1, 2])
            ytok = C.get([128, 512 if layer == 0 else 1024])
            gm_ap = mod[layer][:, 16:24, s]
            shm_ap = mod[layer][:, 0:8, s]
            SB = Rot([0, 1, 2])
            AB = Rot([3, 4])
            MB = Rot([5, 6, 7])

            def outproj(tg):
                if dbg_yc == "l1_yc" and layer == 1:
                    cp("dve", xT[:, :, tg * TG:(tg + 1) * TG], yT, r=["yT", ("xT", tg)], w=[("xT", tg)])
                    return
                for ko in range(KC):
                    b = MB.next()
                    for kf in range(KC):
                        mm(ps[b][:, 0:TG], WB[:, kf, ko * 128:(ko + 1) * 128], yT[:, kf, :], kf == 0, kf == KC - 1,
                           r=["WB", "yT", "yTa", "yTb"], w=[PK[b]])
                    stt(xT[:, ko, tg * TG:(tg + 1) * TG], ps[b][:, 0:TG], gm_ap[:, ko:ko + 1], xT[:, ko, tg * TG:(tg + 1) * TG],
                        ALU.mult, ALU.add, r=[PK[b], "mod%d" % layer, ("xT", tg)], w=[("xT", tg)])

            if layer == 0:
                W1 = [C.get([64, 32, 128], BF16) for _ in range(2)]
                W2 = [C.get([128, 64], BF16) for _ in range(2)]
                BD = [C.get([128, 4, 128], BF16) for _ in range(2)]
                for i, nm_ in enumerate(("k", "v")):
                    w1d = dr["l0_cmp_w1_" + nm_].rearrange("(l d) m -> d l m", d=64)
                    for l0_ in range(0, 32, 4):
                        wdma(W1[i][:, l0_:l0_ + 4, :], w1d[:, l0_:l0_ + 4, :], ["W1%d" % i], "W1%d" % i)
                    wdma(W2[i], dr["l0_cmp_w2_" + nm_], ["W2%d" % i], "W2%d" % i)
                for i, nm_ in enumerate(("wa", "wx")):
                    memset("pool", BD[i], 0.0, ["BD%d" % i])
                    for c in range(4):
                        wdma(BD[i][0:64, c, 0:64], dr["l0_lru_" + nm_][2 * c], ["BD%d" % i], "BD%d" % i)
                        wdma(BD[i][64:128, c, 64:128], dr["l0_lru_" + nm_][2 * c + 1], ["BD%d" % i], "BD%d" % i)
                ksT = C.get([128, 2, SEQ], BF16)
                kwT = C.get([128, 2, 6 * 128], BF16)
                VsA = C.get([128, 16, 2, 66], BF16)
                VwA = C.get([128, 6, 2, 66], BF16)
                kcR = [C.get([64, 2, 16, 17], BF16) for _ in range(2)]
                KcT = C.get([128, 2, 128], BF16)
                VcT = C.get([64, 2, 128])
                VcA = C.get([128, 2, 98], BF16)
                hid = C.get([128, 16]); hidt = C.get([128, 16]); hidb = C.get([128, 16], BF16)
                pebias = C.get([128, 2])
                qT = C.get([128, 8, TG], BF16)
                sig = C.get([128, TPG, 24]); gtsraw = C.get([128, TPG, 24])
                AXb = C.get([128, 4, TG + 3])
                agf = C.get([128, TG]); gt = C.get([128, TG]); xc = C.get([128, TG]); xcb = C.get([128, TG], BF16)
                rr = C.get([128, TG]); ii = C.get([128, TG]); aa = C.get([128, TG])
                carry = C.get([128, 4])
                negE = [C.get([128, 32, 64], BF16) for _ in range(2)]
                impn = C.get([128, 4, 32]); imp = C.get([128, 32]); top8 = C.get([128, 8]); negsel = C.get([128, 32], BF16)
                rec = C.get([128, 4]); coef = C.get([128, 4]); tmpo = C.get([128, 4, 64])

                memset("dve", VsA, 0.0, [("VsA", t) for t in range(NTG)])
                memset("dve", VwA, 0.0, ["VwA"])
                for t_ in range(16):
                    memset("dve", VsA[:, t_, :, 64:65], 1.0, [("VsA", t_ // TPG)])
                for t_ in range(6):
                    memset("dve", VwA[:, t_, :, 64:65], 1.0, ["VwA"])
                memset("dve", KcT, 0.0, ["KcT"])
                memset("dve", qT, 0.0, ["qT"])
                memset("pool", ksT, 0.0, [("ksT", t) for t in range(NTG)])
                memset("pool", kwT, 0.0, [("kwT", t) for t in range(6)])
                memset("dve", VcT, 0.0, ["VcT"])
                memset("dve", AXb, 0.0, ["AXb"])
                for i in range(2):
                    memset("pool", kcR[i], 0.0, ["kcR%d" % i])
                for g in range(2):
                    cp("pool", VcA[:, g, 64:98], cb[:, CB_VAUGC:CB_VAUGC + 34], r=["cb"], w=["VcA"])
                for i in range(2):
                    b = MB.next()
                    for l in range(32):
                        mm(ps[b][:, 0:2], W1[i][:, l, :], pet[:, 32 * i + l:32 * i + l + 2],
                           l == 0, l == 31, r=["W1%d" % i, "pet"], w=[PK[b]])
                    cp("dve", pebias[:, i:i + 1], ps[b][:, 0:1], r=[PK[b]], w=["pebias"])

                LOOK = 2

                def emit_pv0(item, ab, nkt):
                    pi, vrhs, vkey, n_ = item
                    for h in range(4):
                        mm(ps[ab][:, h * 66:(h + 1) * 66], pT[pi][:, h * 128:(h + 1) * 128], vrhs,
                           n_ == 0 and h == 0, n_ == nkt - 1 and h == 3, r=[("pT", pi), vkey], w=[PK[ab]])

                for tg in range(ntg_run):
                    t0 = tg * TG
                    norm_group(tg, gsm[0][:, :, s], shm_ap, hTg, "hTg", nsq, ntf, rstd, ["gs0", "mod0"])
                    if dbg_yc == "l0_h":
                        cp("dve", xT[:, :, tg * TG:(tg + 1) * TG], hTg, r=["hTg", ("xT", tg)], w=[("xT", tg)])
                        continue
                    def lru_gen(tg=tg):
                        for c in range(4):
                            bg = MB.next()
                            for k in range(KC):
                                mm(ps[bg][:, 0:TG], WA[:, k, c * 128:(c + 1) * 128], hTg[:, k, :], k == 0, k == KC - 1, r=["WA", "hTg"], w=[PK[bg]])
                            cp("act", agf, ps[bg][:, 0:TG], r=[PK[bg]], w=["agf"])
                            yield
                            bx_ = MB.next()
                            for k in range(KC):
                                mm(ps[bx_][:, 0:TG], WA[:, k, 512 + c * 128:512 + (c + 1) * 128], hTg[:, k, :], k == 0, k == KC - 1, r=["WA", "hTg"], w=[PK[bx_]])
                            cp("act", AXb[:, c, 3:3 + TG], ps[bx_][:, 0:TG], r=[PK[bx_]], w=["AXb"])
                            yield
                            cw = V_CONVW + c * 4
                            ts("dve", xc, AXb[:, c, 0:TG], vecs[:, cw:cw + 1], vecs[:, V_CONVB + c:V_CONVB + c + 1], ALU.mult, ALU.add, r=["AXb", "vecs"], w=["xc"])
                            yield
                            for kk_ in range(1, 4):
                                stt(xc, AXb[:, c, kk_:kk_ + TG], vecs[:, cw + kk_:cw + kk_ + 1], xc, ALU.mult, ALU.add, r=["AXb", "vecs", "xc"], w=["xc"])
                                yield
                            cp("pool", AXb[:, c, 0:3], AXb[:, c, TG:TG + 3], r=["AXb"], w=["AXb"])
                            cp("act", xcb, xc, r=["xc"], w=["xcb"])
                            yield
                            br = MB.next()
                            mm(ps[br][:, 0:TG], BD[0][:, c, :], xcb, True, True, r=["BD0", "xcb"], w=[PK[br]])
                            bi = MB.next()
                            mm(ps[bi][:, 0:TG], BD[1][:, c, :], xcb, True, True, r=["BD1", "xcb"], w=[PK[bi]])
                            yield
                            act(rr, ps[br][:, 0:TG], AF.Exp, r=[PK[br], "lruc"], w=["rr"], bias=lruc[:, 12 + c:13 + c], scale=-1.0)
                            act(ii, ps[bi][:, 0:TG], AF.Exp, r=[PK[bi], "lruc"], w=["ii"], bias=lruc[:, 16 + c:17 + c], scale=-1.0)
                            yield
                            act(rr, rr, AF.Ln, r=["rr", "c_one"], w=["rr"], bias=c_one[:, 0:1])
                            yield
                            act(rr, rr, AF.Exp, r=["rr"], w=["rr"], scale=-1.0)
                            yield
                            act(ii, ii, AF.Ln, r=["ii", "c_one"], w=["ii"], bias=c_one[:, 0:1])
                            yield
                            act(ii, ii, AF.Exp, r=["ii"], w=["ii"], scale=-1.0)
                            yield
                            act(aa, rr, AF.Exp, r=["rr", "lruc"], w=["aa"], scale=lruc[:, c:c + 1])
                            yield
                            act(rr, rr, AF.Exp, r=["rr", "lruc"], w=["rr"], scale=lruc[:, 4 + c:5 + c])
                            yield
                            ts("dve", rr, rr, -1.0, 1.0, ALU.mult, ALU.add, r=["rr"], w=["rr"])
                            yield
                            ts("dve", rr, rr, 1e-18, None, ALU.max, None, r=["rr"], w=["rr"])
                            yield
                            act(rr, rr, AF.Ln, r=["rr"], w=["rr"])
                            yield
                            act(rr, rr, AF.Exp, r=["rr"], w=["rr"], scale=0.5)
                            yield
                            if tg == 0:
                                memset("dve", rr[:, 0:1], 1.0, ["rr"])
                            tt("dve", ii, ii, xc, ALU.mult, r=["ii", "xc"], w=["ii"])
                            yield
                            tt("dve", ii, ii, rr, ALU.mult, r=["ii", "rr"], w=["ii"])
                            yield
                            init = 0.0 if tg == 0 else carry[:, c:c + 1]
                            scan(xc, aa, ii, init, r=["aa", "ii", "carry"], w=["xc"])
                            yield
                            cp("dve", carry[:, c:c + 1], xc[:, TG - 1:TG], r=["xc"], w=["carry"])
                            tt("pool", gt, agf, agf, ALU.mult, r=["agf"], w=["gt"])
                            yield
                            ts("pool", gt, gt, 0.044715, 1.0, ALU.mult, ALU.add, r=["gt"], w=["gt"])
                            yield
                            tt("pool", gt, gt, agf, ALU.mult, r=["gt", "agf"], w=["gt"])
                            yield
                            act(gt, gt, AF.Exp, r=["gt"], w=["gt"], scale=-1.5957691216)
                            yield
                            act(gt, gt, AF.Ln, r=["gt", "c_one"], w=["gt"], bias=c_one[:, 0:1])
                            yield
                            act(gt, gt, AF.Exp, r=["gt"], w=["gt"], scale=-1.0)
                            yield
                            tt("pool", gt, gt, agf, ALU.mult, r=["gt", "agf"], w=["gt"])
                            yield
                            tt("pool", yT[:, c, :], xc, gt, ALU.mult, r=["xc", "gt"], w=["yTa"])
                            yield

                    lgen = lru_gen()
                    nsteps_ = 0
                    for j_ in range(TPG):
                        ib_ = tg * TPG + j_
                        nsteps_ += 2 * (1 + len([kt for kt in range(ib_ - 4, ib_ + 1) if kt >= 0]) + ib_ + 1)
                    per_step = -(-26 * 4 // max(nsteps_ - 2, 1))

                    def pump(n):
                        for _ in range(n):
                            try:
                                next(lgen)
                            except StopIteration:
                                return
                    for h in range(8):
                        b = MB.next()
                        for k in range(KC):
                            mm(ps[b][0:64, 0:TG], WA[:, k, 1024 + h * 64:1024 + (h + 1) * 64], hTg[:, k, :], k == 0, k == KC - 1, r=["WA", "hTg"], w=[PK[b]])
                        cp("act" if h % 2 == 0 else "dve", qT[0:64, h, :], ps[b][0:64, 0:TG], r=[PK[b]], w=["qT"])
                    for which, cbase in (("kc", 1536), ("vc", 1664), ("ks", 1792), ("kw", 2048)):
                        for g in range(2):
                            b = MB.next()
                            for k in range(KC):
                                mm(ps[b][0:64, 0:TG], WA[:, k, cbase + g * 64:cbase + (g + 1) * 64], hTg[:, k, :], k == 0, k == KC - 1, r=["WA", "hTg"], w=[PK[b]])
                            if which == "ks":
                                cp("act", ksT[0:64, g, t0:t0 + TG], ps[b][0:64, 0:TG], r=[PK[b]], w=[("ksT", tg)])
                            elif which == "kw":
                                for j in range(TPG):
                                    kt = tg * TPG + j
                                    sl = kt % 6
                                    cp("dve", kwT[0:64, g, sl * 128:(sl + 1) * 128], ps[b][0:64, j * 128:(j + 1) * 128], r=[PK[b]], w=[("kwT", sl)])
                            else:
                                i = 0 if which == "kc" else 1
                                if g == 0:
                                    if tg > 0:
                                        cp("pool", kcR[i][:, :, :, 0:1], kcR[i][:, :, :, 16:17], r=["kcR%d" % i], w=["kcR%d" % i])
                                cp("act", kcR[i][:, g, :, 1:17], ps[b][0:64, 0:TG].rearrange("p (b r) -> p r b", r=16), r=[PK[b]], w=["kcR%d" % i])
                    for j in range(TPG):
                        kt = tg * TPG + j
                        b = MB.next()
                        for k in range(KC):
                            mm(ps[b][:, 0:128], hTg[:, k, j * 128:(j + 1) * 128], WA[:, k, 1920:2048], k == 0, k == KC - 1, r=["WA", "hTg"], w=[PK[b]])
                        cp("act", VsA[:, kt, :, 0:64], ps[b][:, 0:128].rearrange("p (g d) -> p g d", d=64), r=[PK[b]], w=[("VsA", tg)])
                        b = MB.next()
                        for k in range(KC):
                            mm(ps[b][:, 0:152], hTg[:, k, j * 128:(j + 1) * 128], WA[:, k, 2176:2328], k == 0, k == KC - 1, r=["WA", "hTg"], w=[PK[b]])
                        cp("dve", VwA[:, kt % 6, :, 0:64], ps[b][:, 0:128].rearrange("p (g d) -> p g d", d=64), r=[PK[b]], w=[("VwA", kt % 6)])
                        cp("dve", gtsraw[:, j, :], ps[b][:, 128:152], r=[PK[b]], w=["gtsraw"])
                    act(sig, gtsraw, AF.Exp, r=["gtsraw"], w=["sig"], scale=-1.0)
                    ts("dve", sig, sig, 1.0, None, ALU.add, None, r=["sig"], w=["sig"])
                    recip(sig, sig, r=["sig"], w=["sig"])
                    for i in range(2):
                        for g in range(2):
                            b = MB.next()
                            for l in range(32):
                                mm(ps[b][:, 0:16], W1[i][:, l, :], kcR[i][:, g, l % 16, (l // 16):(l // 16) + 16], l == 0, l == 31,
                                   r=["W1%d" % i, "kcR%d" % i], w=[PK[b]])
                            act(hid, ps[b][:, 0:16], AF.Identity, r=[PK[b], "pebias"], w=["hid"], bias=pebias[:, i:i + 1])
                            gelu_inplace(hid, hidt, None, "hid", "hidt")
                            cp("pool", hidb, hidt, r=["hidt"], w=["hidb"])
                            b2 = MB.next()
                            mm(ps[b2][0:64, 0:16], W2[i], hidb, True, True, r=["W2%d" % i, "hidb"], w=[PK[b2]])
                            if i == 0:
                                cp("act", KcT[0:64, g, 16 * tg:16 * tg + 16], ps[b2][0:64, 0:16], r=[PK[b2]], w=["KcT"])
                            else:
                                cp("act", VcT[:, g, 16 * tg:16 * tg + 16], ps[b2][0:64, 0:16], r=[PK[b2]], w=["VcT"])
                    if tg == 0:
                        memset("dve", KcT[0:64, :, 0:1], 0.0, ["KcT"])
                        memset("dve", VcT[:, :, 0:1], 0.0, ["VcT"])
                    for g in range(2):
                        b = MB.next()
                        transp(ps[b][:, 0:64], VcT[:, g, :], identf[0:64, 0:64], r=["VcT", "cf"], w=[PK[b]])
                        cp("act", VcA[:, g, 0:64], ps[b][:, 0:64], r=[PK[b]], w=["VcA"])

                    for j in range(TPG):
                        ib = tg * TPG + j
                        qc = slice(j * 128, (j + 1) * 128)
                        for g in range(2):
                            qrhs = qT[:, 4 * g:4 * g + 4, qc]
                            yv = ytok[:, g * 256:(g + 1) * 256].rearrange("p (h d) -> p h d", d=64)
                            M = 8 * (ib + 1)
                            sbk = SB.next()
                            mm(ps[sbk][0:M, :], KcT[:, g, 0:M], qrhs, True, False, r=["KcT", "qT"], w=[PK[sbk]])
                            s0 = 120 - 8 * ib
                            mm(ps[sbk][0:M, :], wd[:, s0:s0 + M], bnd, False, True, r=["cb"], w=[PK[sbk]])
                            pi = pTrot.next()
                            act(pT[pi][0:M, :], ps[sbk][0:M, :], AF.Exp, r=[PK[sbk]], w=[("pT", pi)], scale=0.125)
                            ab = AB.next()
                            for h in range(4):
                                mm(ps[ab][:, h * 98:(h + 1) * 98], pT[pi][0:M, h * 128:(h + 1) * 128], VcA[0:M, g, :], h == 0, h == 3,
                                   r=[("pT", pi), "VcA"], w=[PK[ab]])
                            pump(per_step)
                            accv = ps[ab][:, 0:392].rearrange("p (h c) -> p h c", c=98)
                            ts("dve", rec, accv[:, :, 64], 1e-30, None, ALU.max, None, r=[PK[ab]], w=["rec"])
                            recip(rec, rec, r=["rec"], w=["rec"])
                            tt("dve", impn, accv[:, :, 66:98], rec.unsqueeze(2).to_broadcast([128, 4, 32]), ALU.mult, r=[PK[ab], "rec"], w=["impn"])
                            treduce(imp, impn.rearrange("p h j -> p j h"), ALU.add, r=["impn"], w=["imp"])
                            tt("dve", imp, imp, cf[:, CF_FM + 32 - 2 * ib:CF_FM + 64 - 2 * ib], ALU.add, r=["imp", "cf"], w=["imp"])
                            tt("dve", imp, imp, cf[:, CF_F0:CF_F0 + 32], ALU.add, r=["imp", "cf"], w=["imp"])
                            max8(top8, imp, r=["imp"], w=["top8"])
                            ts("dve", negsel, imp, top8[:, 7:8], 1.0, ALU.is_ge, ALU.subtract, r=["imp", "top8"], w=["negsel"])
                            cp("pool", negE[g], negsel.unsqueeze(2).to_broadcast([128, 32, 64]), r=["negsel"], w=[("negE", g)])
                            tt("dve", coef, rec, sig[:, j, 12 * g + 0:12 * g + 12:3], ALU.mult, r=["rec", "sig"], w=["coef"])
                            tt("dve", yv, accv[:, :, 0:64], coef.unsqueeze(2).to_broadcast([128, 4, 64]), ALU.mult, r=[PK[ab], "coef"], w=["ytok"])
                        for br_, gi_, g in (("win", 2, 0), ("win", 2, 1), ("slc", 1, 0), ("slc", 1, 1)):
                            qrhs = qT[:, 4 * g:4 * g + 4, qc]
                            yv = ytok[:, g * 256:(g + 1) * 256].rearrange("p (h d) -> p h d", d=64)
                            if True:
                                kts = [kt for kt in range(ib - 4, ib + 1) if kt >= 0] if br_ == "win" else list(range(ib + 1))
                                ab = AB.next()
                                pend = []
                                for n_, kt in enumerate(kts):
                                    sbk = SB.next()
                                    if br_ == "win":
                                        klhs = kwT[:, g, (kt % 6) * 128:(kt % 6 + 1) * 128]
                                        kkey = ("kwT", kt % 6)
                                        vrhs = VwA[:, kt % 6, g, :]
                                        vkey = ("VwA", kt % 6)
                                        extra = []
                                        if kt == ib:
                                            extra.append((ident, trineg, ["cb"]))
                                        if kt == ib - 4:
                                            extra.append((ident, trilo, ["cb"]))
                                    else:
                                        klhs = ksT[:, g, kt * 128:(kt + 1) * 128]
                                        kkey = ("ksT", kt // TPG)
                                        vrhs = VsA[:, kt, g, :]
                                        vkey = ("VsA", kt // TPG)
                                        extra = [(negE[g][:, 2 * kt:2 * kt + 2, :].rearrange("p a b -> p (a b)"), irep, [("negE", g), "cb"])]
                                        if kt == ib:
                                            extra.append((ident, trineg, ["cb"]))
                                    mm(ps[sbk][:, :], klhs, qrhs, True, len(extra) == 0, r=[kkey, "qT"], w=[PK[sbk]])
                                    for xi, (l_, r_, ks_) in enumerate(extra):
                                        mm(ps[sbk][:, :], l_, r_, False, xi == len(extra) - 1, r=ks_, w=[PK[sbk]])
                                    pi = pTrot.next()
                                    act(pT[pi], ps[sbk], AF.Exp, r=[PK[sbk]], w=[("pT", pi)], scale=0.125)
                                    pend.append((pi, vrhs, vkey, n_))
                                    if len(pend) > LOOK:
                                        emit_pv0(pend.pop(0), ab, len(kts))
                                    pump(per_step)
                                while pend:
                                    emit_pv0(pend.pop(0), ab, len(kts))
                                accw = ps[ab][:, 0:264].rearrange("p (h c) -> p h c", c=66)
                                ts("dve", rec, accw[:, :, 64], 1e-30, None, ALU.max, None, r=[PK[ab]], w=["rec"])
                                recip(rec, rec, r=["rec"], w=["rec"])
                                tt("dve", coef, rec, sig[:, j, 12 * g + gi_:12 * g + 12:3], ALU.mult, r=["rec", "sig"], w=["coef"])
                                tt("dve", tmpo, accw[:, :, 0:64], coef.unsqueeze(2).to_broadcast([128, 4, 64]), ALU.mult, r=[PK[ab], "coef"], w=["tmpo"])
                                tt("pool", yv, yv, tmpo, ALU.add, r=["ytok", "tmpo"], w=["ytok"])
                        b = MB.next()
                        for c4 in range(4):
                            transp(ps[b][:, c4 * 128:(c4 + 1) * 128], ytok[:, c4 * 128:(c4 + 1) * 128], identf, r=["ytok", "cf"], w=[PK[b]])
                        cp("act", yT[:, 4:8, qc], ps[b].rearrange("p (c q) -> p c q", q=128), r=[PK[b]], w=["yTb"])
                    pump(10 ** 6)
                    outproj(tg)
            else:
                kT = C.get([128, 2, SEQ], BF16)
                kiT = C.get([128, SEQ], BF16)
                VA = C.get([128, 16, 2, 66], BF16)
                qTs = [C.get([128, 16, TG], BF16) for _ in range(2)]
                qiTs = [C.get([128, 8, TG], BF16) for _ in range(2)]
                widxs = [C.get([128, TPG, 8]) for _ in range(2)]
                acc = C.get([128, SEQ])
                rl = [C.get([128, 512]) for _ in range(2)]
                negms = [C.get([128, SEQ], BF16) for _ in range(2)]
                lo = C.get([128, 1]); w0 = C.get([128, 1]); mid = C.get([128, 1]); cnt = C.get([128, 1]); pw = C.get([128, 1])
                junk = C.get([128, SEQ], BF16)
                rec8 = C.get([128, 8])
                memset("dve", VA, 0.0, [("VA", t) for t in range(NTG)])
                for t_ in range(16):
                    memset("dve", VA[:, t_, :, 64:65], 1.0, [("VA", t_ // TPG)])
                memset("pool", kT, 0.0, [("kT", t) for t in range(NTG)])
                memset("pool", kiT, 0.0, [("kiT", t) for t in range(NTG)])
                for i_ in range(2):
                    memset("dve", qTs[i_], 0.0, [("qT", i_)])
                    memset("pool", qiTs[i_], 0.0, [("qiT", i_)])

                def proj1(tg):
                    t0 = tg * TG
                    qT = qTs[tg % 2]; qiT = qiTs[tg % 2]; widx = widxs[tg % 2]
                    qk = ("qT", tg % 2); qik = ("qiT", tg % 2); wk = ("widx", tg % 2)
                    norm_group(tg, gsm[1][:, :, s], shm_ap, hTg, "hTg", nsq, ntf, rstd, ["gs1", "mod1"])
                    for h in range(8):
                        b = MB.next()
                        for k in range(KC):
                            mm(ps[b][0:64, 0:TG], WA[:, k, 1280 + h * 64:1280 + (h + 1) * 64], hTg[:, k, :], k == 0, k == KC - 1, r=["WA", "hTg"], w=[PK[b]])
                        cp("act" if h % 2 == 0 else "dve", qiT[0:64, h, :], ps[b][0:64, 0:TG], r=[PK[b]], w=[qik])
                    b = MB.next()
                    for k in range(KC):
                        mm(ps[b][0:64, 0:TG], WA[:, k, 1792:1856], hTg[:, k, :], k == 0, k == KC - 1, r=["WA", "hTg"], w=[PK[b]])
                    cp("act", kiT[0:64, t0:t0 + TG], ps[b][0:64, 0:TG], r=[PK[b]], w=[("kiT", tg)])
                    for j in range(TPG):
                        b = MB.next()
                        for k in range(KC):
                            mm(ps[b][:, 0:8], hTg[:, k, j * 128:(j + 1) * 128], WA[:, k, 1856:1864], k == 0, k == KC - 1, r=["WA", "hTg"], w=[PK[b]])
                        cp("dve", widx[:, j, :], ps[b][:, 0:8], r=[PK[b]], w=[wk])
                    for h in range(16):
                        b = MB.next()
                        for k in range(KC):
                            mm(ps[b][0:64, 0:TG], WA[:, k, h * 64:(h + 1) * 64], hTg[:, k, :], k == 0, k == KC - 1, r=["WA", "hTg"], w=[PK[b]])
                        cp("act" if h % 2 == 0 else "dve", qT[0:64, h, :], ps[b][0:64, 0:TG], r=[PK[b]], w=[qk])
                    for g in range(2):
                        b = MB.next()
                        for k in range(KC):
                            mm(ps[b][0:64, 0:TG], WA[:, k, 1024 + g * 64:1024 + (g + 1) * 64], hTg[:, k, :], k == 0, k == KC - 1, r=["WA", "hTg"], w=[PK[b]])
                        cp("act", kT[0:64, g, t0:t0 + TG], ps[b][0:64, 0:TG], r=[PK[b]], w=[("kT", tg)])
                    for j in range(TPG):
                        kt = tg * TPG + j
                        b = MB.next()
                        for k in range(KC):
                            mm(ps[b][:, 0:128], hTg[:, k, j * 128:(j + 1) * 128], WA[:, k, 1152:1280], k == 0, k == KC - 1, r=["WA", "hTg"], w=[PK[b]])
                        cp("act", VA[:, kt, :, 0:64], ps[b][:, 0:128].rearrange("p (g d) -> p g d", d=64), r=[PK[b]], w=[("VA", tg)])

                def stageA(ib):
                    tg = ib // TPG; j = ib % TPG
                    qiT = qiTs[tg % 2]; widx = widxs[tg % 2]; negm = negms[ib % 2]
                    qik = ("qiT", tg % 2); wk = ("widx", tg % 2); nk_ = ("negm", ib % 2)
                    qc = slice(j * 128, (j + 1) * 128)
                    nk = (ib + 1) * 128
                    for c0 in range(0, nk, 512):
                        wdt = min(512, nk - c0)
                        for h in range(8):
                            b = MB.next()
                            mm(ps[b][:, 0:wdt], qiT[:, h, qc], kiT[:, c0:c0 + wdt], True, True,
                               r=[qik] + [("kiT", t) for t in range(c0 // TG, (c0 + wdt - 1) // TG + 1)], w=[PK[b]])
                            ri = h % 2
                            act(rl[ri][:, 0:wdt], ps[b][:, 0:wdt], AF.Relu, r=[PK[b]], w=[("rl", ri)], scale=0.125)
                            if h == 0:
                                ts("dve", acc[:, c0:c0 + wdt], rl[ri][:, 0:wdt], widx[:, j, 0:1], None, ALU.mult, None, r=[("rl", ri), wk], w=["acc"])
                            else:
                                stt(acc[:, c0:c0 + wdt], rl[ri][:, 0:wdt], widx[:, j, h:h + 1], acc[:, c0:c0 + wdt], ALU.mult, ALU.add,
                                    r=[("rl", ri), wk, "acc"], w=["acc"])
                    tt("dve", acc[:, ib * 128:nk], acc[:, ib * 128:nk], triqk, ALU.add, r=["acc", "cf"], w=["acc"])
                    if ib >= 2:
                        treduce(lo, acc[:, 0:ib * 128], ALU.min, r=["acc"], w=["lo"])
                        treduce(w0, acc[:, 0:nk], ALU.max, r=["acc"], w=["w0"])
                        tt("dve", w0, w0, lo, ALU.subtract, r=["w0", "lo"], w=["w0"])
                        for it in range(1, NBIS + 1):
                            f = 2.0 ** (-it)
                            stt(mid, w0, f, lo, ALU.mult, ALU.add, r=["w0", "lo"], w=["mid"])
                            ts("dve", junk[:, 0:nk], acc[:, 0:nk], mid[:, 0:1], 0.0, ALU.is_ge, ALU.add, r=["acc", "mid"], w=["junk", "cnt"], accum=cnt)
                            ts("dve", pw, cnt, 255.5, f, ALU.is_ge, ALU.mult, r=["cnt"], w=["pw"])
                            stt(lo, pw, w0[:, 0:1], lo, ALU.mult, ALU.add, r=["pw", "w0", "lo"], w=["lo"])
                        ts("dve", negm[:, 0:nk], acc[:, 0:nk], lo[:, 0:1], 1.0, ALU.is_ge, ALU.subtract, r=["acc", "lo"], w=[nk_])
                    else:
                        ts("dve", negm[:, 0:nk], acc[:, 0:nk], -1.0e8, 1.0, ALU.is_ge, ALU.subtract, r=["acc"], w=[nk_])

                def stageB(ib):
                    tg = ib // TPG; j = ib % TPG
                    qT = qTs[tg % 2]; negm = negms[ib % 2]
                    qk = ("qT", tg % 2); nk_ = ("negm", ib % 2)
                    qc = slice(j * 128, (j + 1) * 128)
                    for g in range(2):
                        abs_ = [AB.next(), AB.next()]
                        pend = []

                        def emit_pv1(item, g=g, abs_=abs_):
                            pi, kt, half = item
                            ab = abs_[half]
                            for h in range(4):
                                mm(ps[ab][:, h * 66:(h + 1) * 66], pT[pi][:, h * 128:(h + 1) * 128], VA[:, kt, g, :],
                                   kt == 0 and h == 0, kt == ib and h == 3, r=[("pT", pi), ("VA", kt // TPG)], w=[PK[ab]])

                        for kt in range(ib + 1):
                            for half in range(2):
                                sbk = SB.next()
                                mm(ps[sbk], kT[:, g, kt * 128:(kt + 1) * 128], qT[:, 8 * g + 4 * half:8 * g + 4 * half + 4, qc], True, False,
                                   r=[("kT", kt // TPG), qk], w=[PK[sbk]])
                                mm(ps[sbk], negm[:, kt * 128:(kt + 1) * 128], irep, False, True, r=[nk_, "cb"], w=[PK[sbk]])
                                pi = pTrot.next()
                                act(pT[pi], ps[sbk], AF.Exp, r=[PK[sbk]], w=[("pT", pi)], scale=0.125)
                                pend.append((pi, kt, half))
                                if len(pend) > 2:
                                    emit_pv1(pend.pop(0))
                        while pend:
                            emit_pv1(pend.pop(0))
                        for half in range(2):
                            ab = abs_[half]
                            accw = ps[ab][:, 0:264].rearrange("p (h c) -> p h c", c=66)
                            recip(rec8[:, 0:4], accw[:, :, 64], r=[PK[ab]], w=["rec8"])
                            yv = ytok[:, (8 * g + 4 * half) * 64:(8 * g + 4 * half + 4) * 64].rearrange("p (h d) -> p h d", d=64)
                            tt("dve", yv, accw[:, :, 0:64], rec8[:, 0:4].unsqueeze(2).to_broadcast([128, 4, 64]), ALU.mult, r=[PK[ab], "rec8"], w=["ytok"])
                    for half in range(2):
                        b = MB.next()
                        for c4 in range(4):
                            cc = half * 4 + c4
                            transp(ps[b][:, c4 * 128:(c4 + 1) * 128], ytok[:, cc * 128:(cc + 1) * 128], identf, r=["ytok", "cf"], w=[PK[b]])
                        cp("act", yT[:, half * 4:half * 4 + 4, qc], ps[b].rearrange("p (c q) -> p c q", q=128), r=[PK[b]], w=["yT"])

                nq = ntg_run * TPG
                proj1(0)
                stageA(0)
                for ib in range(nq):
                    nx = ib + 1
                    if nx < nq:
                        if nx % TPG == 0:
                            proj1(nx // TPG)
                        stageA(nx)
                    stageB(ib)
                    if ib % TPG == TPG - 1:
                        outproj(ib // TPG)

            if stop_after == ("mix", layer):
                break
            S.barrier()
            C = Carver()
            hT = C.get([128, KC, SEQ], BF16)
            nsq = [C.get([128, 512], BF16) for _ in range(2)]
            ntf = [C.get([128, 512]) for _ in range(2)]
            rstd = C.get([128, 512])
            SL = [2, 4, 5, 5, 6]
            WG = [C.get([128, KC, 768], BF16) for _ in range(2)]
            WU = [C.get([128, KC, 768], BF16) for _ in range(2)]
            WDn = [C.get([128, 6, D], BF16) for _ in range(2)]
            actb = [C.get([128, 6, 512], BF16) for _ in range(2)]
            sil = [C.get([128, 512]) for _ in range(2)]
            gf_ap = mod[layer][:, 40:48, s]
            shf_ap = mod[layer][:, 24:32, s]
            wg_d = dr["l%d_ffn_wg" % layer].rearrange("(k p) n -> p k n", p=128)
            wu_d = dr["l%d_ffn_wu" % layer].rearrange("(k p) n -> p k n", p=128)
            wd_d = dr["l%d_ffn_wd" % layer].rearrange("(f p) n -> p f n", p=128)

            def load_slice(si):
                bi = si % 2
                f0 = sum(SL[:si]); nf_ = SL[si]
                for k in range(0, KC, 4):
                    wdma(WG[bi][:, k:k + 4, 0:nf_ * 128], wg_d[:, k:k + 4, f0 * 128:(f0 + nf_) * 128], [("WG", bi)], "WG%d" % bi)
                    wdma(WU[bi][:, k:k + 4, 0:nf_ * 128], wu_d[:, k:k + 4, f0 * 128:(f0 + nf_) * 128], [("WU", bi)], "WU%d" % bi)
                h_ = max(nf_ // 2, 1)
                wdma(WDn[bi][:, 0:h_, :], wd_d[:, f0:f0 + h_, :], [("WD", bi)], "WD%d" % bi)
                wdma(WDn[bi][:, h_:nf_, :], wd_d[:, f0 + h_:f0 + nf_, :], [("WD", bi)], "WD%d" % bi)

            load_slice(0)
            load_slice(1)
            for t4 in range(SEQ // 512):
                hv = hT[:, :, t4 * 512:(t4 + 1) * 512]
                norm_group(t4, gsf[layer][:, :, s], shf_ap, hv, ("hT", t4), nsq, ntf, rstd, ["gs%d" % layer, "mod%d" % layer], width=512)
            GB = Rot([0, 1, 2, 3])
            DB = Rot([4, 5, 6, 7])
            for si in range(len(SL)):
                bi = si % 2
                nf_ = SL[si]
                for t4 in range(SEQ // 512):
                    cols = slice(t4 * 512, (t4 + 1) * 512)
                    ai = (si * 4 + t4) % 2
                    for f in range(nf_):
                        bg = GB.next(); bu = GB.next()
                        for k in range(KC):
                            mm(ps[bg], WG[bi][:, k, f * 128:(f + 1) * 128], hT[:, k, cols], k == 0, k == KC - 1, r=[("WG", bi), ("hT", t4)], w=[PK[bg]])
                        for k in range(KC):
                            mm(ps[bu], WU[bi][:, k, f * 128:(f + 1) * 128], hT[:, k, cols], k == 0, k == KC - 1, r=[("WU", bi), ("hT", t4)], w=[PK[bu]])
                        sl_ = sil[f % 2]
                        act(sl_, ps[bg], AF.Silu, r=[PK[bg]], w=[("sil", f % 2)])
                        tt("dve", actb[ai][:, f, :], sl_, ps[bu], ALU.mult, r=[("sil", f % 2), PK[bu]], w=[("actb", ai)])
                    for ko in range(KC):
                        b = DB.next()
                        for f in range(nf_):
                            mm(ps[b], WDn[bi][:, f, ko * 128:(ko + 1) * 128], actb[ai][:, f, :], f == 0, f == nf_ - 1, r=[("WD", bi), ("actb", ai)], w=[PK[b]])
                        tgs = [("xT", t) for t in range(t4 * 512 // TG, (t4 + 1) * 512 // TG)]
                        stt(xT[:, ko, cols], ps[b], gf_ap[:, ko:ko + 1], xT[:, ko, cols], ALU.mult, ALU.add, r=[PK[b], "mod%d" % layer] + tgs, w=tgs)
                if si + 2 < len(SL):
                    load_slice(si + 2)
            if stop_after == ("ffn", layer):
                break

        S.cut_off = True
        S.barrier()
        C = Carver()
        nsq = [C.get([128, 512], BF16) for _ in range(2)]
        ntf = [C.get([128, 512]) for _ in range(2)]
        rstd = C.get([128, 512])
        ob = [C.get([128, KC, 512]) for _ in range(2)]
        for t4 in range(SEQ // 512):
            o = ob[t4 % 2]
            if stop_after is None:
                c0 = t4 * 512
                b = bankrot.next()
                for k in range(KC):
                    sq = nsq[k % 2]
                    act(sq, xT[:, k, c0:c0 + 512], AF.Square, r=[("xT", c0 // TG), ("xT", c0 // TG + 1)], w=[("nsq", k % 2)])
                    mm(ps[b], onesb, sq, k == 0, k == KC - 1, r=[("nsq", k % 2), "cb"], w=[PK[b]])
                act(rstd, ps[b], AF.Ln, r=[PK[b], "c_eps"], w=["rstd"], bias=c_eps[:, 0:1], scale=1.0 / D)
                act(rstd, rstd, AF.Exp, r=["rstd"], w=["rstd"], scale=-0.5)
                for k in range(KC):
                    stt(o[:, k, :], xT[:, k, c0:c0 + 512], vecs[:, V_NFIN + k:V_NFIN + k + 1], rstd, ALU.mult, ALU.mult,
                        r=[("xT", c0 // TG), ("xT", c0 // TG + 1), "rstd", "vecs"], w=[("ob", t4 % 2)])
            else:
                cp("dve", o, xT[:, :, t4 * 512:(t4 + 1) * 512], r=[("xT", t) for t in range(NTG)], w=[("ob", t4 % 2)])
            for k in range(KC):
                S.dma("sp", lambda e, o=o, k=k, s=s, t4=t4: e.dma_start(out=out_d[s, :, k, t4 * 512:(t4 + 1) * 512], in_=o[:, k, :]),
                      r=[("ob", t4 % 2)], sname="ob%d" % (t4 % 2))
    for nm_ in ("ob0", "ob1"):
        d = S.dsem[nm_]
        S.wait_tok("sp", (d[0], d[1], "dma"))
    S.replay()
    return nc


_CACHE = {}


def _prep_inputs(inputs):
    vecs, pet = pack_vecs(inputs)
    cbv, cfv = make_consts()
    x = np.asarray(inputs["x"], np.float32)
    c = np.asarray(inputs["c"], np.float32)
    shared = {name: np.ascontiguousarray(np.asarray(inputs[name], np.float32)) for name, _ in WEIGHTS}
    shared.update({"vecs": vecs, "pet": pet, "cb": cbv, "cf": cfv})
    in_maps = []
    for i in range(NCORES):
        xs = x[2 * i:2 * i + 2]
        xT = np.ascontiguousarray(xs.reshape(2, SEQ, KC, 128).transpose(0, 3, 2, 1))
        cs = c[2 * i:2 * i + 2]
        cT = np.ascontiguousarray(cs.reshape(2, KC, 128).transpose(2, 1, 0))
        m = dict(shared)
        m["xT"] = xT
        m["cT"] = cT
        in_maps.append(m)
    return in_maps


def kernel(**inputs):
    if "nc" not in _CACHE:
        _CACHE["nc"] = build_program()
    nc = _CACHE["nc"]
    in_maps = _prep_inputs(inputs)
    res = run_bass_kernel_spmd(nc, in_maps, core_ids=list(range(NCORES)))
    out = np.empty((16, SEQ, D), np.float32)
    for i in range(NCORES):
        oT = res.results[i]["outT"]
        out[2 * i:2 * i + 2] = oT.transpose(0, 3, 2, 1).reshape(2, SEQ, D)
    return out
```

```python
import numpy as np
import concourse.bass as bass
import concourse.mybir as mybir
from concourse.bass_utils import run_bass_kernel_spmd

F32 = mybir.dt.float32
BF16 = mybir.dt.bfloat16
AF = mybir.ActivationFunctionType
ALU = mybir.AluOpType
AX = mybir.AxisListType

NCORES = 8
SEQ = 2048
D = 1024
KC = 8
TG = 256
NTG = SEQ // TG
TPG = TG // 128
DFF = 2816
FCH = DFF // 128
IN0 = 2328
IN1 = 1864
BIG = 30000.0
NEG = -1.0e9
EPS = 1e-6
NBIS = 13

V_NMIX0, V_NFFN0, V_NMIX1, V_NFFN1, V_NFIN = 0, 8, 16, 24, 32
V_MODB0, V_MODB1 = 40, 88
V_CONVW, V_CONVB, V_BA, V_BX, V_LAM = 136, 152, 156, 160, 164
NV = 168
CB_ID, CB_IREP, CB_TRI, CB_TRILO, CB_BND, CB_WD, CB_VAUGC, CB_ONES = 0, 128, 640, 1152, 1664, 2176, 2304, 2338
NCB = 2338 + 128
CF_FM, CF_F0, CF_TRIQK, CF_ID = 0, 64, 96, 224
NCF = 352


class Sched:
    ENG = ("pe", "dve", "act", "pool", "sp")

    def __init__(self, nc):
        self.nc = nc
        self.ops = {e: [] for e in self.ENG}
        self.sem = {e: nc.alloc_semaphore("s_" + e) for e in self.ENG}
        self.cnt = {e: 0 for e in self.ENG}
        self.seen = {e: {} for e in self.ENG}
        self.state = {}
        self.dsem = {}
        self.alltoks = {}
        self.nops = 0
        self.max_ops = None
        self.cut_off = False

    def _skip(self):
        if self.cut_off or self.max_ops is None:
            return False
        self.nops += 1
        return self.nops > self.max_ops

    def _deps(self, r, w):
        toks = []
        for k in r:
            st = self.state.get(k)
            if st and st[0] is not None:
                toks.append(st[0])
        for k in w:
            st = self.state.get(k)
            if st:
                if st[0] is not None:
                    toks.append(st[0])
                toks.extend(st[1])
        return toks

    def _commit(self, tok, r, w):
        for k in r:
            st = self.state.setdefault(k, [None, []])
            st[1].append(tok)
            if len(st[1]) > 24:
                best = {}
                for t in st[1]:
                    if id(t[0]) not in best or best[id(t[0])][1] < t[1]:
                        best[id(t[0])] = t
                st[1] = list(best.values())
        for k in w:
            self.state[k] = [tok, []]
        self.alltoks[id(tok[0])] = tok

    def _waits(self, eng, toks, same_ok=False):
        best = {}
        for (s, v, e) in toks:
            if same_ok and e == eng:
                continue
            if self.seen[eng].get(id(s), 0) >= v:
                continue
            if id(s) not in best or best[id(s)][1] < v:
                best[id(s)] = (s, v)
        for (s, v) in best.values():
            self.seen[eng][id(s)] = v
        return list(best.values())

    def op(self, eng, fn, r=(), w=(), same_ok=False):
        if self._skip():
            return None
        toks = self._deps(r, w)
        waits = self._waits(eng, toks, same_ok)
        self.cnt[eng] += 1
        tok = (self.sem[eng], self.cnt[eng], eng)
        self.ops[eng].append((waits, fn, (self.sem[eng], 1)))
        self._commit(tok, r, w)
        return tok

    def dma(self, q, fn, r=(), w=(), sname=None):
        if self._skip():
            return None
        if sname not in self.dsem:
            self.dsem[sname] = [self.nc.alloc_semaphore("d_" + str(sname)), 0]
        d = self.dsem[sname]
        toks = [t for t in self._deps(r, w) if t[0] is not d[0]]
        waits = self._waits(q, toks)
        d[1] += 16
        tok = (d[0], d[1], "dma")
        self.ops[q].append((waits, fn, (d[0], 16)))
        self._commit(tok, r, w)
        return tok

    def wait_tok(self, eng, tok):
        waits = self._waits(eng, [tok])
        if waits:
            self.ops[eng].append((waits, None, None))

    def barrier(self):
        toks = list(self.alltoks.values())
        for e in self.ENG:
            waits = self._waits(e, toks, same_ok=False)
            if waits:
                self.ops[e].append((waits, None, None))
        self.state = {}

    def new_epoch(self):
        self.barrier()
        old = dict(self.sem)
        self.epoch = getattr(self, "epoch", 0) + 1
        for e in self.ENG:
            self.sem[e] = self.nc.alloc_semaphore("s%d_%s" % (self.epoch, e))
            self.cnt[e] = 0
        for s_ in old.values():
            self.alltoks.pop(id(s_), None)
        self._old_sems = getattr(self, "_old_sems", []) + list(old.values())

    def replay(self):
        nc = self.nc
        engs = {"pe": "tensor", "dve": "vector", "act": "scalar", "pool": "gpsimd", "sp": "sync"}
        with nc.Block() as block:
            for e, attr in engs.items():
                lst = self.ops[e]

                def body(eng, lst=lst):
                    for waits, fn, inc in lst:
                        for (s, v) in waits:
                            eng.wait_ge(s, v)
                        if fn is not None:
                            ins = fn(eng)
                            ins.then_inc(inc[0], inc[1])
                getattr(block, attr)(body)


class Rot:
    def __init__(self, items):
        self.items = items
        self.i = 0

    def next(self):
        it = self.items[self.i % len(self.items)]
        self.i += 1
        return it


def make_consts():
    cb = np.zeros((128, NCB), np.float32)
    cb[:, CB_ID:CB_ID + 128] = np.eye(128)
    kk = np.arange(128)[:, None]
    qq = np.arange(128)[None, :]
    for h in range(4):
        cb[:, CB_IREP + h * 128:CB_IREP + (h + 1) * 128] = BIG * np.eye(128)
        cb[:, CB_TRI + h * 128:CB_TRI + (h + 1) * 128] = np.where(kk > qq, -BIG, 0.0)
        cb[:, CB_TRILO + h * 128:CB_TRILO + (h + 1) * 128] = np.where(kk <= qq, -BIG, 0.0)
        j = np.arange(8)[:, None]
        cb[0:8, CB_BND + h * 128:CB_BND + (h + 1) * 128] = np.where(qq >= 16 * j + 15, 0.0, -BIG)
    for j in range(8):
        cb[j, CB_WD + j + 120] = 1.0
    cb[1:128, CB_VAUGC] = 1.0
    for cp in range(1, 128):
        c = cp - 1
        for jj in range(32):
            if 16 * c < 64 * jj + 64 and 16 * c + 32 > 64 * jj:
                cb[cp, CB_VAUGC + 2 + jj] = 1.0
    cb[:, CB_ONES:CB_ONES + 128] = 1.0
    cf = np.zeros((128, NCF), np.float32)
    q = np.arange(128)[:, None]
    hq = (q >= 64).astype(np.int64)
    x = np.arange(64)[None, :]
    dj = x - 32
    fm = np.zeros((128, 64), np.float32)
    fm[(dj == hq) | (dj == hq - 1)] = 1e4
    fm[dj > hq] = NEG
    cf[:, CF_FM:CF_FM + 64] = fm
    cf[:, CF_F0] = 1e4
    cf[:, CF_TRIQK:CF_TRIQK + 128] = np.where(np.arange(128)[None, :] > q, NEG, 0.0)
    cf[:, CF_ID:CF_ID + 128] = np.eye(128)
    return cb, cf


def pack_vecs(inp):
    v = np.zeros((128, NV), np.float32)

    def put(off, vec):
        vec = np.asarray(vec, np.float32).reshape(-1, 128)
        v[:, off:off + vec.shape[0]] = vec.T
    put(V_NMIX0, inp["l0_norm_mix"]); put(V_NFFN0, inp["l0_norm_ffn"])
    put(V_NMIX1, inp["l1_norm_mix"]); put(V_NFFN1, inp["l1_norm_ffn"]); put(V_NFIN, inp["final_norm"])
    put(V_MODB0, inp["l0_mod_b"]); put(V_MODB1, inp["l1_mod_b"])
    cw = np.asarray(inp["l0_conv_w"], np.float32)
    for c in range(4):
        v[:, V_CONVW + c * 4:V_CONVW + c * 4 + 4] = cw[:, c * 128:(c + 1) * 128].T
    put(V_CONVB, inp["l0_conv_b"]); put(V_BA, inp["l0_lru_ba"]); put(V_BX, inp["l0_lru_bx"]); put(V_LAM, inp["l0_lru_lambda"])
    pet = np.zeros((64, 66), np.float32)
    pet[:, 0:32] = np.asarray(inp["l0_cmp_pe_k"], np.float32).T
    pet[:, 32:64] = np.asarray(inp["l0_cmp_pe_v"], np.float32).T
    return v, pet


WEIGHTS = [("l0_mod_w", [D, 6 * D]), ("l1_mod_w", [D, 6 * D]), ("l0_w_in", [D, IN0]), ("l1_w_in", [D, IN1]),
           ("l0_w_out", [D, D]), ("l1_w_out", [D, D]),
           ("l0_ffn_wg", [D, DFF]), ("l0_ffn_wu", [D, DFF]), ("l0_ffn_wd", [DFF, D]),
           ("l1_ffn_wg", [D, DFF]), ("l1_ffn_wu", [D, DFF]), ("l1_ffn_wd", [DFF, D]),
           ("l0_lru_wa", [8, 64, 64]), ("l0_lru_wx", [8, 64, 64]),
           ("l0_cmp_w1_k", [2048, 128]), ("l0_cmp_w2_k", [128, 64]), ("l0_cmp_w1_v", [2048, 128]), ("l0_cmp_w2_v", [128, 64])]


def build_program(stop_after=None, nseq=2, ntg_run=NTG, dbg_yc=False, layers=(0, 1), max_ops=None):
    nc = bass.Bass("TRN2", target_bir_lowering=False)
    S = Sched(nc)
    S.max_ops = max_ops
    dr = {}
    for name, shp in WEIGHTS:
        dr[name] = nc.dram_tensor(name, shp, F32, kind="ExternalInput").ap()
    xT_d = nc.dram_tensor("xT", [2, 128, KC, SEQ], F32, kind="ExternalInput").ap()
    cT_d = nc.dram_tensor("cT", [128, KC, 2], F32, kind="ExternalInput").ap()
    vecs_d = nc.dram_tensor("vecs", [128, NV], F32, kind="ExternalInput").ap()
    pet_d = nc.dram_tensor("pet", [64, 66], F32, kind="ExternalInput").ap()
    cb_d = nc.dram_tensor("cb", [128, NCB], F32, kind="ExternalInput").ap()
    cf_d = nc.dram_tensor("cf", [128, NCF], F32, kind="ExternalInput").ap()
    out_d = nc.dram_tensor("outT", [2, 128, KC, SEQ], F32, kind="ExternalOutput").ap()

    def sb(name, shape, dt=F32):
        return nc.alloc_sbuf_tensor(name, list(shape), dt).ap()

    xT = sb("xT_sb", [128, KC, SEQ])
    vecs = sb("vecs_sb", [128, NV])
    cb = sb("cb_sb", [128, NCB], BF16)
    cf = sb("cf_sb", [128, NCF])
    pet = sb("pet_sb", [64, 66], BF16)
    mod = [sb("mod%d" % l, [128, 48, 2]) for l in range(2)]
    gsm = [sb("gsm%d" % l, [128, KC, 2]) for l in range(2)]
    gsf = [sb("gsf%d" % l, [128, KC, 2]) for l in range(2)]
    siluc = sb("siluc", [128, KC, 2])
    lruc = sb("lruc", [128, 24])
    c_one = sb("c_one", [128, 1]); c_eps = sb("c_eps", [128, 1])
    ps = [nc.alloc_psum_tensor("ps%d" % i, [128, 512], F32).ap() for i in range(8)]
    PK = [("ps", i) for i in range(8)]

    ident = cb[:, CB_ID:CB_ID + 128]
    irep = cb[:, CB_IREP:CB_IREP + 512]
    trineg = cb[:, CB_TRI:CB_TRI + 512]
    trilo = cb[:, CB_TRILO:CB_TRILO + 512]
    bnd = cb[0:8, CB_BND:CB_BND + 512]
    wd = cb[0:8, CB_WD:CB_WD + 128]
    onesb = cb[:, CB_ONES:CB_ONES + 128]
    identf = cf[:, CF_ID:CF_ID + 128]
    triqk = cf[:, CF_TRIQK:CF_TRIQK + 128]

    OV_BYTES = nc.sbuf_bytes_remaining - 2048
    ov = nc.alloc_sbuf_tensor("ov", [128, OV_BYTES // 2], BF16).ap()
    ovf = ov.bitcast(F32)

    class Carver:
        def __init__(self):
            self.off = 0

        def get(self, shape, dt=F32, parts=128):
            n = int(np.prod(shape[1:]))
            esz = 4 if dt == F32 else 2
            self.off = (self.off + 31) // 32 * 32
            o = self.off
            self.off += n * esz
            assert self.off <= OV_BYTES, ("overlay overflow", self.off, OV_BYTES)
            base = ovf if dt == F32 else ov
            a = base[0:shape[0], o // esz:o // esz + n]
            if len(shape) == 3:
                a = a.rearrange("p (a b) -> p a b", b=shape[2])
            elif len(shape) == 4:
                a = a.rearrange("p (a b c) -> p a b c", b=shape[2], c=shape[3])
            return a

    def mm(out, lhsT, rhs, start, stop, r, w):
        S.op("pe", lambda e: e.matmul(out, lhsT=lhsT, rhs=rhs, start=start, stop=stop), r=r, w=w, same_ok=True)

    def act(out, in_, func, r, w, bias=None, scale=None, accum=None):
        kw = {}
        if bias is not None:
            kw["bias"] = bias
        if scale is not None:
            kw["scale"] = scale
        if accum is not None:
            kw["accum_out"] = accum
        S.op("act", lambda e: e.activation(out=out, in_=in_, func=func, **kw), r=r, w=w)

    def ts(eng, out, in0, s1, s2, op0, op1, r, w, accum=None):
        kw = {}
        if accum is not None:
            kw["accum_out"] = accum
        if op1 is None:
            S.op(eng, lambda e: e.tensor_scalar(out=out, in0=in0, scalar1=s1, scalar2=None, op0=op0, **kw), r=r, w=w)
        else:
            S.op(eng, lambda e: e.tensor_scalar(out=out, in0=in0, scalar1=s1, scalar2=s2, op0=op0, op1=op1, **kw), r=r, w=w)

    def tt(eng, out, in0, in1, op, r, w):
        S.op(eng, lambda e: e.tensor_tensor(out=out, in0=in0, in1=in1, op=op), r=r, w=w)

    def stt(out, in0, scalar, in1, op0, op1, r, w):
        S.op("dve", lambda e: e.scalar_tensor_tensor(out=out, in0=in0, scalar=scalar, in1=in1, op0=op0, op1=op1), r=r, w=w)

    def cp(eng, out, in_, r, w):
        if eng == "act":
            S.op("act", lambda e: e.copy(out=out, in_=in_), r=r, w=w)
        else:
            S.op(eng, lambda e: e.tensor_copy(out=out, in_=in_), r=r, w=w)

    def memset(eng, ap, val, w):
        S.op(eng, lambda e: e.memset(ap, val), w=w)

    def recip(out, in_, r, w):
        S.op("dve", lambda e: e.reciprocal(out=out, in_=in_), r=r, w=w)

    def treduce(out, in_, op, r, w):
        S.op("dve", lambda e: e.tensor_reduce(out=out, in_=in_, axis=AX.X, op=op), r=r, w=w)

    def max8(out, in_, r, w):
        S.op("dve", lambda e: e.max(out=out, in_=in_), r=r, w=w)

    def transp(out, in_, idn, r, w):
        S.op("pe", lambda e: e.transpose(out, in_, idn), r=r, w=w, same_ok=True)

    def scan(out, d0, d1, init, r, w):
        S.op("dve", lambda e: e.tensor_tensor_scan(out=out, data0=d0, data1=d1, initial=init, op0=ALU.mult, op1=ALU.add), r=r, w=w)

    wprev = [None]

    def wdma(out, in_, w, sname):
        if wprev[0] is not None:
            S.wait_tok("pool", wprev[0])
        t_ = S.dma("pool", lambda e: e.dma_start(out=out, in_=in_), w=w, sname=sname)
        if t_ is not None:
            wprev[0] = t_

    def gelu_inplace(z, t, r_keys, zk, tk):
        tt("pool", t, z, z, ALU.mult, r=[zk], w=[tk])
        ts("pool", t, t, 0.044715, 1.0, ALU.mult, ALU.add, r=[tk], w=[tk])
        tt("pool", t, t, z, ALU.mult, r=[tk, zk], w=[tk])
        act(t, t, AF.Exp, r=[tk], w=[tk], scale=-1.5957691216)
        ts("dve", t, t, 1.0, None, ALU.add, None, r=[tk], w=[tk])
        recip(t, t, r=[tk], w=[tk])
        tt("pool", t, t, z, ALU.mult, r=[tk, zk], w=[tk])

    S.dma("sp", lambda e: e.dma_start(out=vecs, in_=vecs_d), w=["vecs"], sname="vecs")
    S.dma("sp", lambda e: e.dma_start(out=cf, in_=cf_d), w=["cf"], sname="cf")
    S.dma("sp", lambda e: e.dma_start(out=siluc, in_=cT_d), w=["siluc"], sname="siluc")
    wdma(cb, cb_d, ["cb"], "cb")
    wdma(pet, pet_d, ["pet"], "pet")
    memset("dve", c_one, 1.0, ["c_one"])
    memset("dve", c_eps, EPS, ["c_eps"])
    act(siluc, siluc, AF.Silu, r=["siluc"], w=["siluc"])
    act(lruc[:, 8:12], vecs[:, V_LAM:V_LAM + 4], AF.Exp, r=["vecs"], w=["lruc"], scale=-1.0)
    act(lruc[:, 8:12], lruc[:, 8:12], AF.Ln, r=["lruc", "c_one"], w=["lruc"], bias=c_one[:, 0:1])
    ts("dve", lruc[:, 0:4], lruc[:, 8:12], -8.0, None, ALU.mult, None, r=["lruc"], w=["lruc"])
    ts("dve", lruc[:, 4:8], lruc[:, 8:12], -16.0, None, ALU.mult, None, r=["lruc"], w=["lruc"])
    ts("dve", lruc[:, 12:16], vecs[:, V_BA:V_BA + 4], -1.0, None, ALU.mult, None, r=["vecs", "lruc"], w=["lruc"])
    ts("dve", lruc[:, 16:20], vecs[:, V_BX:V_BX + 4], -1.0, None, ALU.mult, None, r=["vecs", "lruc"], w=["lruc"])

    NG = 768
    C0 = Carver()
    stg = [C0.get([128, KC, NG]) for i in range(2)]
    gi = 0
    bankrot = Rot([0, 1, 2, 3, 4, 5, 6, 7])
    for l in range(2):
        mw = dr["l%d_mod_w" % l].rearrange("(k p) n -> p k n", p=128)
        vb = V_MODB0 if l == 0 else V_MODB1
        for j in range(6 * D // NG):
            st = stg[gi % 2]
            sk = "modstg%d" % (gi % 2)
            S.dma("sp", lambda e, st=st, j=j, mw=mw: e.dma_start(out=st, in_=mw[:, :, j * NG:(j + 1) * NG]), w=[sk], sname=sk)
            gi += 1
            for nn in range(NG // 128):
                b = bankrot.next()
                col = j * (NG // 128) + nn
                for k in range(KC):
                    mm(ps[b][:, 0:2], st[:, k, nn * 128:(nn + 1) * 128], siluc[:, k, :], k == 0, k == KC - 1,
                       r=[sk, "siluc"], w=[PK[b]])
                ts("dve", mod[l][:, col, :], ps[b][:, 0:2], vecs[:, vb + col:vb + col + 1], None, ALU.add, None,
                   r=[PK[b], "vecs"], w=["mod%d" % l])
        nm = V_NMIX0 if l == 0 else V_NMIX1
        nf = V_NFFN0 if l == 0 else V_NFFN1
        for s in range(2):
            stt(gsm[l][:, :, s], mod[l][:, 8:16, s], 1.0, vecs[:, nm:nm + 8], ALU.add, ALU.mult, r=["mod%d" % l, "vecs"], w=["gs%d" % l])
            stt(gsf[l][:, :, s], mod[l][:, 32:40, s], 1.0, vecs[:, nf:nf + 8], ALU.add, ALU.mult, r=["mod%d" % l, "vecs"], w=["gs%d" % l])

    def norm_group(tg, gs_ap, sh_ap, hT_out, hkey, tmp_sq, tmp_f, rstd, rkeys, scale_only=False, width=TG):
        c0 = tg * width
        xk = [("xT", t) for t in range(c0 // TG, (c0 + width) // TG)]
        b = bankrot.next()
        for k in range(KC):
            sq = tmp_sq[k % 2]
            act(sq, xT[:, k, c0:c0 + width], AF.Square, r=xk + rkeys, w=[("nsq", k % 2)])
            mm(ps[b][:, 0:width], onesb, sq, k == 0, k == KC - 1, r=[("nsq", k % 2), "cb"], w=[PK[b]])
        act(rstd, ps[b][:, 0:width], AF.Ln, r=[PK[b], "c_eps"], w=["rstd"], bias=c_eps[:, 0:1], scale=1.0 / D)
        act(rstd, rstd, AF.Exp, r=["rstd"], w=["rstd"], scale=-0.5)
        for k in range(KC):
            tf = tmp_f[k % 2]
            stt(tf, xT[:, k, c0:c0 + width], gs_ap[:, k:k + 1], rstd, ALU.mult, ALU.mult,
                r=xk + ["rstd"] + rkeys, w=[("ntf", k % 2)])
            if sh_ap is not None:
                act(hT_out[:, k, :], tf, AF.Identity, r=[("ntf", k % 2)] + rkeys, w=[hkey], bias=sh_ap[:, k:k + 1])
            else:
                cp("act", hT_out[:, k, :], tf, r=[("ntf", k % 2)], w=[hkey])

    dbg = {}

    for s in range(nseq):
        if s > 0:
            S.new_epoch()
        for k in range(KC):
            S.dma("sp", lambda e, k=k, s=s: e.dma_start(out=xT[:, k, :], in_=xT_d[s, :, k, :]),
                  w=[("xT", t) for t in range(NTG)], sname="xT%d" % k)

        for layer in layers:
            if stop_after == ("p0",):
                break
            S.barrier()
            C = Carver()
            nin = IN0 if layer == 0 else IN1
            WA = C.get([128, KC, nin], BF16)
            WB = C.get([128, KC, D], BF16)
            win_d = dr["l%d_w_in" % layer].rearrange("(k p) n -> p k n", p=128)
            for k in range(KC):
                wdma(WA[:, k, :], win_d[:, k, :], ["WA"], "WA")
            wout_d = dr["l%d_w_out" % layer].rearrange("(k p) n -> p k n", p=128)
            for k in range(0, KC, 4):
                wdma(WB[:, k:k + 4, :], wout_d[:, k:k + 4, :], ["WB"], "WB")
            hTg = C.get([128, KC, TG], BF16)
            nsq = [C.get([128, TG], BF16) for _ in range(2)]
            ntf = [C.get([128, TG]) for _ in range(2)]
            rstd = C.get([128, TG])
            yT = C.get([128, KC, TG], BF16)
            pT = [C.get([128, 512], BF16) for _ in range(3)]
            pTrot = Rot([0, 1, 2])
            ytok = C.get([128, 512 if layer == 0 else 1024])
            gm_ap = mod[layer][:, 16:24, s]
            shm_ap = mod[layer][:, 0:8, s]
            SB = Rot([0, 1, 2])
            AB = Rot([3, 4])
            MB = Rot([5, 6, 7])

            def outproj(tg):
                if dbg_yc == "l1_yc" and layer == 1:
                    cp("dve", xT[:, :, tg * TG:(tg + 1) * TG], yT, r=["yT", ("xT", tg)], w=[("xT", tg)])
                    return
                for ko in range(KC):
                    b = MB.next()
                    for kf in range(KC):
                        mm(ps[b][:, 0:TG], WB[:, kf, ko * 128:(ko + 1) * 128], yT[:, kf, :], kf == 0, kf == KC - 1,
                           r=["WB", "yT", "yTa", "yTb"], w=[PK[b]])
                    stt(xT[:, ko, tg * TG:(tg + 1) * TG], ps[b][:, 0:TG], gm_ap[:, ko:ko + 1], xT[:, ko, tg * TG:(tg + 1) * TG],
                        ALU.mult, ALU.add, r=[PK[b], "mod%d" % layer, ("xT", tg)], w=[("xT", tg)])

            if layer == 0:
                W1 = [C.get([64, 32, 128], BF16) for _ in range(2)]
                W2 = [C.get([128, 64], BF16) for _ in range(2)]
                BD = [C.get([128, 4, 128], BF16) for _ in range(2)]
                for i, nm_ in enumerate(("k", "v")):
                    w1d = dr["l0_cmp_w1_" + nm_].rearrange("(l d) m -> d l m", d=64)
                    for l0_ in range(0, 32, 4):
                        wdma(W1[i][:, l0_:l0_ + 4, :], w1d[:, l0_:l0_ + 4, :], ["W1%d" % i], "W1%d" % i)
                    wdma(W2[i], dr["l0_cmp_w2_" + nm_], ["W2%d" % i], "W2%d" % i)
                for i, nm_ in enumerate(("wa", "wx")):
                    memset("pool", BD[i], 0.0, ["BD%d" % i])
                    for c in range(4):
                        wdma(BD[i][0:64, c, 0:64], dr["l0_lru_" + nm_][2 * c], ["BD%d" % i], "BD%d" % i)
                        wdma(BD[i][64:128, c, 64:128], dr["l0_lru_" + nm_][2 * c + 1], ["BD%d" % i], "BD%d" % i)
                ksT = C.get([128, 2, SEQ], BF16)
                kwT = C.get([128, 2, 6 * 128], BF16)
                VsA = C.get([128, 16, 2, 66], BF16)
                VwA = C.get([128, 6, 2, 66], BF16)
                kcR = [C.get([64, 2, 16, 17], BF16) for _ in range(2)]
                KcT = C.get([128, 2, 128], BF16)
                VcT = C.get([64, 2, 128])
                VcA = C.get([128, 2, 98], BF16)
                hid = C.get([128, 16]); hidt = C.get([128, 16]); hidb = C.get([128, 16], BF16)
                pebias = C.get([128, 2])
                qT = C.get([128, 8, TG], BF16)
                sig = C.get([128, TPG, 24]); gtsraw = C.get([128, TPG, 24])
                AXb = C.get([128, 4, TG + 3])
                agf = C.get([128, TG]); gt = C.get([128, TG]); xc = C.get([128, TG]); xcb = C.get([128, TG], BF16)
                rr = C.get([128, TG]); ii = C.get([128, TG]); aa = C.get([128, TG])
                carry = C.get([128, 4])
                negE = [C.get([128, 32, 64], BF16) for _ in range(2)]
                impn = C.get([128, 4, 32]); imp = C.get([128, 32]); top8 = C.get([128, 8]); negsel = C.get([128, 32], BF16)
                rec = C.get([128, 4]); coef = C.get([128, 4]); tmpo = C.get([128, 4, 64])

                memset("dve", VsA, 0.0, [("VsA", t) for t in range(NTG)])
                memset("dve", VwA, 0.0, ["VwA"])
                for t_ in range(16):
                    memset("dve", VsA[:, t_, :, 64:65], 1.0, [("VsA", t_ // TPG)])
                for t_ in range(6):
                    memset("dve", VwA[:, t_, :, 64:65], 1.0, ["VwA"])
                memset("dve", KcT, 0.0, ["KcT"])
                memset("dve", qT, 0.0, ["qT"])
                memset("pool", ksT, 0.0, [("ksT", t) for t in range(NTG)])
                memset("pool", kwT, 0.0, [("kwT", t) for t in range(6)])
                memset("dve", VcT, 0.0, ["VcT"])
                memset("dve", AXb, 0.0, ["AXb"])
                for i in range(2):
                    memset("pool", kcR[i], 0.0, ["kcR%d" % i])
                for g in range(2):
                    cp("pool", VcA[:, g, 64:98], cb[:, CB_VAUGC:CB_VAUGC + 34], r=["cb"], w=["VcA"])
                for i in range(2):
                    b = MB.next()
                    for l in range(32):
                        mm(ps[b][:, 0:2], W1[i][:, l, :], pet[:, 32 * i + l:32 * i + l + 2],
                           l == 0, l == 31, r=["W1%d" % i, "pet"], w=[PK[b]])
                    cp("dve", pebias[:, i:i + 1], ps[b][:, 0:1], r=[PK[b]], w=["pebias"])

                LOOK = 2
                pq = []

                def make_evac(ab, g, gi_, j, yv):
                    def ev():
                        accw = ps[ab][:, 0:264].rearrange("p (h c) -> p h c", c=66)
                        ts("dve", rec, accw[:, :, 64], 1e-30, None, ALU.max, None, r=[PK[ab]], w=["rec"])
                        recip(rec, rec, r=["rec"], w=["rec"])
                        tt("dve", coef, rec, sig[:, j, 12 * g + gi_:12 * g + 12:3], ALU.mult, r=["rec", "sig"], w=["coef"])
                        tt("dve", tmpo, accw[:, :, 0:64], coef.unsqueeze(2).to_broadcast([128, 4, 64]), ALU.mult, r=[PK[ab], "coef"], w=["tmpo"])
                        tt("pool", yv, yv, tmpo, ALU.add, r=["ytok", "tmpo"], w=["ytok"])
                    return ev

                def drain(limit):
                    while pq and (pq[0][0] == "ev" or sum(1 for it in pq if it[0] == "pv") > limit):
                        kind, dat = pq.pop(0)
                        if kind == "pv":
                            emit_pv0(dat[0], dat[1], dat[2])
                        else:
                            dat()

                def emit_pv0(item, ab, nkt):
                    pi, vrhs, vkey, n_ = item
                    for h in range(4):
                        mm(ps[ab][:, h * 66:(h + 1) * 66], pT[pi][:, h * 128:(h + 1) * 128], vrhs,
                           n_ == 0 and h == 0, n_ == nkt - 1 and h == 3, r=[("pT", pi), vkey], w=[PK[ab]])

                for tg in range(ntg_run):
                    t0 = tg * TG
                    norm_group(tg, gsm[0][:, :, s], shm_ap, hTg, "hTg", nsq, ntf, rstd, ["gs0", "mod0"])
                    if dbg_yc == "l0_h":
                        cp("dve", xT[:, :, tg * TG:(tg + 1) * TG], hTg, r=["hTg", ("xT", tg)], w=[("xT", tg)])
                        continue
                    def lru_gen(tg=tg):
                        for c in range(4):
                            bg = MB.next()
                            for k in range(KC):
                                mm(ps[bg][:, 0:TG], WA[:, k, c * 128:(c + 1) * 128], hTg[:, k, :], k == 0, k == KC - 1, r=["WA", "hTg"], w=[PK[bg]])
                            cp("act", agf, ps[bg][:, 0:TG], r=[PK[bg]], w=["agf"])
                            yield
                            bx_ = MB.next()
                            for k in range(KC):
                                mm(ps[bx_][:, 0:TG], WA[:, k, 512 + c * 128:512 + (c + 1) * 128], hTg[:, k, :], k == 0, k == KC - 1, r=["WA", "hTg"], w=[PK[bx_]])
                            cp("act", AXb[:, c, 3:3 + TG], ps[bx_][:, 0:TG], r=[PK[bx_]], w=["AXb"])
                            yield
                            cw = V_CONVW + c * 4
                            ts("dve", xc, AXb[:, c, 0:TG], vecs[:, cw:cw + 1], vecs[:, V_CONVB + c:V_CONVB + c + 1], ALU.mult, ALU.add, r=["AXb", "vecs"], w=["xc"])
                            yield
                            for kk_ in range(1, 4):
                                stt(xc, AXb[:, c, kk_:kk_ + TG], vecs[:, cw + kk_:cw + kk_ + 1], xc, ALU.mult, ALU.add, r=["AXb", "vecs", "xc"], w=["xc"])
                                yield
                            cp("pool", AXb[:, c, 0:3], AXb[:, c, TG:TG + 3], r=["AXb"], w=["AXb"])
                            cp("act", xcb, xc, r=["xc"], w=["xcb"])
                            yield
                            br = MB.next()
                            mm(ps[br][:, 0:TG], BD[0][:, c, :], xcb, True, True, r=["BD0", "xcb"], w=[PK[br]])
                            bi = MB.next()
                            mm(ps[bi][:, 0:TG], BD[1][:, c, :], xcb, True, True, r=["BD1", "xcb"], w=[PK[bi]])
                            yield
                            act(rr, ps[br][:, 0:TG], AF.Exp, r=[PK[br], "lruc"], w=["rr"], bias=lruc[:, 12 + c:13 + c], scale=-1.0)
                            act(ii, ps[bi][:, 0:TG], AF.Exp, r=[PK[bi], "lruc"], w=["ii"], bias=lruc[:, 16 + c:17 + c], scale=-1.0)
                            yield
                            act(rr, rr, AF.Ln, r=["rr", "c_one"], w=["rr"], bias=c_one[:, 0:1])
                            yield
                            act(rr, rr, AF.Exp, r=["rr"], w=["rr"], scale=-1.0)
                            yield
                            act(ii, ii, AF.Ln, r=["ii", "c_one"], w=["ii"], bias=c_one[:, 0:1])
                            yield
                            act(ii, ii, AF.Exp, r=["ii"], w=["ii"], scale=-1.0)
                            yield
                            act(aa, rr, AF.Exp, r=["rr", "lruc"], w=["aa"], scale=lruc[:, c:c + 1])
                            yield
                            act(rr, rr, AF.Exp, r=["rr", "lruc"], w=["rr"], scale=lruc[:, 4 + c:5 + c])
                            yield
                            ts("dve", rr, rr, -1.0, 1.0, ALU.mult, ALU.add, r=["rr"], w=["rr"])
                            yield
                            ts("dve", rr, rr, 1e-18, None, ALU.max, None, r=["rr"], w=["rr"])
                            yield
                            act(rr, rr, AF.Ln, r=["rr"], w=["rr"])
                            yield
                            act(rr, rr, AF.Exp, r=["rr"], w=["rr"], scale=0.5)
                            yield
                            if tg == 0:
                                memset("dve", rr[:, 0:1], 1.0, ["rr"])
                            tt("dve", ii, ii, xc, ALU.mult, r=["ii", "xc"], w=["ii"])
                            yield
                            tt("dve", ii, ii, rr, ALU.mult, r=["ii", "rr"], w=["ii"])
                            yield
                            init = 0.0 if tg == 0 else carry[:, c:c + 1]
                            scan(xc, aa, ii, init, r=["aa", "ii", "carry"], w=["xc"])
                            yield
                            cp("dve", carry[:, c:c + 1], xc[:, TG - 1:TG], r=["xc"], w=["carry"])
                            tt("pool", gt, agf, agf, ALU.mult, r=["agf"], w=["gt"])
                            yield
                            ts("pool", gt, gt, 0.044715, 1.0, ALU.mult, ALU.add, r=["gt"], w=["gt"])
                            yield
                            tt("pool", gt, gt, agf, ALU.mult, r=["gt", "agf"], w=["gt"])
                            yield
                            act(gt, gt, AF.Exp, r=["gt"], w=["gt"], scale=-1.5957691216)
                            yield
                            act(gt, gt, AF.Ln, r=["gt", "c_one"], w=["gt"], bias=c_one[:, 0:1])
                            yield
                            act(gt, gt, AF.Exp, r=["gt"], w=["gt"], scale=-1.0)
                            yield
                            tt("pool", gt, gt, agf, ALU.mult, r=["gt", "agf"], w=["gt"])
                            yield
                            tt("pool", yT[:, c, :], xc, gt, ALU.mult, r=["xc", "gt"], w=["yTa"])
                            yield

                    lgen = lru_gen()
                    nsteps_ = 0
                    for j_ in range(TPG):
                        ib_ = tg * TPG + j_
                        nsteps_ += 2 * (1 + len([kt for kt in range(ib_ - 4, ib_ + 1) if kt >= 0]) + ib_ + 1)
                    per_step = -(-26 * 4 // max(nsteps_ - 2, 1))

                    def pump(n):
                        for _ in range(n):
                            try:
                                next(lgen)
                            except StopIteration:
                                return
                    for h in range(8):
                        b = MB.next()
                        for k in range(KC):
                            mm(ps[b][0:64, 0:TG], WA[:, k, 1024 + h * 64:1024 + (h + 1) * 64], hTg[:, k, :], k == 0, k == KC - 1, r=["WA", "hTg"], w=[PK[b]])
                        cp("act" if h % 2 == 0 else "dve", qT[0:64, h, :], ps[b][0:64, 0:TG], r=[PK[b]], w=["qT"])
                    for which, cbase in (("kc", 1536), ("vc", 1664), ("ks", 1792), ("kw", 2048)):
                        for g in range(2):
                            b = MB.next()
                            for k in range(KC):
                                mm(ps[b][0:64, 0:TG], WA[:, k, cbase + g * 64:cbase + (g + 1) * 64], hTg[:, k, :], k == 0, k == KC - 1, r=["WA", "hTg"], w=[PK[b]])
                            if which == "ks":
                                cp("act", ksT[0:64, g, t0:t0 + TG], ps[b][0:64, 0:TG], r=[PK[b]], w=[("ksT", tg)])
                            elif which == "kw":
                                for j in range(TPG):
                                    kt = tg * TPG + j
                                    sl = kt % 6
                                    cp("dve", kwT[0:64, g, sl * 128:(sl + 1) * 128], ps[b][0:64, j * 128:(j + 1) * 128], r=[PK[b]], w=[("kwT", sl)])
                            else:
                                i = 0 if which == "kc" else 1
                                if g == 0:
                                    if tg > 0:
                                        cp("pool", kcR[i][:, :, :, 0:1], kcR[i][:, :, :, 16:17], r=["kcR%d" % i], w=["kcR%d" % i])
                                cp("act", kcR[i][:, g, :, 1:17], ps[b][0:64, 0:TG].rearrange("p (b r) -> p r b", r=16), r=[PK[b]], w=["kcR%d" % i])
                    for j in range(TPG):
                        kt = tg * TPG + j
                        b = MB.next()
                        for k in range(KC):
                            mm(ps[b][:, 0:128], hTg[:, k, j * 128:(j + 1) * 128], WA[:, k, 1920:2048], k == 0, k == KC - 1, r=["WA", "hTg"], w=[PK[b]])
                        cp("act", VsA[:, kt, :, 0:64], ps[b][:, 0:128].rearrange("p (g d) -> p g d", d=64), r=[PK[b]], w=[("VsA", tg)])
                        b = MB.next()
                        for k in range(KC):
                            mm(ps[b][:, 0:152], hTg[:, k, j * 128:(j + 1) * 128], WA[:, k, 2176:2328], k == 0, k == KC - 1, r=["WA", "hTg"], w=[PK[b]])
                        cp("dve", VwA[:, kt % 6, :, 0:64], ps[b][:, 0:128].rearrange("p (g d) -> p g d", d=64), r=[PK[b]], w=[("VwA", kt % 6)])
                        cp("dve", gtsraw[:, j, :], ps[b][:, 128:152], r=[PK[b]], w=["gtsraw"])
                    act(sig, gtsraw, AF.Exp, r=["gtsraw"], w=["sig"], scale=-1.0)
                    ts("dve", sig, sig, 1.0, None, ALU.add, None, r=["sig"], w=["sig"])
                    recip(sig, sig, r=["sig"], w=["sig"])
                    for i in range(2):
                        for g in range(2):
                            b = MB.next()
                            for l in range(32):
                                mm(ps[b][:, 0:16], W1[i][:, l, :], kcR[i][:, g, l % 16, (l // 16):(l // 16) + 16], l == 0, l == 31,
                                   r=["W1%d" % i, "kcR%d" % i], w=[PK[b]])
                            act(hid, ps[b][:, 0:16], AF.Identity, r=[PK[b], "pebias"], w=["hid"], bias=pebias[:, i:i + 1])
                            gelu_inplace(hid, hidt, None, "hid", "hidt")
                            cp("pool", hidb, hidt, r=["hidt"], w=["hidb"])
                            b2 = MB.next()
                            mm(ps[b2][0:64, 0:16], W2[i], hidb, True, True, r=["W2%d" % i, "hidb"], w=[PK[b2]])
                            if i == 0:
                                cp("act", KcT[0:64, g, 16 * tg:16 * tg + 16], ps[b2][0:64, 0:16], r=[PK[b2]], w=["KcT"])
                            else:
                                cp("act", VcT[:, g, 16 * tg:16 * tg + 16], ps[b2][0:64, 0:16], r=[PK[b2]], w=["VcT"])
                    if tg == 0:
                        memset("dve", KcT[0:64, :, 0:1], 0.0, ["KcT"])
                        memset("dve", VcT[:, :, 0:1], 0.0, ["VcT"])
                    for g in range(2):
                        b = MB.next()
                        transp(ps[b][:, 0:64], VcT[:, g, :], identf[0:64, 0:64], r=["VcT", "cf"], w=[PK[b]])
                        cp("act", VcA[:, g, 0:64], ps[b][:, 0:64], r=[PK[b]], w=["VcA"])

                    for j in range(TPG):
                        ib = tg * TPG + j
                        qc = slice(j * 128, (j + 1) * 128)
                        for g in range(2):
                            qrhs = qT[:, 4 * g:4 * g + 4, qc]
                            yv = ytok[:, g * 256:(g + 1) * 256].rearrange("p (h d) -> p h d", d=64)
                            M = 8 * (ib + 1)
                            sbk = SB.next()
                            mm(ps[sbk][0:M, :], KcT[:, g, 0:M], qrhs, True, False, r=["KcT", "qT"], w=[PK[sbk]])
                            s0 = 120 - 8 * ib
                            mm(ps[sbk][0:M, :], wd[:, s0:s0 + M], bnd, False, True, r=["cb"], w=[PK[sbk]])
                            pi = pTrot.next()
                            act(pT[pi][0:M, :], ps[sbk][0:M, :], AF.Exp, r=[PK[sbk]], w=[("pT", pi)], scale=0.125)
                            ab = AB.next()
                            for h in range(4):
                                mm(ps[ab][:, h * 98:(h + 1) * 98], pT[pi][0:M, h * 128:(h + 1) * 128], VcA[0:M, g, :], h == 0, h == 3,
                                   r=[("pT", pi), "VcA"], w=[PK[ab]])
                            pump(per_step)
                            accv = ps[ab][:, 0:392].rearrange("p (h c) -> p h c", c=98)
                            ts("dve", rec, accv[:, :, 64], 1e-30, None, ALU.max, None, r=[PK[ab]], w=["rec"])
                            recip(rec, rec, r=["rec"], w=["rec"])
                            tt("dve", impn, accv[:, :, 66:98], rec.unsqueeze(2).to_broadcast([128, 4, 32]), ALU.mult, r=[PK[ab], "rec"], w=["impn"])
                            treduce(imp, impn.rearrange("p h j -> p j h"), ALU.add, r=["impn"], w=["imp"])
                            tt("dve", imp, imp, cf[:, CF_FM + 32 - 2 * ib:CF_FM + 64 - 2 * ib], ALU.add, r=["imp", "cf"], w=["imp"])
                            tt("dve", imp, imp, cf[:, CF_F0:CF_F0 + 32], ALU.add, r=["imp", "cf"], w=["imp"])
                            max8(top8, imp, r=["imp"], w=["top8"])
                            ts("dve", negsel, imp, top8[:, 7:8], 1.0, ALU.is_ge, ALU.subtract, r=["imp", "top8"], w=["negsel"])
                            cp("pool", negE[g], negsel.unsqueeze(2).to_broadcast([128, 32, 64]), r=["negsel"], w=[("negE", g)])
                            tt("dve", coef, rec, sig[:, j, 12 * g + 0:12 * g + 12:3], ALU.mult, r=["rec", "sig"], w=["coef"])
                            tt("dve", yv, accv[:, :, 0:64], coef.unsqueeze(2).to_broadcast([128, 4, 64]), ALU.mult, r=[PK[ab], "coef"], w=["ytok"])
                        for br_, gi_, g in (("win", 2, 0), ("win", 2, 1), ("slc", 1, 0), ("slc", 1, 1)):
                            qrhs = qT[:, 4 * g:4 * g + 4, qc]
                            yv = ytok[:, g * 256:(g + 1) * 256].rearrange("p (h d) -> p h d", d=64)
                            if True:
                                kts = [kt for kt in range(ib - 4, ib + 1) if kt >= 0] if br_ == "win" else list(range(ib + 1))
                                ab = AB.next()
                                for n_, kt in enumerate(kts):
                                    sbk = SB.next()
                                    if br_ == "win":
                                        klhs = kwT[:, g, (kt % 6) * 128:(kt % 6 + 1) * 128]
                                        kkey = ("kwT", kt % 6)
                                        vrhs = VwA[:, kt % 6, g, :]
                                        vkey = ("VwA", kt % 6)
                                        extra = []
                                        if kt == ib:
                                            extra.append((ident, trineg, ["cb"]))
                                        if kt == ib - 4:
                                            extra.append((ident, trilo, ["cb"]))
                                    else:
                                        klhs = ksT[:, g, kt * 128:(kt + 1) * 128]
                                        kkey = ("ksT", kt // TPG)
                                        vrhs = VsA[:, kt, g, :]
                                        vkey = ("VsA", kt // TPG)
                                        extra = [(negE[g][:, 2 * kt:2 * kt + 2, :].rearrange("p a b -> p (a b)"), irep, [("negE", g), "cb"])]
                                        if kt == ib:
                                            extra.append((ident, trineg, ["cb"]))
                                    mm(ps[sbk][:, :], klhs, qrhs, True, len(extra) == 0, r=[kkey, "qT"], w=[PK[sbk]])
                                    for xi, (l_, r_, ks_) in enumerate(extra):
                                        mm(ps[sbk][:, :], l_, r_, False, xi == len(extra) - 1, r=ks_, w=[PK[sbk]])
                                    pi = pTrot.next()
                                    act(pT[pi], ps[sbk], AF.Exp, r=[PK[sbk]], w=[("pT", pi)], scale=0.125)
                                    pq.append(("pv", ((pi, vrhs, vkey, n_), ab, len(kts))))
                                    drain(LOOK)
                                    pump(per_step)
                                pq.append(("ev", make_evac(ab, g, gi_, j, yv)))
                        drain(0)
                        b = MB.next()
                        for c4 in range(4):
                            transp(ps[b][:, c4 * 128:(c4 + 1) * 128], ytok[:, c4 * 128:(c4 + 1) * 128], identf, r=["ytok", "cf"], w=[PK[b]])
                        cp("act", yT[:, 4:8, qc], ps[b].rearrange("p (c q) -> p c q", q=128), r=[PK[b]], w=["yTb"])
                    pump(10 ** 6)
                    outproj(tg)
            else:
                kT = C.get([128, 2, SEQ], BF16)
                kiT = C.get([128, SEQ], BF16)
                VA = C.get([128, 16, 2, 66], BF16)
                qTs = [C.get([128, 16, TG], BF16) for _ in range(2)]
                qiTs = [C.get([128, 8, TG], BF16) for _ in range(2)]
                widxs = [C.get([128, TPG, 8]) for _ in range(2)]
                acc = C.get([128, SEQ])
                rl = [C.get([128, 512]) for _ in range(2)]
                negms = [C.get([128, SEQ], BF16) for _ in range(2)]
                lo = C.get([128, 1]); w0 = C.get([128, 1]); mid = C.get([128, 1]); cnt = C.get([128, 1]); pw = C.get([128, 1])
                junk = C.get([128, SEQ], BF16)
                rec8 = C.get([128, 8])
                memset("dve", VA, 0.0, [("VA", t) for t in range(NTG)])
                for t_ in range(16):
                    memset("dve", VA[:, t_, :, 64:65], 1.0, [("VA", t_ // TPG)])
                memset("pool", kT, 0.0, [("kT", t) for t in range(NTG)])
                memset("pool", kiT, 0.0, [("kiT", t) for t in range(NTG)])
                for i_ in range(2):
                    memset("dve", qTs[i_], 0.0, [("qT", i_)])
                    memset("pool", qiTs[i_], 0.0, [("qiT", i_)])

                def proj1(tg):
                    t0 = tg * TG
                    qT = qTs[tg % 2]; qiT = qiTs[tg % 2]; widx = widxs[tg % 2]
                    qk = ("qT", tg % 2); qik = ("qiT", tg % 2); wk = ("widx", tg % 2)
                    norm_group(tg, gsm[1][:, :, s], shm_ap, hTg, "hTg", nsq, ntf, rstd, ["gs1", "mod1"])
                    for h in range(8):
                        b = MB.next()
                        for k in range(KC):
                            mm(ps[b][0:64, 0:TG], WA[:, k, 1280 + h * 64:1280 + (h + 1) * 64], hTg[:, k, :], k == 0, k == KC - 1, r=["WA", "hTg"], w=[PK[b]])
                        cp("act" if h % 2 == 0 else "dve", qiT[0:64, h, :], ps[b][0:64, 0:TG], r=[PK[b]], w=[qik])
                    b = MB.next()
                    for k in range(KC):
                        mm(ps[b][0:64, 0:TG], WA[:, k, 1792:1856], hTg[:, k, :], k == 0, k == KC - 1, r=["WA", "hTg"], w=[PK[b]])
                    cp("act", kiT[0:64, t0:t0 + TG], ps[b][0:64, 0:TG], r=[PK[b]], w=[("kiT", tg)])
                    for j in range(TPG):
                        b = MB.next()
                        for k in range(KC):
                            mm(ps[b][:, 0:8], hTg[:, k, j * 128:(j + 1) * 128], WA[:, k, 1856:1864], k == 0, k == KC - 1, r=["WA", "hTg"], w=[PK[b]])
                        cp("dve", widx[:, j, :], ps[b][:, 0:8], r=[PK[b]], w=[wk])
                    for h in range(16):
                        b = MB.next()
                        for k in range(KC):
                            mm(ps[b][0:64, 0:TG], WA[:, k, h * 64:(h + 1) * 64], hTg[:, k, :], k == 0, k == KC - 1, r=["WA", "hTg"], w=[PK[b]])
                        cp("act" if h % 2 == 0 else "dve", qT[0:64, h, :], ps[b][0:64, 0:TG], r=[PK[b]], w=[qk])
                    for g in range(2):
                        b = MB.next()
                        for k in range(KC):
                            mm(ps[b][0:64, 0:TG], WA[:, k, 1024 + g * 64:1024 + (g + 1) * 64], hTg[:, k, :], k == 0, k == KC - 1, r=["WA", "hTg"], w=[PK[b]])
                        cp("act", kT[0:64, g, t0:t0 + TG], ps[b][0:64, 0:TG], r=[PK[b]], w=[("kT", tg)])
                    for j in range(TPG):
                        kt = tg * TPG + j
                        b = MB.next()
                        for k in range(KC):
                            mm(ps[b][:, 0:128], hTg[:, k, j * 128:(j + 1) * 128], WA[:, k, 1152:1280], k == 0, k == KC - 1, r=["WA", "hTg"], w=[PK[b]])
                        cp("act", VA[:, kt, :, 0:64], ps[b][:, 0:128].rearrange("p (g d) -> p g d", d=64), r=[PK[b]], w=[("VA", tg)])

                def stageA(ib):
                    tg = ib // TPG; j = ib % TPG
                    qiT = qiTs[tg % 2]; widx = widxs[tg % 2]; negm = negms[ib % 2]
                    qik = ("qiT", tg % 2); wk = ("widx", tg % 2); nk_ = ("negm", ib % 2)
                    qc = slice(j * 128, (j + 1) * 128)
                    nk = (ib + 1) * 128
                    for c0 in range(0, nk, 512):
                        wdt = min(512, nk - c0)
                        for h in range(8):
                            b = MB.next()
                            mm(ps[b][:, 0:wdt], qiT[:, h, qc], kiT[:, c0:c0 + wdt], True, True,
                               r=[qik] + [("kiT", t) for t in range(c0 // TG, (c0 + wdt - 1) // TG + 1)], w=[PK[b]])
                            ri = h % 2
                            act(rl[ri][:, 0:wdt], ps[b][:, 0:wdt], AF.Relu, r=[PK[b]], w=[("rl", ri)], scale=0.125)
                            if h == 0:
                                ts("dve", acc[:, c0:c0 + wdt], rl[ri][:, 0:wdt], widx[:, j, 0:1], None, ALU.mult, None, r=[("rl", ri), wk], w=["acc"])
                            else:
                                stt(acc[:, c0:c0 + wdt], rl[ri][:, 0:wdt], widx[:, j, h:h + 1], acc[:, c0:c0 + wdt], ALU.mult, ALU.add,
                                    r=[("rl", ri), wk, "acc"], w=["acc"])
                    tt("dve", acc[:, ib * 128:nk], acc[:, ib * 128:nk], triqk, ALU.add, r=["acc", "cf"], w=["acc"])
                    if ib >= 2:
                        treduce(lo, acc[:, 0:ib * 128], ALU.min, r=["acc"], w=["lo"])
                        treduce(w0, acc[:, 0:nk], ALU.max, r=["acc"], w=["w0"])
                        tt("dve", w0, w0, lo, ALU.subtract, r=["w0", "lo"], w=["w0"])
                        for it in range(1, NBIS + 1):
                            f = 2.0 ** (-it)
                            stt(mid, w0, f, lo, ALU.mult, ALU.add, r=["w0", "lo"], w=["mid"])
                            ts("dve", junk[:, 0:nk], acc[:, 0:nk], mid[:, 0:1], 0.0, ALU.is_ge, ALU.add, r=["acc", "mid"], w=["junk", "cnt"], accum=cnt)
                            ts("dve", pw, cnt, 255.5, f, ALU.is_ge, ALU.mult, r=["cnt"], w=["pw"])
                            stt(lo, pw, w0[:, 0:1], lo, ALU.mult, ALU.add, r=["pw", "w0", "lo"], w=["lo"])
                        ts("dve", negm[:, 0:nk], acc[:, 0:nk], lo[:, 0:1], 1.0, ALU.is_ge, ALU.subtract, r=["acc", "lo"], w=[nk_])
                    else:
                        ts("dve", negm[:, 0:nk], acc[:, 0:nk], -1.0e8, 1.0, ALU.is_ge, ALU.subtract, r=["acc"], w=[nk_])

                def stageB(ib):
                    tg = ib // TPG; j = ib % TPG
                    qT = qTs[tg % 2]; negm = negms[ib % 2]
                    qk = ("qT", tg % 2); nk_ = ("negm", ib % 2)
                    qc = slice(j * 128, (j + 1) * 128)
                    for g in range(2):
                        abs_ = [AB.next(), AB.next()]
                        pend = []

                        def emit_pv1(item, g=g, abs_=abs_):
                            pi, kt, half = item
                            ab = abs_[half]
                            for h in range(4):
                                mm(ps[ab][:, h * 66:(h + 1) * 66], pT[pi][:, h * 128:(h + 1) * 128], VA[:, kt, g, :],
                                   kt == 0 and h == 0, kt == ib and h == 3, r=[("pT", pi), ("VA", kt // TPG)], w=[PK[ab]])

                        for kt in range(ib + 1):
                            for half in range(2):
                                sbk = SB.next()
                                mm(ps[sbk], kT[:, g, kt * 128:(kt + 1) * 128], qT[:, 8 * g + 4 * half:8 * g + 4 * half + 4, qc], True, False,
                                   r=[("kT", kt // TPG), qk], w=[PK[sbk]])
                                mm(ps[sbk], negm[:, kt * 128:(kt + 1) * 128], irep, False, True, r=[nk_, "cb"], w=[PK[sbk]])
                                pi = pTrot.next()
                                act(pT[pi], ps[sbk], AF.Exp, r=[PK[sbk]], w=[("pT", pi)], scale=0.125)
                                pend.append((pi, kt, half))
                                if len(pend) > 2:
                                    emit_pv1(pend.pop(0))
                        while pend:
                            emit_pv1(pend.pop(0))
                        for half in range(2):
                            ab = abs_[half]
                            accw = ps[ab][:, 0:264].rearrange("p (h c) -> p h c", c=66)
                            recip(rec8[:, 0:4], accw[:, :, 64], r=[PK[ab]], w=["rec8"])
                            yv = ytok[:, (8 * g + 4 * half) * 64:(8 * g + 4 * half + 4) * 64].rearrange("p (h d) -> p h d", d=64)
                            tt("dve", yv, accw[:, :, 0:64], rec8[:, 0:4].unsqueeze(2).to_broadcast([128, 4, 64]), ALU.mult, r=[PK[ab], "rec8"], w=["ytok"])
                    for half in range(2):
                        b = MB.next()
                        for c4 in range(4):
                            cc = half * 4 + c4
                            transp(ps[b][:, c4 * 128:(c4 + 1) * 128], ytok[:, cc * 128:(cc + 1) * 128], identf, r=["ytok", "cf"], w=[PK[b]])
                        cp("act", yT[:, half * 4:half * 4 + 4, qc], ps[b].rearrange("p (c q) -> p c q", q=128), r=[PK[b]], w=["yT"])

                nq = ntg_run * TPG
                proj1(0)
                stageA(0)
                for ib in range(nq):
                    nx = ib + 1
                    if nx < nq:
                        if nx % TPG == 0:
                            proj1(nx // TPG)
                        stageA(nx)
                    stageB(ib)
                    if ib % TPG == TPG - 1:
                        outproj(ib // TPG)

            if stop_after == ("mix", layer):
                break
            S.barrier()
            C = Carver()
            hT = C.get([128, KC, SEQ], BF16)
            nsq = [C.get([128, 512], BF16) for _ in range(2)]
            ntf = [C.get([128, 512]) for _ in range(2)]
            rstd = C.get([128, 512])
            SL = [2, 4, 5, 5, 6]
            WG = [C.get([128, KC, 768], BF16) for _ in range(2)]
            WU = [C.get([128, KC, 768], BF16) for _ in range(2)]
            WDn = [C.get([128, 6, D], BF16) for _ in range(2)]
            actb = [C.get([128, 6, 512], BF16) for _ in range(2)]
            sil = [C.get([128, 512]) for _ in range(2)]
            gf_ap = mod[layer][:, 40:48, s]
            shf_ap = mod[layer][:, 24:32, s]
            wg_d = dr["l%d_ffn_wg" % layer].rearrange("(k p) n -> p k n", p=128)
            wu_d = dr["l%d_ffn_wu" % layer].rearrange("(k p) n -> p k n", p=128)
            wd_d = dr["l%d_ffn_wd" % layer].rearrange("(f p) n -> p f n", p=128)

            def load_slice(si):
                bi = si % 2
                f0 = sum(SL[:si]); nf_ = SL[si]
                for k in range(0, KC, 4):
                    wdma(WG[bi][:, k:k + 4, 0:nf_ * 128], wg_d[:, k:k + 4, f0 * 128:(f0 + nf_) * 128], [("WG", bi)], "WG%d" % bi)
                    wdma(WU[bi][:, k:k + 4, 0:nf_ * 128], wu_d[:, k:k + 4, f0 * 128:(f0 + nf_) * 128], [("WU", bi)], "WU%d" % bi)
                h_ = max(nf_ // 2, 1)
                wdma(WDn[bi][:, 0:h_, :], wd_d[:, f0:f0 + h_, :], [("WD", bi)], "WD%d" % bi)
                wdma(WDn[bi][:, h_:nf_, :], wd_d[:, f0 + h_:f0 + nf_, :], [("WD", bi)], "WD%d" % bi)

            load_slice(0)
            load_slice(1)
            for t4 in range(SEQ // 512):
                hv = hT[:, :, t4 * 512:(t4 + 1) * 512]
                norm_group(t4, gsf[layer][:, :, s], shf_ap, hv, ("hT", t4), nsq, ntf, rstd, ["gs%d" % layer, "mod%d" % layer], width=512)
            GB = Rot([0, 1, 2, 3])
            DB = Rot([4, 5, 6, 7])
            for si in range(len(SL)):
                bi = si % 2
                nf_ = SL[si]
                for t4 in range(SEQ // 512):
                    cols = slice(t4 * 512, (t4 + 1) * 512)
                    ai = (si * 4 + t4) % 2
                    for f in range(nf_):
                        bg = GB.next(); bu = GB.next()
                        for k in range(KC):
                            mm(ps[bg], WG[bi][:, k, f * 128:(f + 1) * 128], hT[:, k, cols], k == 0, k == KC - 1, r=[("WG", bi), ("hT", t4)], w=[PK[bg]])
                        for k in range(KC):
                            mm(ps[bu], WU[bi][:, k, f * 128:(f + 1) * 128], hT[:, k, cols], k == 0, k == KC - 1, r=[("WU", bi), ("hT", t4)], w=[PK[bu]])
                        sl_ = sil[f % 2]
                        act(sl_, ps[bg], AF.Silu, r=[PK[bg]], w=[("sil", f % 2)])
                        tt("dve", actb[ai][:, f, :], sl_, ps[bu], ALU.mult, r=[("sil", f % 2), PK[bu]], w=[("actb", ai)])
                    for ko in range(KC):
                        b = DB.next()
                        for f in range(nf_):
                            mm(ps[b], WDn[bi][:, f, ko * 128:(ko + 1) * 128], actb[ai][:, f, :], f == 0, f == nf_ - 1, r=[("WD", bi), ("actb", ai)], w=[PK[b]])
                        tgs = [("xT", t) for t in range(t4 * 512 // TG, (t4 + 1) * 512 // TG)]
                        stt(xT[:, ko, cols], ps[b], gf_ap[:, ko:ko + 1], xT[:, ko, cols], ALU.mult, ALU.add, r=[PK[b], "mod%d" % layer] + tgs, w=tgs)
                if si + 2 < len(SL):
                    load_slice(si + 2)
            if stop_after == ("ffn", layer):
                break

        S.cut_off = True
        S.barrier()
        C = Carver()
        nsq = [C.get([128, 512], BF16) for _ in range(2)]
        ntf = [C.get([128, 512]) for _ in range(2)]
        rstd = C.get([128, 512])
        ob = [C.get([128, KC, 512]) for _ in range(2)]
        for t4 in range(SEQ // 512):
            o = ob[t4 % 2]
            if stop_after is None:
                c0 = t4 * 512
                b = bankrot.next()
                for k in range(KC):
                    sq = nsq[k % 2]
                    act(sq, xT[:, k, c0:c0 + 512], AF.Square, r=[("xT", c0 // TG), ("xT", c0 // TG + 1)], w=[("nsq", k % 2)])
                    mm(ps[b], onesb, sq, k == 0, k == KC - 1, r=[("nsq", k % 2), "cb"], w=[PK[b]])
                act(rstd, ps[b], AF.Ln, r=[PK[b], "c_eps"], w=["rstd"], bias=c_eps[:, 0:1], scale=1.0 / D)
                act(rstd, rstd, AF.Exp, r=["rstd"], w=["rstd"], scale=-0.5)
                for k in range(KC):
                    stt(o[:, k, :], xT[:, k, c0:c0 + 512], vecs[:, V_NFIN + k:V_NFIN + k + 1], rstd, ALU.mult, ALU.mult,
                        r=[("xT", c0 // TG), ("xT", c0 // TG + 1), "rstd", "vecs"], w=[("ob", t4 % 2)])
            else:
                cp("dve", o, xT[:, :, t4 * 512:(t4 + 1) * 512], r=[("xT", t) for t in range(NTG)], w=[("ob", t4 % 2)])
            for k in range(KC):
                S.dma("sp", lambda e, o=o, k=k, s=s, t4=t4: e.dma_start(out=out_d[s, :, k, t4 * 512:(t4 + 1) * 512], in_=o[:, k, :]),
                      r=[("ob", t4 % 2)], sname="ob%d" % (t4 % 2))
    for nm_ in ("ob0", "ob1"):
        d = S.dsem[nm_]
        S.wait_tok("sp", (d[0], d[1], "dma"))
    S.replay()
    return nc


_CACHE = {}


def _prep_inputs(inputs):
    vecs, pet = pack_vecs(inputs)
    cbv, cfv = make_consts()
    x = np.asarray(inputs["x"], np.float32)
    c = np.asarray(inputs["c"], np.float32)
    shared = {name: np.ascontiguousarray(np.asarray(inputs[name], np.float32)) for name, _ in WEIGHTS}
    shared.update({"vecs": vecs, "pet": pet, "cb": cbv, "cf": cfv})
    in_maps = []
    for i in range(NCORES):
        xs = x[2 * i:2 * i + 2]
        xT = np.ascontiguousarray(xs.reshape(2, SEQ, KC, 128).transpose(0, 3, 2, 1))
        cs = c[2 * i:2 * i + 2]
        cT = np.ascontiguousarray(cs.reshape(2, KC, 128).transpose(2, 1, 0))
        m = dict(shared)
        m["xT"] = xT
        m["cT"] = cT
        in_maps.append(m)
    return in_maps


def kernel(**inputs):
    if "nc" not in _CACHE:
        _CACHE["nc"] = build_program()
    nc = _CACHE["nc"]
    in_maps = _prep_inputs(inputs)
    res = run_bass_kernel_spmd(nc, in_maps, core_ids=list(range(NCORES)))
    out = np.empty((16, SEQ, D), np.float32)
    for i in range(NCORES):
        oT = res.results[i]["outT"]
        out[2 * i:2 * i + 2] = oT.transpose(0, 3, 2, 1).reshape(2, SEQ, D)
    return out
```
